# Optimizing a Trainium2 kernel written in Bass

```python
import jax
import jax.numpy as jnp
from jax import lax
import numpy as np

D_MODEL = 1024
BATCH = 4
SEQ = 4096
DEPTH = 2
DEC_BATCH = 32
DEC_SEQ = 1
PAST_LEN = 8192
PAGE_SIZE = 128

HEAD_DIM = 64
H_FOX = (3 * D_MODEL) // (8 * HEAD_DIM)
H_MLSTM = (3 * D_MODEL) // (8 * HEAD_DIM)
POOL_WINDOWS = (2, 4, 8, 16)
POOL_GROUP = D_MODEL // 16
FOX_W = H_FOX * HEAD_DIM
MLSTM_W = H_MLSTM * HEAD_DIM
POOL_W = len(POOL_WINDOWS) * POOL_GROUP
D_MIX = FOX_W + MLSTM_W + POOL_W
POOL_BUF = max(POOL_WINDOWS) - 1
IN_SPLITS = (FOX_W, FOX_W, FOX_W, MLSTM_W, MLSTM_W, MLSTM_W, MLSTM_W, POOL_W, H_FOX, H_MLSTM, H_MLSTM)
IN_W = sum(IN_SPLITS)
Q_BLOCK = 128
MLSTM_CHUNK = 128
D_FF = ((8 * D_MODEL // 3 + 255) // 256) * 256
N_EXPERTS = 8
TOP_K = 2
E_FF = D_FF
EPS = 1e-6

kernel_name = 'hybrid_fox_mlstm_pool_adaln_step'


def rmsnorm(x, w):
    x32 = x.astype(jnp.float32)
    y = x32 * lax.rsqrt(jnp.mean(x32 * x32, axis=-1, keepdims=True) + EPS)
    return (y * w.astype(jnp.float32)).astype(x.dtype)


def split_in(z):
    offs, acc = [], 0
    for w in IN_SPLITS[:-1]:
        acc += w
        offs.append(acc)
    return jnp.split(z, offs, axis=-1)


def fox_attend(q, k, v, Fq, Fk, pos_q, pos_k):
    s = jnp.einsum('bqhd,bkhd->bhqk', q, k) * (HEAD_DIM ** -0.5)
    s = s + jnp.transpose(Fq, (0, 2, 1))[..., :, None] - jnp.transpose(Fk, (0, 2, 1))[..., None, :]
    s = jnp.where(pos_k[None, :] <= pos_q[:, None], s, -jnp.inf)
    p = jax.nn.softmax(s, axis=-1)
    return jnp.einsum('bhqk,bkhd->bqhd', p, v)


def fox_prompt(q, k, v, lf):
    B, S, H, d = q.shape
    F = jnp.cumsum(lf, axis=1)
    nb = S // Q_BLOCK
    qb = q.reshape(B, nb, Q_BLOCK, H, d).transpose(1, 0, 2, 3, 4)
    Fb = F.reshape(B, nb, Q_BLOCK, H).transpose(1, 0, 2, 3)
    pb = jnp.arange(S).reshape(nb, Q_BLOCK)
    pos_k = jnp.arange(S)
    out = lax.map(lambda a: fox_attend(a[0], k, v, a[1], F, a[2], pos_k), (qb, Fb, pb))
    return out.transpose(1, 0, 2, 3, 4).reshape(B, S, H, d)


def mlstm_chunk(q, k, v, li, lf, C, n, m):
    L = q.shape[1]
    b = jnp.cumsum(lf, axis=1).transpose(0, 2, 1)
    li_t = li.transpose(0, 2, 1)
    causal = jnp.tril(jnp.ones((L, L), dtype=bool))
    D = jnp.where(causal, b[..., :, None] - b[..., None, :] + li_t[..., None, :], -jnp.inf)
    inter = b + m[..., None]
    m_t = jnp.maximum(inter, jnp.max(D, axis=-1))
    a_inter = jnp.exp(inter - m_t)
    S = jnp.einsum('blhd,bshd->bhls', q, k) * jnp.exp(D - m_t[..., None])
    num = a_inter[..., None] * jnp.einsum('blhd,bhde->bhle', q, C) + jnp.einsum('bhls,bshe->bhle', S, v)
    den = a_inter * jnp.einsum('blhd,bhd->bhl', q, n) + jnp.sum(S, axis=-1)
    h = num / jnp.maximum(jnp.abs(den), jnp.exp(-m_t))[..., None]
    b_end = b[..., -1]
    g = b_end[..., None] - b + li_t
    m_new = jnp.maximum(b_end + m, jnp.max(g, axis=-1))
    a_state = jnp.exp(b_end + m - m_new)
    wg = jnp.exp(g - m_new[..., None])
    C_new = a_state[..., None, None] * C + jnp.einsum('bhs,bshd,bshe->bhde', wg, k, v)
    n_new = a_state[..., None] * n + jnp.einsum('bhs,bshd->bhd', wg, k)
    return h.transpose(0, 2, 1, 3), (C_new, n_new, m_new)


def mlstm_prompt(q, k, v, li, lf):
    B, S, H, d = q.shape
    nc = S // MLSTM_CHUNK

    def to_chunks(a):
        return a.reshape((B, nc, MLSTM_CHUNK) + a.shape[2:]).swapaxes(0, 1)

    init = (jnp.zeros((B, H, d, d), q.dtype), jnp.zeros((B, H, d), q.dtype), jnp.zeros((B, H), q.dtype))

    def step(carry, xs):
        h, new = mlstm_chunk(*xs, *carry)
        return new, h

    final, hs = lax.scan(step, init, (to_chunks(q), to_chunks(k), to_chunks(v), to_chunks(li), to_chunks(lf)))
    return hs.swapaxes(0, 1).reshape(B, S, H, d), final


def pool_mix(u_ext, n_prefix, pos0, w_pool, scale):
    B, Lx, _ = u_ext.shape
    T = Lx - n_prefix
    cs = jnp.concatenate([jnp.zeros((B, 1, POOL_W), u_ext.dtype), jnp.cumsum(u_ext, axis=1)], axis=1)
    end = n_prefix + 1 + np.arange(T)
    pos = pos0 + np.arange(T)
    u_new = u_ext[:, n_prefix:]
    diffs = []
    for g, w in enumerate(POOL_WINDOWS):
        sl = slice(g * POOL_GROUP, (g + 1) * POOL_GROUP)
        start = np.maximum(end - w, 0)
        cnt = jnp.asarray(np.minimum(pos + 1, w), jnp.float32)[None, :, None]
        mean = (cs[:, end][..., sl] - cs[:, start][..., sl]) / cnt
        diffs.append(mean - u_new[..., sl])
    d = jnp.stack(diffs, axis=2)
    y = jnp.einsum('btgc,gce->btge', d, w_pool.astype(jnp.float32)).reshape(B, T, POOL_W)
    return y * scale.astype(jnp.float32)


def token_mixers(h, p, past):
    B, T, _ = h.shape
    f32 = jnp.float32
    z = (h @ p['w_in']).astype(f32)
    qf, kf, vf, qm, km, vm, om, u, ff, ig, fg = split_in(z)
    qf = qf.reshape(B, T, H_FOX, HEAD_DIM)
    kf = kf.reshape(B, T, H_FOX, HEAD_DIM)
    vf = vf.reshape(B, T, H_FOX, HEAD_DIM)
    qm = qm.reshape(B, T, H_MLSTM, HEAD_DIM)
    km = km.reshape(B, T, H_MLSTM, HEAD_DIM) * (HEAD_DIM ** -0.5)
    vm = vm.reshape(B, T, H_MLSTM, HEAD_DIM)
    lf_fox = jax.nn.log_sigmoid(ff + p['b_fox_f'].astype(f32))
    li_m = ig + p['b_mlstm_i'].astype(f32)
    lf_m = jax.nn.log_sigmoid(fg + p['b_mlstm_f'].astype(f32))
    if past is None:
        o_fox = fox_prompt(qf, kf, vf, lf_fox)
        h_m, (C, n, m) = mlstm_prompt(qm, km, vm, li_m, lf_m)
        o_pool = pool_mix(u, 0, 0, p['w_pool'], p['pool_scale'])
        buf = u[:, -POOL_BUF:]
    else:
        k_past, v_past, lf_past, C0, n0, m0, buf0 = [a.astype(f32) for a in past]
        p_len = k_past.shape[1]
        k_all = jnp.concatenate([k_past, kf], axis=1)
        v_all = jnp.concatenate([v_past, vf], axis=1)
        F = jnp.cumsum(jnp.concatenate([lf_past, lf_fox], axis=1), axis=1)
        o_fox = fox_attend(qf, k_all, v_all, F[:, p_len:], F, p_len + jnp.arange(T), jnp.arange(p_len + T))
        h_m, (C, n, m) = mlstm_chunk(qm, km, vm, li_m, lf_m, C0, n0, m0)
        u_ext = jnp.concatenate([buf0, u], axis=1)
        o_pool = pool_mix(u_ext, POOL_BUF, p_len, p['w_pool'], p['pool_scale'])
        buf = u_ext[:, -POOL_BUF:]
    h_m = h_m * lax.rsqrt(jnp.mean(h_m * h_m, axis=-1, keepdims=True) + EPS)
    h_m = h_m * p['mlstm_norm_w'].astype(f32).reshape(H_MLSTM, HEAD_DIM)
    o_m = jax.nn.sigmoid(om) * h_m.reshape(B, T, MLSTM_W)
    mix = jnp.concatenate([o_fox.reshape(B, T, FOX_W), o_m, o_pool], axis=-1).astype(h.dtype)
    y = mix @ p['w_out']
    dt = h.dtype
    state = (kf.astype(dt), vf.astype(dt), lf_fox.astype(dt), C.astype(dt), n.astype(dt), m.astype(dt), buf.astype(dt))
    return y, state


def swiglu(h, wg, wu, wd):
    return (jax.nn.silu(h @ wg) * (h @ wu)) @ wd


def moe(h, w_router, wg, wu, wd):
    B, T, D = h.shape
    xt = h.reshape(B * T, D)
    probs = jax.nn.softmax((xt @ w_router).astype(jnp.float32), axis=-1)
    top_p, top_i = lax.top_k(probs, TOP_K)
    top_p = top_p / jnp.sum(top_p, axis=-1, keepdims=True)
    gates = jnp.sum(jax.nn.one_hot(top_i, N_EXPERTS, dtype=jnp.float32) * top_p[..., None], axis=1)
    y = jnp.zeros((B * T, D), jnp.float32)
    for e in range(N_EXPERTS):
        y = y + gates[:, e:e + 1] * swiglu(xt, wg[e], wu[e], wd[e]).astype(jnp.float32)
    return y.reshape(B, T, D)


def layer(x, c, l, P, past):
    mod = jax.nn.silu(c) @ P['w_ada'][l] + P['b_ada'][l]
    sh1, sc1, g1, sh2, sc2, g2 = jnp.split(mod[:, None, :], 6, axis=-1)
    h = rmsnorm(x, P['norm1_w'][l]) * (1 + sc1) + sh1
    names = ('w_in', 'b_fox_f', 'b_mlstm_i', 'b_mlstm_f', 'mlstm_norm_w', 'w_pool', 'pool_scale', 'w_out')
    p = {name: P[name][l] for name in names}
    y, state = token_mixers(h, p, past)
    x = x + (g1 * y).astype(x.dtype)
    h = rmsnorm(x, P['norm2_w'][l]) * (1 + sc2) + sh2
    j = l // 2
    if l % 2 == 0:
        f = swiglu(h, P['w_ffn_gate'][j], P['w_ffn_up'][j], P['w_ffn_down'][j])
    else:
        f = moe(h, P['w_router'][j], P['w_exp_gate'][j], P['w_exp_up'][j], P['w_exp_down'][j])
    x = x + (g2 * f).astype(x.dtype)
    return x, state


def setup_inputs(seed: int = 0) -> dict:
    key = jax.random.key(seed)
    keys = jax.random.split(key, 40)
    f32 = jnp.float32

    def nrm(i, shape, s=1.0):
        return jax.random.normal(keys[i], shape, f32) * s

    n_pages = PAST_LEN // PAGE_SIZE
    n_used = DEC_BATCH * n_pages
    n_pool = n_used + (n_used + 3) // 4
    page_table = jax.random.permutation(keys[0], n_pool)[:n_used].reshape(DEC_BATCH, n_pages).astype(jnp.int32)
    n_dense = (DEPTH + 1) // 2
    n_moe = DEPTH // 2
    D = D_MODEL
    return {
        'x_prompt': nrm(1, (BATCH, SEQ, D)),
        'x_sample': nrm(2, (DEC_BATCH, DEC_SEQ, D)),
        'c_prompt': nrm(3, (BATCH, D)),
        'c_sample': nrm(4, (DEC_BATCH, D)),
        'cache_fox_k': nrm(5, (DEPTH, n_pool, PAGE_SIZE, H_FOX, HEAD_DIM)),
        'cache_fox_v': nrm(6, (DEPTH, n_pool, PAGE_SIZE, H_FOX, HEAD_DIM)),
        'cache_fox_lf': jax.nn.log_sigmoid(3.0 + nrm(7, (DEPTH, n_pool, PAGE_SIZE, H_FOX))),
        'page_table': page_table,
        'state_mlstm_C': nrm(8, (DEPTH, DEC_BATCH, H_MLSTM, HEAD_DIM, HEAD_DIM), 0.1),
        'state_mlstm_n': nrm(9, (DEPTH, DEC_BATCH, H_MLSTM, HEAD_DIM), 0.1),
        'state_mlstm_m': 1.0 + nrm(10, (DEPTH, DEC_BATCH, H_MLSTM), 0.5),
        'state_pool': nrm(11, (DEPTH, DEC_BATCH, POOL_BUF, POOL_W)),
        'w_ada': nrm(12, (DEPTH, D, 6 * D), 0.5 * D ** -0.5),
        'b_ada': nrm(13, (DEPTH, 6 * D), 0.01),
        'norm1_w': 1.0 + nrm(14, (DEPTH, D), 0.01),
        'norm2_w': 1.0 + nrm(15, (DEPTH, D), 0.01),
        'w_in': nrm(16, (DEPTH, D, IN_W), D ** -0.5),
        'b_fox_f': 3.0 + nrm(17, (DEPTH, H_FOX), 0.5),
        'b_mlstm_i': nrm(18, (DEPTH, H_MLSTM), 0.1),
        'b_mlstm_f': jnp.linspace(3.0, 6.0, H_MLSTM, dtype=f32)[None, :] + nrm(19, (DEPTH, H_MLSTM), 0.01),
        'mlstm_norm_w': 1.0 + nrm(20, (DEPTH, MLSTM_W), 0.01),
        'w_pool': nrm(21, (DEPTH, len(POOL_WINDOWS), POOL_GROUP, POOL_GROUP), POOL_GROUP ** -0.5),
        'pool_scale': 1.0 + nrm(22, (DEPTH, POOL_W), 0.1),
        'w_out': nrm(23, (DEPTH, D_MIX, D), D_MIX ** -0.5),
        'w_ffn_gate': nrm(24, (n_dense, D, D_FF), D ** -0.5),
        'w_ffn_up': nrm(25, (n_dense, D, D_FF), D ** -0.5),
        'w_ffn_down': nrm(26, (n_dense, D_FF, D), D_FF ** -0.5),
        'w_router': nrm(27, (n_moe, D, N_EXPERTS), D ** -0.5),
        'w_exp_gate': nrm(28, (n_moe, N_EXPERTS, D, E_FF), D ** -0.5),
        'w_exp_up': nrm(29, (n_moe, N_EXPERTS, D, E_FF), D ** -0.5),
        'w_exp_down': nrm(30, (n_moe, N_EXPERTS, E_FF, D), E_FF ** -0.5),
        'final_norm_w': 1.0 + nrm(31, (D,), 0.01),
    }


def reference(x_prompt, x_sample, c_prompt, c_sample, cache_fox_k, cache_fox_v, cache_fox_lf, page_table,
              state_mlstm_C, state_mlstm_n, state_mlstm_m, state_pool, w_ada, b_ada, norm1_w, norm2_w,
              w_in, b_fox_f, b_mlstm_i, b_mlstm_f, mlstm_norm_w, w_pool, pool_scale, w_out,
              w_ffn_gate, w_ffn_up, w_ffn_down, w_router, w_exp_gate, w_exp_up, w_exp_down, final_norm_w):
    P = {'w_ada': w_ada, 'b_ada': b_ada, 'norm1_w': norm1_w, 'norm2_w': norm2_w, 'w_in': w_in,
         'b_fox_f': b_fox_f, 'b_mlstm_i': b_mlstm_i, 'b_mlstm_f': b_mlstm_f, 'mlstm_norm_w': mlstm_norm_w,
         'w_pool': w_pool, 'pool_scale': pool_scale, 'w_out': w_out, 'w_ffn_gate': w_ffn_gate,
         'w_ffn_up': w_ffn_up, 'w_ffn_down': w_ffn_down, 'w_router': w_router, 'w_exp_gate': w_exp_gate,
         'w_exp_up': w_exp_up, 'w_exp_down': w_exp_down}
    db = x_sample.shape[0]
    xp, xs = x_prompt, x_sample
    st_p, st_s = [], []
    for l in range(DEPTH):
        k_past = cache_fox_k[l][page_table].reshape(db, -1, H_FOX, HEAD_DIM)
        v_past = cache_fox_v[l][page_table].reshape(db, -1, H_FOX, HEAD_DIM)
        lf_past = cache_fox_lf[l][page_table].reshape(db, -1, H_FOX)
        past = (k_past, v_past, lf_past, state_mlstm_C[l], state_mlstm_n[l], state_mlstm_m[l], state_pool[l])
        xp, sp = layer(xp, c_prompt, l, P, None)
        xs, ss = layer(xs, c_sample, l, P, past)
        st_p.append(sp)
        st_s.append(ss)
    y_prompt = rmsnorm(xp, final_norm_w)
    y_sample = rmsnorm(xs, final_norm_w)
    fox_k_p, fox_v_p, fox_lf_p, mlstm_C_p, mlstm_n_p, mlstm_m_p, pool_buf_p = [jnp.stack(a) for a in zip(*st_p)]
    fox_k_s, fox_v_s, fox_lf_s, mlstm_C_s, mlstm_n_s, mlstm_m_s, pool_buf_s = [jnp.stack(a) for a in zip(*st_s)]
    return (y_prompt, y_sample, fox_k_p, fox_v_p, fox_lf_p, mlstm_C_p, mlstm_n_p, mlstm_m_p, pool_buf_p,
            fox_k_s, fox_v_s, fox_lf_s, mlstm_C_s, mlstm_n_s, mlstm_m_s, pool_buf_s)
```

```python
import bisect
from contextlib import ExitStack
import numpy as np
import concourse.bass as bass
import concourse.mybir as mybir
from concourse.bass_utils import run_bass_kernel_spmd

F32 = mybir.dt.float32
BF16 = mybir.dt.bfloat16
I32 = mybir.dt.int32
AF = mybir.ActivationFunctionType
ALU = mybir.AluOpType
AX = mybir.AxisListType

EPOCH = 30000
D = 1024
NH = 6
HD = 64
INW = 2962
DFF = 2816
NE = 8
EPS = 1e-6
NS = 32
NPG = 64
NPOOL = 2560
NCORES = 8
LPG = NPOOL // NCORES
CHUNKS = [("qf", 0, 384), ("kf", 384, 768), ("vf", 768, 1152), ("qm", 1152, 1536),
          ("km", 1536, 1920), ("vm", 1920, 2304), ("om", 2304, 2688), ("ug", 2688, 2962)]
POOL_W = (2, 4, 8, 16)
DBG = {"mstop": None, "tiles": None}


class _Rec:
    def __getattr__(self, name):
        def f(*a, **k):
            self.call = (name, a, k)
            return self
        return f


class Sched:
    ENGS = ("pe", "act", "dve", "pool", "sp")

    def __init__(self, nc):
        self.nc = nc
        self.ops = []
        self.last_w = {}
        self.readers = {}
        self.dkey_ops = {}
        self.out_dmas = []
        self.bar_start = 0

    def op(self, eng, fn, reads=(), writes=(), dma=False, dkey=None, out=False):
        idx = len(self.ops)
        deps = set()
        writes = list(writes) + [r for r in reads if isinstance(r, str) and r[:2] in ("PB", "PT")]
        reads = [r for r in reads if not (isinstance(r, str) and r[:2] in ("PB", "PT"))]
        for r in reads:
            w = self.last_w.get(r)
            if w is not None:
                deps.add(w)
        for w in writes:
            lw = self.last_w.get(w)
            if lw is not None:
                deps.add(lw)
            for rd in self.readers.get(w, ()):
                deps.add(rd)
        rec = _Rec()
        fn(rec)
        name_, a_, k_ = rec.call
        fn = (lambda e, name_=name_, a_=a_, k_=k_: getattr(e, name_)(*a_, **k_))
        self.ops.append(dict(eng=eng, fn=fn, deps=deps, dma=dma, dkey=dkey, sig=False))
        for r in reads:
            self.readers.setdefault(r, []).append(idx)
        for w in writes:
            self.last_w[w] = idx
            self.readers[w] = []
        if dma:
            assert dkey is not None
            self.dkey_ops.setdefault(dkey, []).append(idx)
            if out:
                self.out_dmas.append(idx)
        return idx

    def barrier(self):
        n = len(self.ops)
        deps = set()
        last = {}
        for i in range(self.bar_start, n):
            o = self.ops[i]
            if o["dma"]:
                deps.add(i)
            elif o["fn"] is not None:
                last[o["eng"]] = i
        deps.update(last.values())
        for e in self.ENGS:
            self.ops.append(dict(eng=e, fn=None, deps=set(deps), dma=False, dkey=None, sig=False))
        self.bar_start = len(self.ops)
        self.last_w = {}
        self.readers = {}

    def finish(self):
        self.ops.append(dict(eng="sp", fn=None, deps=set(self.out_dmas), dma=False, dkey=None, sig=False))

    def emit(self, stack):
        nc = self.nc
        ops = self.ops
        for i, o in enumerate(ops):
            for d in o["deps"]:
                od = ops[d]
                if od["dma"] or od["fn"] is None:
                    continue
                if od["eng"] == "pe" and o["eng"] == "pe" and not o["dma"]:
                    continue
                od["sig"] = True
        cnt = {e: 0 for e in self.ENGS}
        sems = {}

        def get_sem(name):
            if name not in sems:
                sems[name] = stack.enter_context(nc.semaphore(name))
            return sems[name]

        for o in ops:
            if o["dma"] or not o["sig"]:
                continue
            e = o["eng"]
            c = cnt[e]
            o["sem"] = get_sem("c_%s_%d" % (e, c // EPOCH))
            o["val"] = c % EPOCH + 1
            cnt[e] = c + 1
        for k, lst in self.dkey_ops.items():
            s = get_sem("d%d" % len(sems))
            for j, i in enumerate(lst):
                ops[i]["sem"] = s
                ops[i]["val"] = 16 * (j + 1)
        self.nsem = len(sems)
        per_eng = {e: [] for e in self.ENGS}
        for i, o in enumerate(ops):
            per_eng[o["eng"]].append(i)

        def collect(i, acc, depth=0):
            o = ops[i]
            for d in o["deps"]:
                od = ops[d]
                if od["fn"] is None:
                    if od["eng"] == o["eng"]:
                        continue
                    collect(d, acc, depth + 1)
                    continue
                if od["dma"]:
                    lst = self.dkey_ops[od["dkey"]]
                    n = bisect.bisect_left(lst, i)
                    s, v = od["sem"], 16 * n
                else:
                    if od["eng"] == "pe" and o["eng"] == "pe" and not o["dma"]:
                        continue
                    s, v = od["sem"], od["val"]
                key = id(s)
                if key not in acc or acc[key][1] < v:
                    acc[key] = (s, v)

        def run_engine(ename, eng):
            known = {}
            for i in per_eng[ename]:
                o = ops[i]
                acc = {}
                collect(i, acc)
                for s, v in acc.values():
                    if known.get(id(s), 0) >= v:
                        continue
                    eng.wait_ge(s, v)
                    known[id(s)] = v
                if o["fn"] is None:
                    continue
                ins = o["fn"](eng)
                if o["dma"]:
                    ins.then_inc(o["sem"], 16)
                elif o["sig"]:
                    ins.then_inc(o["sem"], 1)

        block = stack.enter_context(nc.Block())

        @block.tensor
        def _(e):
            run_engine("pe", e)

        @block.scalar
        def _(e):
            run_engine("act", e)

        @block.vector
        def _(e):
            run_engine("dve", e)

        @block.gpsimd
        def _(e):
            run_engine("pool", e)

        @block.sync
        def _(e):
            run_engine("sp", e)


class Arena:
    def __init__(self, base, width):
        self.base = base
        self.W = width
        self.off = 0

    def reset(self):
        self.off = 0

    def alloc(self, shape, dt=F32):
        p = shape[0]
        n = int(np.prod(shape[1:]))
        words = n if dt in (F32, I32) else (n + 1) // 2
        if words > 32:
            words = (words + 63) // 64 * 64
            self.off = (self.off + 63) // 64 * 64
        else:
            words = (words + 15) // 16 * 16
        v = self.base[0:p, self.off:self.off + words]
        self.off += words
        assert self.off <= self.W, ("arena overflow", self.off, self.W)
        if dt != F32:
            v = v.bitcast(dt)
        v = v[:, 0:n]
        if len(shape) == 3:
            v = v.rearrange("p (a b) -> p a b", a=shape[1])
        elif len(shape) == 4:
            v = v.rearrange("p (a b c) -> p a b c", a=shape[1], b=shape[2])
        return v


def build(ntp=32, with_samples=True, arena_kb=180, phases=None, lpg=LPG, use_cc=False, ncores=NCORES, nvs=1):
    nc = bass.Bass("TRN2", target_bir_lowering=False)
    NTOK = ntp * 128
    NT = ntp + 1

    def din(name, shape, dt=F32):
        return nc.dram_tensor(name, list(shape), dt, kind="ExternalInput").ap()

    def dout(name, shape, dt=F32):
        return nc.dram_tensor(name, list(shape), dt, kind="ExternalOutput").ap()

    def dscr(name, shape, dt=F32):
        return nc.dram_tensor(name, list(shape), dt, kind="Internal").ap()

    xp = din("xp", [NTOK, D])
    xs = din("xs", [NS, D])
    call = din("call", [NS + 1, D])
    w_ada = din("w_ada", [2, D, 6 * D])
    b_ada = din("b_ada", [2, 6 * D])
    norm1_w = din("norm1_w", [2, D])
    norm2_w = din("norm2_w", [2, D])
    w_in = din("w_in", [2, D, INW])
    gbias = din("gbias", [2, 18])
    mnorm_w = din("mnorm_w", [2, 384])
    w_pool = din("w_pool", [2, 4, 64, 64])
    pool_scale = din("pool_scale", [2, 256])
    w_out = din("w_out", [2, D, D])
    w_fg = din("w_fg", [D, DFF])
    w_fu = din("w_fu", [D, DFF])
    w_fd = din("w_fd", [DFF, D])
    w_router = din("w_router", [D, NE])
    w_eg = din("w_eg", [NE, D, DFF])
    w_eu = din("w_eu", [NE, D, DFF])
    w_ed = din("w_ed", [NE, DFF, D])
    fnorm_w = din("fnorm_w", [1, D])
    kpool = din("kpool", [2, nvs * lpg * 128, 384])
    vpool = din("vpool", [2, nvs * lpg * 128, 384])
    lfc = din("lfc", [2 * NPOOL, 768])
    ptab = din("ptab", [NS * NPG, 1], I32)
    gbase = din("gbase", [1, lpg])
    stC = din("stC", [2, NS, NH, HD, HD])
    stn = din("stn", [2, NS, NH, HD])
    stm = din("stm", [2, NS, NH])
    stp = din("stp", [2, NS, 15, 256])

    yp = dout("yp", [NTOK, D])
    ys = dout("ys", [NS, D])
    fkp = dout("fkp", [2, NTOK, 384])
    fvp = dout("fvp", [2, NTOK, 384])
    flp = dout("flp", [2, NTOK, NH])
    mCp = dout("mCp", [2, NH, HD, HD])
    mnp = dout("mnp", [2, NH, HD])
    mmp = dout("mmp", [2, NH])
    pbp = dout("pbp", [2, 15, 256])
    fks = dout("fks", [2, NS, 384])
    fvs = dout("fvs", [2, NS, 384])
    fls = dout("fls", [2, NS, NH])
    mCs = dout("mCs", [2, NS, NH, HD, HD])
    mns = dout("mns", [2, NS, NH, HD])
    mms = dout("mms", [2, NS, NH])
    pbs = dout("pbs", [2, NS, 15, 256])
    ARin = dscr("ARin", [2, 2 * 96, 385])
    ARout = dscr("ARout", [2, 2 * 96, 385])

    XA = dscr("XA", [NT * 128, D])
    XB = dscr("XB", [NT * 128, D])
    WINB = dscr("WINB", [2, D, INW], BF16)
    WOUTB = dscr("WOUTB", [2, D, D], BF16)
    MODP = dscr("MODP", [2, 2, 128, 3, D])
    MODS = dscr("MODS", [2, 2, NS, 3, D])

    st = ExitStack()
    with st:
        S = Sched(nc)
        AW = arena_kb * 256
        arena_t = st.enter_context(nc.sbuf_tensor("arena", [128, AW], F32))
        AR = Arena(arena_t, AW)

        def sb(name, shape, dt=F32):
            return st.enter_context(nc.sbuf_tensor(name, shape, dt))

        PB = [st.enter_context(nc.psum_tensor("pb%d" % i, [128, 512], F32)) for i in range(6)]
        PT = [st.enter_context(nc.psum_tensor("pt%d" % i, [128, 1024], BF16)) for i in range(2)]

        def V(fn, r=(), w=()):
            S.op("dve", fn, reads=r, writes=w)

        def A(fn, r=(), w=()):
            S.op("act", fn, reads=r, writes=w)

        def G(fn, r=(), w=()):
            S.op("pool", fn, reads=r, writes=w)

        def T(fn, r=(), w=()):
            S.op("pe", fn, reads=r, writes=w)

        def DMA(out, in_, r=(), w=(), dkey=None, eng="sp", o=False):
            S.op(eng, lambda e: e.dma_start(out=out, in_=in_), reads=r, writes=w, dma=True, dkey=dkey, out=o)

        identf = sb("identf", [128, 128])
        identb = sb("identb", [128, 128], BF16)
        trif = sb("trif", [128, 128])
        trib = sb("trib", [128, 128], BF16)
        maskb = sb("maskb", [128, 128], BF16)
        onesf = sb("onesf", [128, 128])
        sel32 = sb("sel32", [NS + 1, 128])
        bandc = sb("bandc", [128, 4, 128], BF16)
        bandp = sb("bandp", [128, 4, 128], BF16)
        band0 = sb("band0", [128, 4, 128], BF16)
        ctmp = sb("ctmp", [128, 128])
        ctmp2 = sb("ctmp2", [128, 128])
        ioti = sb("ioti", [128, 128], I32)
        iotf = sb("iotf", [128, 128])

        G(lambda e: e.memset(identf[:], 1.0), w=["identf"])
        G(lambda e: e.affine_select(out=identf[:], in_=identf[:], pattern=[[-1, 128]], compare_op=ALU.is_equal,
                                    fill=0.0, base=0, channel_multiplier=1), r=["identf"], w=["identf"])
        V(lambda e: e.tensor_copy(out=identb[:], in_=identf[:]), r=["identf"], w=["identb"])
        G(lambda e: e.memset(trif[:], 1.0), w=["trif"])
        G(lambda e: e.affine_select(out=trif[:], in_=trif[:], pattern=[[1, 128]], compare_op=ALU.is_ge,
                                    fill=0.0, base=0, channel_multiplier=-1), r=["trif"], w=["trif"])
        V(lambda e: e.tensor_copy(out=trib[:], in_=trif[:]), r=["trif"], w=["trib"])
        G(lambda e: e.memset(ctmp[:], 0.0), w=["ctmp"])
        G(lambda e: e.affine_select(out=ctmp[:], in_=ctmp[:], pattern=[[1, 128]], compare_op=ALU.is_ge,
                                    fill=-1e30, base=0, channel_multiplier=-1), r=["ctmp"], w=["ctmp"])
        V(lambda e: e.tensor_copy(out=maskb[:], in_=ctmp[:]), r=["ctmp"], w=["maskb"])
        G(lambda e: e.memset(onesf[:], 1.0), w=["onesf"])
        G(lambda e: e.memset(sel32[:], 0.0), w=["sel32"])
        G(lambda e: e.memset(sel32[32:33, :], 1.0), r=["sel32"], w=["sel32"])
        G(lambda e: e.iota(ioti[:], pattern=[[1, 128]], base=1, channel_multiplier=0), w=["ioti"])
        V(lambda e: e.tensor_copy(out=iotf[:], in_=ioti[:]), r=["ioti"], w=["iotf"])
        for g, wd_ in enumerate(POOL_W):
            G(lambda e: e.memset(ctmp[:], 1.0), r=["maskb"], w=["ctmp"])
            G(lambda e: e.affine_select(out=ctmp[:], in_=ctmp[:], pattern=[[1, 128]], compare_op=ALU.is_ge,
                                        fill=0.0, base=0, channel_multiplier=-1), r=["ctmp"], w=["ctmp"])
            G(lambda e, wd_=wd_: e.affine_select(out=ctmp[:], in_=ctmp[:], pattern=[[-1, 128]], compare_op=ALU.is_ge,
                                                 fill=0.0, base=wd_ - 1, channel_multiplier=1), r=["ctmp"], w=["ctmp"])
            V(lambda e, wd_=wd_: e.scalar_tensor_tensor(out=ctmp2[:], in0=ctmp[:], scalar=1.0 / wd_, in1=identf[:],
                                                       op0=ALU.mult, op1=ALU.subtract), r=["ctmp", "identf"], w=["ctmp2"])
            V(lambda e, g=g: e.tensor_copy(out=bandc[:, g, :], in_=ctmp2[:]), r=["ctmp2"], w=["bandc"])
            V(lambda e, wd_=wd_: e.tensor_scalar(out=ctmp2[:], in0=iotf[:], scalar1=float(wd_), scalar2=None, op0=ALU.min),
              r=["iotf", "bandc"], w=["ctmp2"])
            V(lambda e: e.reciprocal(out=ctmp2[:], in_=ctmp2[:]), r=["ctmp2"], w=["ctmp2"])
            V(lambda e: e.tensor_tensor(out=ctmp2[:], in0=ctmp2[:], in1=ctmp[:], op=ALU.mult), r=["ctmp2", "ctmp"], w=["ctmp2"])
            V(lambda e: e.tensor_tensor(out=ctmp2[:], in0=ctmp2[:], in1=identf[:], op=ALU.subtract), r=["ctmp2"], w=["ctmp2"])
            V(lambda e, g=g: e.tensor_copy(out=band0[:, g, :], in_=ctmp2[:]), r=["ctmp2"], w=["band0"])
            G(lambda e, wd_=wd_: e.memset(ctmp[:], 1.0 / wd_), r=["ctmp2", "band0"], w=["ctmp"])
            G(lambda e, wd_=wd_: e.affine_select(out=ctmp[:], in_=ctmp[:], pattern=[[-1, 128]], compare_op=ALU.is_ge,
                                                 fill=0.0, base=wd_ - 129, channel_multiplier=1), r=["ctmp"], w=["ctmp"])
            V(lambda e, g=g: e.tensor_copy(out=bandp[:, g, :], in_=ctmp[:]), r=["ctmp"], w=["bandp"])

        S.barrier()
        AR.reset()
        stg = [AR.alloc([128, INW]) for _ in range(2)]
        stb = [AR.alloc([128, INW], BF16) for _ in range(2)]
        it = 0
        for l in (range(2) if (phases is None or "W" in phases) else []):
            for k in range(8):
                s_ = it % 2
                DMA(stg[s_][:, :], w_in[l, k * 128:(k + 1) * 128, :], w=["stg%d" % s_], dkey="stg%d" % s_)
                G(lambda e, s_=s_: e.tensor_copy(out=stb[s_][:, :], in_=stg[s_][:, :]), r=["stg%d" % s_], w=["stb%d" % s_])
                DMA(WINB[l, k * 128:(k + 1) * 128, :], stb[s_][:, :], r=["stb%d" % s_], w=[("WINB", l)], dkey="stb%d" % s_)
                it += 1
            for k in range(8):
                s_ = it % 2
                DMA(stg[s_][:, 0:D], w_out[l, k * 128:(k + 1) * 128, :], w=["stg%d" % s_], dkey="stg%d" % s_)
                G(lambda e, s_=s_: e.tensor_copy(out=stb[s_][:, 0:D], in_=stg[s_][:, 0:D]), r=["stg%d" % s_], w=["stb%d" % s_])
                DMA(WOUTB[l, k * 128:(k + 1) * 128, :], stb[s_][:, 0:D], r=["stb%d" % s_], w=[("WOUTB", l)], dkey="stb%d" % s_)
                it += 1

        S.barrier()
        AR.reset()
        NR = NS + 1
        cin = AR.alloc([NR, D])
        ce = AR.alloc([NR, D])
        sB = AR.alloc([NR, D], BF16)
        sT = AR.alloc([128, 8, NR], BF16)
        modall = AR.alloc([NR, 6 * D])
        wst = [AR.alloc([128, 8, 512]) for _ in range(2)]
        wbf = [AR.alloc([128, 8, 512], BF16) for _ in range(2)]
        bst = [AR.alloc([1, 512]) for _ in range(2)]
        nwb = AR.alloc([128, D])
        modP = AR.alloc([128, 3, D])
        modS = AR.alloc([NS, 3, D])
        DMA(cin[:, :], call, w=["cin"], dkey="cin")
        A(lambda e: e.activation(out=ce[:, :], in_=cin[:, :], func=AF.Exp, scale=-1.0), r=["cin"], w=["ce"])
        V(lambda e: e.tensor_scalar_add(out=ce[:, :], in0=ce[:, :], scalar1=1.0), r=["ce"], w=["ce"])
        V(lambda e: e.reciprocal(out=ce[:, :], in_=ce[:, :]), r=["ce"], w=["ce"])
        V(lambda e: e.tensor_tensor(out=sB[:, :], in0=cin[:, :], in1=ce[:, :], op=ALU.mult), r=["ce", "cin"], w=["sB"])
        for k in range(8):
            T(lambda e, k=k: e.transpose(out=PT[0][:, k * 64:k * 64 + NR], in_=sB[:, k * 128:(k + 1) * 128],
                                         identity=identb[0:NR, 0:NR]), r=["sB", "identb"], w=["PT0"])
        A(lambda e: e.copy(out=sT[:, :, :], in_=PT[0][:, 0:512].rearrange("p (a b) -> p a b", a=8)[:, :, 0:NR]), r=["PT0"], w=["sT"])
        it = 0
        for l in (range(2) if (phases is None or "ADA" in phases) else []):
            for j in range(12):
                s_ = it % 2
                DMA(wst[s_][:, :, :], w_ada[l, :, j * 512:(j + 1) * 512].rearrange("(k p) c -> p k c", p=128),
                    w=["wst%d" % s_], dkey="wst%d" % s_)
                DMA(bst[s_][:, :], b_ada[l:l + 1, j * 512:(j + 1) * 512], w=["bst%d" % s_], dkey="bst%d" % s_)
                G(lambda e, s_=s_: e.tensor_copy(out=wbf[s_][:, :, :], in_=wst[s_][:, :, :]), r=["wst%d" % s_], w=["wbf%d" % s_])
                pz = PB[it % 2]
                for k in range(8):
                    T(lambda e, k=k, s_=s_, pz=pz: e.matmul(pz[0:NR, :], lhsT=sT[:, k, :], rhs=wbf[s_][:, k, :],
                                                             start=(k == 0), stop=False),
                      r=["sT", "wbf%d" % s_], w=["PB%d" % (it % 2)])
                T(lambda e, s_=s_, pz=pz: e.matmul(pz[0:NR, :], lhsT=onesf[0:1, 0:NR], rhs=bst[s_][:, :], start=False, stop=True),
                  r=["onesf", "bst%d" % s_], w=["PB%d" % (it % 2)])
                A(lambda e, j=j, pz=pz: e.copy(out=modall[:, j * 512:(j + 1) * 512], in_=pz[0:NR, :]),
                  r=["PB%d" % (it % 2)], w=["modall"])
                it += 1
            for ph in range(2):
                nw = norm1_w if ph == 0 else norm2_w
                DMA(nwb[:, :], nw[l:l + 1, :].partition_broadcast(128), w=["nwb"], dkey="nwb")
                base = ph * 3 * D
                for part, (src, dst) in enumerate([(1, 0), (0, 1), (2, 2)]):
                    for hf in range(2):
                        c0 = base + src * D + hf * 512
                        pz = PB[2 + (part * 2 + hf) % 2]
                        pk = "PB%d" % (2 + (part * 2 + hf) % 2)
                        T(lambda e, c0=c0, pz=pz: e.matmul(pz[:, :], lhsT=sel32[:, :], rhs=modall[:, c0:c0 + 512], start=True, stop=True),
                          r=["sel32", "modall"], w=[pk])
                        if src == 1:
                            V(lambda e, pz=pz, dst=dst, hf=hf: e.scalar_tensor_tensor(
                                out=modP[:, dst, hf * 512:(hf + 1) * 512], in0=pz[:, :], scalar=1.0,
                                in1=nwb[:, hf * 512:(hf + 1) * 512], op0=ALU.add, op1=ALU.mult), r=[pk, "nwb"], w=["modP"])
                        else:
                            A(lambda e, pz=pz, dst=dst, hf=hf: e.copy(out=modP[:, dst, hf * 512:(hf + 1) * 512], in_=pz[:, :]),
                              r=[pk], w=["modP"])
                    if src == 1:
                        V(lambda e, base=base, dst=dst: e.scalar_tensor_tensor(
                            out=modS[:, dst, :], in0=modall[0:NS, base + D:base + 2 * D], scalar=1.0,
                            in1=nwb[0:NS, :], op0=ALU.add, op1=ALU.mult), r=["modall", "nwb"], w=["modS"])
                    else:
                        V(lambda e, base=base, src=src, dst=dst: e.tensor_copy(
                            out=modS[:, dst, :], in_=modall[0:NS, base + src * D:base + (src + 1) * D]), r=["modall"], w=["modS"])
                DMA(MODP[l, ph], modP[:, :, :], r=["modP"], w=[("MODP", l, ph)], dkey="modP")
                DMA(MODS[l, ph], modS[:, :, :], r=["modS"], w=[("MODS", l, ph)], dkey="modS")

        def phase_M(l):
            S.barrier()
            AR.reset()
            wch = [AR.alloc([128, 8, 384], BF16) for _ in range(2)]
            woutb = AR.alloc([128, 8, D], BF16)
            kt_w = (NH * NTOK // 2 + 63) // 64 * 64
            va_w = (ntp * NH * 66 // 2 + 63) // 64 * 64
            SAMP_W = 76 * 256
            blk = AR.alloc([128, max(kt_w + va_w, SAMP_W if with_samples else 0)])
            KT = blk[0:72, 0:NH * NTOK // 2].bitcast(BF16).rearrange("p (a b) -> p a b", a=NH)
            Vaug = blk[:, kt_w:kt_w + ntp * NH * 66 // 2].bitcast(BF16).rearrange("p (a b c) -> p a b c", a=ntp, b=NH)
            SA = Arena(blk, max(kt_w + va_w, SAMP_W))
            qs32 = AR.alloc([NS, 384])
            qms32 = AR.alloc([NS, 384])
            vms32 = AR.alloc([NS, 384])
            modp = AR.alloc([128, 3, D])
            xt = [AR.alloc([128, D]) for _ in range(2)]
            tmp = AR.alloc([128, D])
            hb = AR.alloc([128, D], BF16)
            hT = AR.alloc([128, 8, 128], BF16)
            zk = [AR.alloc([128, 384]) for _ in range(2)]
            zv = [AR.alloc([128, 384]) for _ in range(2)]
            qaug = AR.alloc([128, NH, 72], BF16)
            kaug = AR.alloc([128, NH, 72], BF16)
            QT = AR.alloc([72, NH, 128], BF16)
            qmb = AR.alloc([128, 384], BF16)
            kmf = AR.alloc([128, 384])
            vmaug = AR.alloc([128, NH, 66], BF16)
            gate = AR.alloc([128, 384])
            uf = AR.alloc([128, 256])
            ubf = [AR.alloc([128, 256], BF16) for _ in range(2)]
            gpre = AR.alloc([128, 18])
            lfn = AR.alloc([128, 18])
            lfcat = [AR.alloc([128, 12]) for _ in range(2)]
            gbb = AR.alloc([128, 18])
            carryB = AR.alloc([128, NH])
            Ff = AR.alloc([128, NH])
            r1 = AR.alloc([128, NH])
            Fabc = AR.alloc([128, NH, 3])
            negF = AR.alloc([128, NH, 3])
            tmpb = AR.alloc([128, NH], BF16)
            bm = AR.alloc([128, NH])
            Bb = AR.alloc([128, NH])
            ptb = [AR.alloc([128, 512], BF16) for _ in range(2)]
            mix = AR.alloc([128, D], BF16)
            mixT = AR.alloc([128, 8, 128], BF16)
            rden = AR.alloc([128, NH])
            ss = AR.alloc([128, 1])
            rstd = AR.alloc([128, 1])
            um = AR.alloc([128, NH])
            umax = AR.alloc([NH, 1])
            d6 = AR.alloc([NH, NH])
            mb = AR.alloc([128, NH])
            Mb = AR.alloc([128, NH])
            ab = AR.alloc([128, NH])
            wk = AR.alloc([128, NH])
            flr = AR.alloc([128, NH])
            kpb = AR.alloc([128, NH, HD], BF16)
            kpT = AR.alloc([64, NH, 128], BF16)
            qmT = AR.alloc([64, NH, 128], BF16)
            AT = AR.alloc([128, NH, 128], BF16)
            Cst = AR.alloc([64, NH, 65])
            Cbf = AR.alloc([64, NH, 66], BF16)
            hm = AR.alloc([128, NH, HD])
            sq = AR.alloc([128, NH, HD])
            hss = AR.alloc([128, NH])
            mnw = AR.alloc([128, 384])
            dT = AR.alloc([64, 4, 128], BF16)
            wpf = AR.alloc([64, 4, 64])
            wpad = AR.alloc([64, 4, 128], BF16)
            psc = AR.alloc([128, 2])
            x2o = xt

            DMA(woutb[:, :, :], WOUTB[l].rearrange("(k p) c -> p k c", p=128), r=[("WOUTB", l)], w=["woutb"], dkey="woutb")
            DMA(modp[:, :, :], MODP[l, 0], r=[("MODP", l, 0)], w=["modp"], dkey="modp")
            DMA(gbb[:, :], gbias[l:l + 1, :].partition_broadcast(128), w=["gbb"], dkey="gbb")
            DMA(mnw[:, :], mnorm_w[l:l + 1, :].partition_broadcast(128), w=["mnw"], dkey="mnw")
            DMA(wpf[:, :, :], w_pool[l].rearrange("g c e -> c g e"), w=["wpf"], dkey="wpf")
            S.op("sp", lambda e: e.dma_start(out=psc[:, :], in_=pool_scale[l].rearrange("(j p) -> p j", p=128),
                                             allow_slow_non_contiguous=True), writes=["psc"], dma=True, dkey="psc")
            G(lambda e: e.memset(wpad[:, :, :], 0.0), w=["wpad"])
            for g in range(4):
                o_ = (g % 2) * 64
                V(lambda e, g=g, o_=o_: e.tensor_copy(out=wpad[:, g, o_:o_ + 64], in_=wpf[:, g, :]), r=["wpf", "wpad"], w=["wpad"])
            G(lambda e: e.memset(qaug[:, :, :], 0.0), w=["qaug"])
            G(lambda e: e.memset(kaug[:, :, :], 0.0), w=["kaug"])
            G(lambda e: e.memset(qaug[:, :, 68:71], 1.0), r=["qaug"], w=["qaug"])
            G(lambda e: e.memset(kaug[:, :, 64:67], 1.0), r=["kaug"], w=["kaug"])
            G(lambda e: e.memset(Vaug[:, :, :, :], 1.0), w=["VaugAll"])
            G(lambda e: e.memset(vmaug[:, :, :], 1.0), w=["vmaug"])
            G(lambda e: e.memset(carryB[:, :], 0.0), w=["carryB"])
            G(lambda e: e.memset(mb[:, :], 0.0), w=["mb"])
            G(lambda e: e.memset(Cst[:, :, :], 0.0), w=["Cst"])
            G(lambda e: e.memset(ubf[1][:, :], 0.0), w=["ubf1"])
            G(lambda e: e.memset(mix[:, :], 0.0), w=["mix"])
            G(lambda e: e.memset(mixT[:, :, :], 0.0), w=["mixT"])

            for t in range(NT):
                if DBG["tiles"] is not None and t >= DBG["tiles"]:
                    continue
                smp = (t == ntp)
                if smp and not with_samples:
                    continue
                R = NS if smp else 128
                sl = t % 2
                xk = "x%d" % sl
                if smp:
                    S.barrier()
                x = xt[sl]
                rows = slice(t * 128, t * 128 + R)
                if smp:
                    DMA(modp[0:NS, :, :], MODS[l, 0], r=[("MODS", l, 0)], w=["modp"], dkey="modp")
                if l == 0:
                    src = xs if smp else xp[rows, :]
                else:
                    src = XB[rows, :]
                DMA(x[0:R, :], src, r=[("XB", t)] if l else [], w=[xk], dkey=xk)
                A(lambda e, x=x, R=R: e.activation(out=tmp[0:R, :], in_=x[0:R, :], func=AF.Square, accum_out=ss[0:R, :]),
                  r=[xk], w=["tmp", "ss"])
                if DBG.get("astop") == 2:
                    continue
                A(lambda e, R=R: e.activation(out=rstd[0:R, :], in_=ss[0:R, :], func=AF.Ln, scale=1.0 / D, bias=EPS), r=["ss"], w=["rstd"])
                if DBG.get("astop") == 3:
                    continue
                A(lambda e, R=R: e.activation(out=rstd[0:R, :], in_=rstd[0:R, :], func=AF.Exp, scale=-0.5), r=["rstd"], w=["rstd"])
                if DBG.get("astop") == 4:
                    continue
                V(lambda e, x=x, R=R: e.scalar_tensor_tensor(out=tmp[0:R, :], in0=x[0:R, :], scalar=rstd[0:R, 0:1],
                                                             in1=modp[0:R, 0, :], op0=ALU.mult, op1=ALU.mult),
                  r=[xk, "rstd", "modp", "tmp"], w=["tmp"])
                if DBG.get("astop") == 5:
                    continue
                if DBG.get("v6") == 1:
                    V(lambda e, R=R: e.tensor_copy(out=hb[0:R, :], in_=tmp[0:R, :]), r=["tmp", "modp"], w=["hb"])
                elif DBG.get("v6") == 2:
                    V(lambda e, R=R: e.tensor_tensor(out=tmp[0:R, :], in0=tmp[0:R, :], in1=modp[0:R, 1, :], op=ALU.add), r=["tmp", "modp"], w=["tmp"])
                elif DBG.get("v6") == 3:
                    G(lambda e, R=R: e.tensor_tensor(out=hb[0:R, :], in0=tmp[0:R, :], in1=modp[0:R, 1, :], op=ALU.add), r=["tmp", "modp"], w=["hb"])
                else:
                    V(lambda e, R=R: e.tensor_tensor(out=hb[0:R, :], in0=tmp[0:R, :], in1=modp[0:R, 1, :], op=ALU.add),
                      r=["tmp", "modp"], w=["hb"])
                if DBG["mstop"] == "A":
                    continue
                for k in range(8):
                    T(lambda e, k=k, R=R: e.transpose(out=PT[0][:, k * 128:k * 128 + R], in_=hb[0:R, k * 128:(k + 1) * 128],
                                                      identity=identb[0:R, 0:R]), r=["hb", "identb"], w=["PT0"])
                A(lambda e, R=R: e.copy(out=hT[:, :, 0:R], in_=PT[0][:, :].rearrange("p (a b) -> p a b", a=8)[:, :, 0:R]),
                  r=["PT0"], w=["hT"])
                if DBG["mstop"] == "B":
                    continue
                for ci, (cn, c0, c1) in enumerate(CHUNKS):
                    if DBG.get("cmax") is not None and ci >= DBG["cmax"]:
                        continue
                    wd_ = c1 - c0
                    ws = ci % 2
                    wkey = "wch%d" % ws
                    DMA(wch[ws][:, :, 0:wd_], WINB[l, :, c0:c1].rearrange("(k p) c -> p k c", p=128),
                        r=[("WINB", l)], w=[wkey], dkey=wkey)
                    pz = PB[ci % 2]
                    pk = "PB%d" % (ci % 2)
                    for k in range(8):
                        T(lambda e, k=k, ws=ws, wd_=wd_, pz=pz, R=R: e.matmul(pz[0:R, 0:wd_], lhsT=hT[:, k, 0:R], rhs=wch[ws][:, k, 0:wd_],
                                                                            start=(k == 0), stop=(k == 7)),
                          r=["hT", wkey], w=[pk])
                    pz3 = pz[0:R, 0:384].rearrange("p (h d) -> p h d", h=NH)
                    if DBG.get("cevac") == 0:
                        continue
                    if cn == "qf":
                        A(lambda e, pz3=pz3, R=R: e.mul(out=qaug[0:R, :, 0:64], in_=pz3, mul=0.125), r=[pk], w=["qaug"])
                        if smp:
                            A(lambda e, pz=pz: e.mul(out=qs32[:, :], in_=pz[0:NS, 0:384], mul=0.125), r=[pk], w=["qs32"])
                    elif cn == "kf":
                        km_ = DBG.get("kf", 7)
                        if km_ & 1:
                            V(lambda e, pz=pz, R=R, sl=sl: e.tensor_copy(out=zk[sl][0:R, :], in_=pz[0:R, 0:384]), r=[pk], w=["zk%d" % sl])
                        if km_ & 2:
                            A(lambda e, pz3=pz3, R=R: e.copy(out=kaug[0:R, :, 0:64], in_=pz3), r=[pk], w=["kaug"])
                        if km_ & 4:
                            if smp:
                                DMA(fks[l], zk[sl][0:R, :], r=["zk%d" % sl], dkey="zk%d" % sl, o=True)
                            else:
                                DMA(fkp[l, rows, :], zk[sl][0:R, :], r=["zk%d" % sl], dkey="zk%d" % sl, o=True)
                    elif cn == "vf":
                        V(lambda e, pz=pz, R=R, sl=sl: e.tensor_copy(out=zv[sl][0:R, :], in_=pz[0:R, 0:384]), r=[pk], w=["zv%d" % sl])
                        if smp:
                            DMA(fvs[l], zv[sl][0:R, :], r=["zv%d" % sl], dkey="zv%d" % sl, o=True)
                        else:
                            A(lambda e, pz3=pz3, t=t: e.copy(out=Vaug[:, t, :, 0:64], in_=pz3), r=[pk, "VaugAll"], w=[("Vaug", t)])
                            DMA(fvp[l, rows, :], zv[sl][0:R, :], r=["zv%d" % sl], dkey="zv%d" % sl, o=True)
                    elif cn == "qm":
                        A(lambda e, pz=pz, R=R: e.copy(out=qmb[0:R, :], in_=pz[0:R, 0:384]), r=[pk], w=["qmb"])
                        if smp:
                            V(lambda e, pz=pz: e.tensor_copy(out=qms32[:, :], in_=pz[0:NS, 0:384]), r=[pk], w=["qms32"])
                    elif cn == "km":
                        V(lambda e, pz=pz, R=R: e.tensor_copy(out=kmf[0:R, :], in_=pz[0:R, 0:384]), r=[pk], w=["kmf"])
                    elif cn == "vm":
                        A(lambda e, pz3=pz3, R=R: e.copy(out=vmaug[0:R, :, 0:64], in_=pz3), r=[pk], w=["vmaug"])
                        if smp:
                            V(lambda e, pz=pz: e.tensor_copy(out=vms32[:, :], in_=pz[0:NS, 0:384]), r=[pk], w=["vms32"])
                    elif cn == "om":
                        A(lambda e, pz=pz, R=R: e.activation(out=gate[0:R, :], in_=pz[0:R, 0:384], func=AF.Exp, scale=-1.0), r=[pk], w=["gate"])
                        V(lambda e, R=R: e.tensor_scalar_add(out=gate[0:R, :], in0=gate[0:R, :], scalar1=1.0), r=["gate"], w=["gate"])
                        V(lambda e, R=R: e.reciprocal(out=gate[0:R, :], in_=gate[0:R, :]), r=["gate"], w=["gate"])
                    else:
                        V(lambda e, pz=pz, R=R: e.tensor_copy(out=uf[0:R, :], in_=pz[0:R, 0:256]), r=[pk], w=["uf"])
                        A(lambda e, pz=pz, R=R, sl=sl: e.copy(out=ubf[sl][0:R, :], in_=pz[0:R, 0:256]), r=[pk], w=["ubf%d" % sl])
                        V(lambda e, pz=pz, R=R: e.tensor_tensor(out=gpre[0:R, :], in0=pz[0:R, 256:274], in1=gbb[0:R, :], op=ALU.add),
                          r=[pk, "gbb"], w=["gpre"])
                if DBG["mstop"] == "C":
                    continue
                lc = lfcat[sl]
                lck = "lfcat%d" % sl
                A(lambda e, R=R: e.activation(out=lfn[0:R, :], in_=gpre[0:R, :], func=AF.Exp, scale=-1.0), r=["gpre"], w=["lfn"])
                A(lambda e, R=R: e.activation(out=lfn[0:R, :], in_=lfn[0:R, :], func=AF.Ln, bias=1.0), r=["lfn"], w=["lfn"])
                V(lambda e, R=R, lc=lc: e.tensor_scalar(out=lc[0:R, 0:6], in0=lfn[0:R, 0:6], scalar1=-1.0, scalar2=None, op0=ALU.mult),
                  r=["lfn"], w=[lck])
                V(lambda e, R=R, lc=lc: e.tensor_scalar(out=lc[0:R, 6:12], in0=lfn[0:R, 12:18], scalar1=-1.0, scalar2=None, op0=ALU.mult),
                  r=["lfn", lck], w=[lck])
                if smp:
                    S.op("sp", lambda e, lc=lc: e.dma_start(out=fls[l], in_=lc[0:NS, 0:6]), reads=[lck], writes=[("fls", l)], dma=True, dkey=lck, out=True)
                    sample_mixers(l, locals())
                else:
                    S.op("sp", lambda e, lc=lc, rows=rows: e.dma_start(out=flp[l, rows, :], in_=lc[:, 0:6]), reads=[lck], dma=True, dkey=lck, out=True)
                    T(lambda e, lc=lc: e.matmul(PB[5][:, 0:12], lhsT=trif[:, :], rhs=lc[:, :], start=True, stop=True), r=["trif", lck], w=["PB5"])
                    T(lambda e, lc=lc: e.matmul(PB[5][:, 16:28], lhsT=onesf[:, :], rhs=lc[:, :], start=True, stop=True), r=["onesf", lck], w=["PB5"])
                    V(lambda e: e.tensor_tensor(out=Ff[:, :], in0=PB[5][:, 0:6], in1=carryB[:, :], op=ALU.add), r=["PB5", "carryB"], w=["Ff"])
                    V(lambda e: e.tensor_tensor(out=carryB[:, :], in0=PB[5][:, 16:22], in1=carryB[:, :], op=ALU.add), r=["PB5", "carryB"], w=["carryB"])
                    V(lambda e: e.tensor_copy(out=bm[:, :], in_=PB[5][:, 6:12]), r=["PB5"], w=["bm"])
                    V(lambda e: e.tensor_copy(out=Bb[:, :], in_=PB[5][:, 22:28]), r=["PB5"], w=["Bb"])
                    for i3 in range(3):
                        src3 = Ff if i3 == 0 else r1
                        V(lambda e, src3=src3: e.tensor_copy(out=tmpb[:, :], in_=src3[:, :]), r=["Ff", "r1"], w=["tmpb"])
                        V(lambda e, i3=i3: e.tensor_copy(out=Fabc[:, :, i3], in_=tmpb[:, :]), r=["tmpb"], w=["Fabc"])
                        if i3 < 2:
                            V(lambda e, i3=i3, src3=src3: e.tensor_tensor(out=r1[:, :], in0=src3[:, :], in1=Fabc[:, :, i3], op=ALU.subtract), r=["Ff", "r1", "Fabc"], w=["r1"])
                    V(lambda e: e.tensor_scalar(out=negF[:, :, :], in0=Fabc[:, :, :], scalar1=-1.0, scalar2=None, op0=ALU.mult), r=["Fabc"], w=["negF"])
                    V(lambda e: e.tensor_copy(out=qaug[:, :, 64:67], in_=Fabc[:, :, :]), r=["Fabc", "qaug"], w=["qaug"])
                    V(lambda e: e.tensor_copy(out=kaug[:, :, 68:71], in_=negF[:, :, :]), r=["negF", "kaug"], w=["kaug"])
                    if DBG["mstop"] == "D":
                        continue
                    for h in range(NH):
                        T(lambda e, h=h: e.transpose(out=PT[1][0:72, h * 128:(h + 1) * 128], in_=kaug[:, h, :], identity=identb[:, :]),
                          r=["kaug", "identb"], w=["PT1"])
                    A(lambda e, t=t: e.copy(out=KT[:, :, t * 128:(t + 1) * 128], in_=PT[1][0:72, 0:768].rearrange("p (a b) -> p a b", a=NH)),
                      r=["PT1"], w=[("KT", t)])
                    for h in range(NH):
                        T(lambda e, h=h: e.transpose(out=PT[1][0:72, h * 128:(h + 1) * 128], in_=qaug[:, h, :], identity=identb[:, :]),
                          r=["qaug", "identb"], w=["PT1"])
                    V(lambda e: e.tensor_copy(out=QT[:, :, :], in_=PT[1][0:72, 0:768].rearrange("p (a b) -> p a b", a=NH)), r=["PT1"], w=["QT"])
                    if DBG["mstop"] == "E":
                        continue
                    gi = 0
                    for h in range(NH):
                        for j0 in range(0, t + 1, 4):
                            js = list(range(j0, min(j0 + 4, t + 1)))
                            pb_ = PB[2 + gi % 2]
                            pk = "PB%d" % (2 + gi % 2)
                            pt_ = ptb[gi % 2]
                            ptk = "ptb%d" % (gi % 2)
                            for i, j in enumerate(js):
                                T(lambda e, i=i, j=j, h=h, pb_=pb_: e.matmul(pb_[:, i * 128:(i + 1) * 128], lhsT=KT[:, h, j * 128:(j + 1) * 128],
                                                                            rhs=QT[:, h, :], start=True, stop=(j != t)),
                                  r=[("KT", j), "QT"], w=[pk])
                                if j == t:
                                    T(lambda e, i=i, pb_=pb_: e.matmul(pb_[:, i * 128:(i + 1) * 128], lhsT=identb[:, :], rhs=maskb[:, :],
                                                                      start=False, stop=True), r=["identb", "maskb"], w=[pk])
                            n = len(js) * 128
                            A(lambda e, pb_=pb_, pt_=pt_, n=n: e.activation(out=pt_[:, 0:n], in_=pb_[:, 0:n], func=AF.Exp), r=[pk], w=[ptk])
                            for i, j in enumerate(js):
                                T(lambda e, i=i, j=j, h=h, pt_=pt_: e.matmul(PB[4][:, h * 65:(h + 1) * 65], lhsT=pt_[:, i * 128:(i + 1) * 128],
                                                                            rhs=Vaug[:, j, h, 0:65], start=(j == 0), stop=(j == t)),
                                  r=[ptk, ("Vaug", j)], w=["PB4"])
                            gi += 1
                    po3 = PB[4][:, 0:390].rearrange("p (h d) -> p h d", h=NH)
                    V(lambda e, po3=po3: e.reciprocal(out=rden[:, :], in_=po3[:, :, 64]), r=["PB4"], w=["rden"])
                    V(lambda e, po3=po3: e.tensor_tensor(out=mix[:, 0:384].rearrange("p (h d) -> p h d", h=NH), in0=po3[:, :, 0:64],
                                                         in1=rden[:, :].unsqueeze(2).to_broadcast([128, NH, HD]), op=ALU.mult),
                      r=["PB4", "rden"], w=["mix"])
                    if DBG["mstop"] == "F":
                        continue
                    V(lambda e: e.tensor_tensor(out=um[:, :], in0=gpre[:, 6:12], in1=bm[:, :], op=ALU.subtract), r=["gpre", "bm"], w=["um"])
                    T(lambda e: e.transpose(out=PB[5][0:NH, 0:128], in_=um[:, :], identity=identf[:, :]), r=["um", "identf"], w=["PB5"])
                    V(lambda e: e.reduce_max(out=umax[:, :], in_=PB[5][0:NH, 0:128], axis=AX.X), r=["PB5"], w=["umax"])
                    V(lambda e: e.tensor_scalar(out=d6[:, :], in0=identf[0:NH, 0:NH], scalar1=umax[:, 0:1], scalar2=None, op0=ALU.mult),
                      r=["umax", "identf"], w=["d6"])
                    T(lambda e: e.matmul(PB[5][:, 128:134], lhsT=onesf[0:NH, :], rhs=d6[:, :], start=True, stop=True), r=["d6", "onesf"], w=["PB5"])
                    V(lambda e: e.tensor_tensor(out=Mb[:, :], in0=PB[5][:, 128:134], in1=mb[:, :], op=ALU.max), r=["PB5", "mb"], w=["Mb"])
                    V(lambda e: e.tensor_tensor(out=ab[:, :], in0=mb[:, :], in1=Mb[:, :], op=ALU.subtract), r=["mb", "Mb"], w=["ab"])
                    A(lambda e: e.activation(out=ab[:, :], in_=ab[:, :], func=AF.Exp), r=["ab"], w=["ab"])
                    V(lambda e: e.tensor_tensor(out=wk[:, :], in0=um[:, :], in1=Mb[:, :], op=ALU.subtract), r=["um", "Mb"], w=["wk"])
                    A(lambda e: e.activation(out=wk[:, :], in_=wk[:, :], func=AF.Exp), r=["wk"], w=["wk"])
                    V(lambda e: e.tensor_tensor(out=mb[:, :], in0=Bb[:, :], in1=Mb[:, :], op=ALU.add), r=["Bb", "Mb", "mb", "ab"], w=["mb"])
                    V(lambda e: e.tensor_tensor(out=flr[:, :], in0=bm[:, :], in1=Mb[:, :], op=ALU.add), r=["bm", "Mb"], w=["flr"])
                    A(lambda e: e.activation(out=flr[:, :], in_=flr[:, :], func=AF.Exp, scale=-1.0), r=["flr"], w=["flr"])
                    V(lambda e: e.scalar_tensor_tensor(out=kpb[:, :, :], in0=kmf[:, :].rearrange("p (h d) -> p h d", h=NH), scalar=0.125,
                                                       in1=wk[:, :].unsqueeze(2).to_broadcast([128, NH, HD]), op0=ALU.mult, op1=ALU.mult),
                      r=["kmf", "wk"], w=["kpb"])
                    for h in range(NH):
                        T(lambda e, h=h: e.transpose(out=PT[1][0:64, h * 128:(h + 1) * 128], in_=kpb[:, h, :], identity=identb[:, :]),
                          r=["kpb", "identb"], w=["PT1"])
                    A(lambda e: e.copy(out=kpT[:, :, :], in_=PT[1][0:64, 0:768].rearrange("p (a b) -> p a b", a=NH)), r=["PT1"], w=["kpT"])
                    for h in range(NH):
                        T(lambda e, h=h: e.transpose(out=PT[0][0:64, h * 128:(h + 1) * 128], in_=qmb[:, h * 64:(h + 1) * 64], identity=identb[:, :]),
                          r=["qmb", "identb"], w=["PT0"])
                    V(lambda e: e.tensor_copy(out=qmT[:, :, :], in_=PT[0][0:64, 0:768].rearrange("p (a b) -> p a b", a=NH)), r=["PT0"], w=["qmT"])
                    for h in range(NH):
                        pb_ = PB[2 + h // 3]
                        T(lambda e, h=h, pb_=pb_: e.matmul(pb_[:, (h % 3) * 128:(h % 3 + 1) * 128], lhsT=kpT[:, h, :], rhs=qmT[:, h, :], start=True, stop=True),
                          r=["kpT", "qmT"], w=["PB%d" % (2 + h // 3)])
                    for half in range(2):
                        V(lambda e, half=half: e.tensor_tensor(out=AT[:, half * 3:(half + 1) * 3, :],
                                                               in0=PB[2 + half][:, 0:384].rearrange("p (a b) -> p a b", a=3),
                                                               in1=trif[:, :].unsqueeze(1).to_broadcast([128, 3, 128]), op=ALU.mult),
                          r=["PB%d" % (2 + half), "trif"], w=["AT"])
                    V(lambda e: e.tensor_tensor(out=Cst[:, :, :], in0=Cst[:, :, :], in1=ab[0:64, :].unsqueeze(2).to_broadcast([64, NH, 65]), op=ALU.mult),
                      r=["Cst", "ab"], w=["Cst"])
                    V(lambda e: e.tensor_copy(out=Cbf[:, :, 0:65], in_=Cst[:, :, :]), r=["Cst"], w=["Cbf"])
                    for h in range(NH):
                        T(lambda e, h=h: e.matmul(PB[5][:, h * 65:(h + 1) * 65], lhsT=AT[:, h, :], rhs=vmaug[:, h, 0:65], start=True, stop=False),
                          r=["AT", "vmaug"], w=["PB5"])
                        T(lambda e, h=h: e.matmul(PB[5][:, h * 65:(h + 1) * 65], lhsT=qmT[:, h, :], rhs=Cbf[:, h, 0:65], start=False, stop=True),
                          r=["qmT", "Cbf"], w=["PB5"])
                    pn3 = PB[5][:, 0:390].rearrange("p (h d) -> p h d", h=NH)
                    V(lambda e, pn3=pn3: e.tensor_copy(out=hss[:, :], in_=pn3[:, :, 64]), r=["PB5", "mix"], w=["hss"])
                    V(lambda e: e.scalar_tensor_tensor(out=rden[:, :], in0=hss[:, :], scalar=-1.0, in1=hss[:, :], op0=ALU.mult, op1=ALU.max), r=["hss"], w=["rden"])
                    V(lambda e: e.tensor_tensor(out=rden[:, :], in0=rden[:, :], in1=flr[:, :], op=ALU.max), r=["rden", "flr"], w=["rden"])
                    V(lambda e: e.reciprocal(out=rden[:, :], in_=rden[:, :]), r=["rden"], w=["rden"])
                    V(lambda e, pn3=pn3: e.tensor_tensor(out=hm[:, :, :], in0=pn3[:, :, 0:64], in1=rden[:, :].unsqueeze(2).to_broadcast([128, NH, HD]), op=ALU.mult),
                      r=["PB5", "rden"], w=["hm"])
                    for h in range(NH):
                        T(lambda e, h=h: e.matmul(PB[4][0:64, h * 65:(h + 1) * 65], lhsT=kpb[:, h, :], rhs=vmaug[:, h, 0:65], start=True, stop=True),
                          r=["kpb", "vmaug", "mix"], w=["PB4"])
                    V(lambda e: e.tensor_tensor(out=Cst[:, :, :], in0=Cst[:, :, :], in1=PB[4][0:64, 0:390].rearrange("p (h d) -> p h d", h=NH), op=ALU.add),
                      r=["Cst", "PB4", "Cbf"], w=["Cst"])
                    mlstm_finish(locals(), 128)
                    if DBG["mstop"] == "G":
                        continue
                    bcur = band0 if t == 0 else bandc
                    for g in range(4):
                        T(lambda e, g=g, sl=sl, bcur=bcur: e.matmul(PB[4][0:64, g * 128:(g + 1) * 128], lhsT=ubf[sl][:, g * 64:(g + 1) * 64], rhs=bcur[:, g, :],
                                                                  start=True, stop=False), r=["ubf%d" % sl, "bandc", "band0"], w=["PB4"])
                        T(lambda e, g=g, sl=sl: e.matmul(PB[4][0:64, g * 128:(g + 1) * 128], lhsT=ubf[1 - sl][:, g * 64:(g + 1) * 64], rhs=bandp[:, g, :],
                                                         start=False, stop=True), r=["ubf%d" % (1 - sl), "bandp"], w=["PB4"])
                    A(lambda e: e.copy(out=dT[:, :, :], in_=PB[4][0:64, :].rearrange("p (a b) -> p a b", a=4)), r=["PB4"], w=["dT"])
                    for c in range(2):
                        T(lambda e, c=c: e.matmul(PB[5][:, c * 128:(c + 1) * 128], lhsT=wpad[:, 2 * c, :], rhs=dT[:, 2 * c, :], start=True, stop=False),
                          r=["wpad", "dT", "hm"], w=["PB5"])
                        T(lambda e, c=c: e.matmul(PB[5][:, c * 128:(c + 1) * 128], lhsT=wpad[:, 2 * c + 1, :], rhs=dT[:, 2 * c + 1, :], start=False, stop=True),
                          r=["wpad", "dT"], w=["PB5"])
                        V(lambda e, c=c: e.tensor_scalar(out=mixT[:, 6 + c, :], in0=PB[5][:, c * 128:(c + 1) * 128], scalar1=psc[:, c:c + 1], scalar2=None, op0=ALU.mult),
                          r=["PB5", "psc"], w=["mixT"])
                    if t == ntp - 1:
                        DMA(pbp[l], uf[113:128, :], r=["uf"], dkey="uf", o=True)
                if DBG["mstop"] == "H":
                    continue
                for k in range(6):
                    T(lambda e, k=k, R=R: e.transpose(out=PT[0][:, k * 128:k * 128 + R], in_=mix[0:R, k * 128:(k + 1) * 128], identity=identb[0:R, 0:R]),
                      r=["mix", "identb"], w=["PT0"])
                A(lambda e, R=R: e.copy(out=mixT[:, 0:6, 0:R], in_=PT[0][:, 0:768].rearrange("p (a b) -> p a b", a=6)[:, :, 0:R]), r=["PT0"], w=["mixT"])
                if DBG["mstop"] == "I":
                    continue
                for hf in range(2):
                    pz = PB[hf]
                    pk = "PB%d" % hf
                    for k in range(8):
                        T(lambda e, k=k, hf=hf, pz=pz, R=R: e.matmul(pz[0:R, :], lhsT=mixT[:, k, 0:R], rhs=woutb[:, k, hf * 512:(hf + 1) * 512],
                                                                     start=(k == 0), stop=(k == 7)), r=["mixT", "woutb"], w=[pk])
                    V(lambda e, hf=hf, pz=pz, R=R: e.tensor_tensor(out=tmp[0:R, hf * 512:(hf + 1) * 512], in0=pz[0:R, :], in1=modp[0:R, 2, hf * 512:(hf + 1) * 512], op=ALU.mult),
                      r=[pk, "modp", "tmp"], w=["tmp"])
                    V(lambda e, hf=hf, x=x, R=R: e.tensor_tensor(out=x[0:R, hf * 512:(hf + 1) * 512], in0=tmp[0:R, hf * 512:(hf + 1) * 512], in1=x[0:R, hf * 512:(hf + 1) * 512], op=ALU.add),
                      r=["tmp", xk], w=[xk])
                DMA(XA[rows, :], x[0:R, :], r=[xk], w=[("XA", t)], dkey=xk)
            DMA(mCp[l].rearrange("h k v -> k h v"), Cst[:, :, 0:64], r=["Cst"], dkey="Cst", o=True)
            S.op("sp", lambda e: e.dma_start(out=mnp[l].rearrange("h k -> k h"), in_=Cst[:, :, 64], allow_slow_non_contiguous=True),
                 reads=["Cst"], dma=True, dkey="Cst", out=True)
            DMA(mmp[l:l + 1, :], mb[0:1, :], r=["mb"], dkey="mb", o=True)

        def mlstm_finish(L, R):
            hm, sq, hss, gate, mnw, mix = L["hm"], L["sq"], L["hss"], L["gate"], L["mnw"], L["mix"]
            V(lambda e: e.tensor_tensor(out=sq[0:R, :, :], in0=hm[0:R, :, :], in1=hm[0:R, :, :], op=ALU.mult), r=["hm"], w=["sq"])
            V(lambda e: e.reduce_sum(out=hss[0:R, :], in_=sq[0:R, :, :], axis=AX.X), r=["sq"], w=["hss"])
            A(lambda e: e.activation(out=hss[0:R, :], in_=hss[0:R, :], func=AF.Ln, scale=1.0 / HD, bias=EPS), r=["hss"], w=["hss"])
            A(lambda e: e.activation(out=hss[0:R, :], in_=hss[0:R, :], func=AF.Exp, scale=-0.5), r=["hss"], w=["hss"])
            V(lambda e: e.tensor_tensor(out=sq[0:R, :, :], in0=hm[0:R, :, :], in1=hss[0:R, :].unsqueeze(2).to_broadcast([R, NH, HD]), op=ALU.mult),
              r=["hm", "hss", "sq"], w=["sq"])
            V(lambda e: e.tensor_tensor(out=sq[0:R, :, :], in0=sq[0:R, :, :], in1=mnw[0:R, :].rearrange("p (h d) -> p h d", h=NH), op=ALU.mult),
              r=["sq", "mnw"], w=["sq"])
            V(lambda e: e.tensor_tensor(out=mix[0:R, 384:768].rearrange("p (h d) -> p h d", h=NH), in0=sq[0:R, :, :],
                                        in1=gate[0:R, :].rearrange("p (h d) -> p h d", h=NH), op=ALU.mult), r=["sq", "gate", "mix"], w=["mix"])

        def sample_mixers(l, L):
            SA, lc, gpre, sl = L["SA"], L["lc"], L["gpre"], L["sl"]
            zk, zv, kmf, uf, mix, mixT, hm = L["zk"][sl], L["zv"][sl], L["kmf"], L["uf"], L["mix"], L["mixT"], L["hm"]
            qs32, qms32, vms32, wpad, psc = L["qs32"], L["qms32"], L["vms32"], L["wpad"], L["psc"]
            lck = "lfcat%d" % sl
            NCH = NS * NPG // 128
            PTf = [PT[0][:, :].bitcast(F32), PT[1][:, :].bitcast(F32)]
            SA.reset()
            acct = SA.alloc([96, 2, 385])
            base0 = SA.off
            for vs in range(nvs):
                SA.off = base0
                if vs > 0:
                    S.barrier()
                Ownb = SA.alloc([128, NS, lpg], BF16)
                biasL = SA.alloc([128, lpg, NH])
                OwnT = SA.alloc([NS, lpg])
                mark = SA.off
                pti = SA.alloc([128, NCH], I32)
                ptf = SA.alloc([128, NCH])
                gidx = SA.alloc([128, lpg])
                EqF = SA.alloc([128, lpg])
                EqB = SA.alloc([128, lpg], BF16)
                lfg = SA.alloc([128, 768])
                pref = SA.alloc([128, NH, 128])
                Fs = SA.alloc([128, NH, 128])
                tot = SA.alloc([128, NH])
                lat = SA.alloc([128, NH])
                lfnb = SA.alloc([128, NH])
                BT = SA.alloc([128, 128])
                halfsel = SA.alloc([128, 2, 128], BF16)
                IndRow = SA.alloc([128, NCH, NS])
                S.op("sp", lambda e: e.dma_start(out=pti[:, :], in_=ptab.rearrange("(c p) o -> p (c o)", p=128), allow_slow_non_contiguous=True),
                     writes=["pti"], dma=True, dkey="pti")
                V(lambda e: e.tensor_copy(out=ptf[:, :], in_=pti[:, :]), r=["pti"], w=["ptf"])
                DMA(gidx[:, :], gbase.partition_broadcast(128), w=["gidx"], dkey="gidx")
                if vs > 0:
                    V(lambda e: e.tensor_scalar_add(out=gidx[:, :], in0=gidx[:, :], scalar1=float(vs * lpg)), r=["gidx"], w=["gidx"])
                G(lambda e: e.memset(BT[:, :], 1.0), w=["BT"])
                G(lambda e: e.affine_select(out=BT[:, :], in_=BT[:, :], pattern=[[-1, 128]], compare_op=ALU.is_ge, fill=0.0, base=-1, channel_multiplier=1),
                  r=["BT"], w=["BT"])
                G(lambda e: e.memset(BT[64:128, 0:64], 0.0), r=["BT"], w=["BT"])
                G(lambda e: e.memset(halfsel[:, :, :], 0.0), w=["halfsel"])
                G(lambda e: e.memset(halfsel[0:64, 0, :], 1.0), r=["halfsel"], w=["halfsel"])
                G(lambda e: e.memset(halfsel[64:128, 1, :], 1.0), r=["halfsel"], w=["halfsel"])
                G(lambda e: e.memset(IndRow[:, :, :], 1.0), w=["IndRow"])
                for s_ in range(2):
                    G(lambda e, s_=s_: e.affine_select(out=IndRow[s_ * 64:(s_ + 1) * 64, :, :], in_=IndRow[s_ * 64:(s_ + 1) * 64, :, :],
                                                       pattern=[[-2, NCH], [1, NS]], compare_op=ALU.is_equal, fill=0.0, base=-s_, channel_multiplier=0),
                      r=["IndRow"], w=["IndRow"])
                ptl = SA.alloc([128, NCH], I32)
                ptlf = SA.alloc([128, NCH])
                V(lambda e: e.tensor_scalar_add(out=ptlf[:, :], in0=ptf[:, :], scalar1=float(l * NPOOL)), r=["ptf"], w=["ptlf"])
                V(lambda e: e.tensor_copy(out=ptl[:, :], in_=ptlf[:, :]), r=["ptlf"], w=["ptl"])
                for c in range(NCH):
                    V(lambda e, c=c: e.tensor_scalar(out=EqF[:, :], in0=gidx[:, :], scalar1=ptf[:, c:c + 1], scalar2=None, op0=ALU.is_equal),
                      r=["gidx", "ptf"], w=["EqF"])
                    A(lambda e: e.copy(out=EqB[:, :], in_=EqF[:, :]), r=["EqF"], w=["EqB"])
                    for s_ in range(2):
                        T(lambda e, s_=s_: e.matmul(PTf[1][:, 0:lpg], lhsT=halfsel[:, s_, :], rhs=EqB[:, :], start=True, stop=True), r=["halfsel", "EqB"], w=["PT1"])
                        A(lambda e, c=c, s_=s_: e.copy(out=Ownb[:, 2 * c + s_, :], in_=PTf[1][:, 0:lpg]), r=["PT1"], w=["Ownb"])
                    S.op("pool", lambda e, c=c: e.indirect_dma_start(out=lfg[:, :], out_offset=None, in_=lfc,
                                                                     in_offset=bass.IndirectOffsetOnAxis(ap=ptl[:, c:c + 1], axis=0)),
                         reads=["ptl"], writes=["lfg"], dma=True, dkey="lfg")
                    V(lambda e: e.reduce_sum(out=tot[:, :], in_=lfg[:, :].rearrange("p (t h) -> p h t", h=NH), axis=AX.X), r=["lfg"], w=["tot"])
                    for s_ in range(2):
                        b_ = 2 * c + s_
                        DMA(lfnb[s_ * 64:(s_ + 1) * 64, :], fls[l, b_:b_ + 1, :].partition_broadcast(64), r=[("fls", l)], w=["lfnb"], dkey="lfnb")
                    T(lambda e: e.matmul(PTf[0][:, 0:NH], lhsT=BT[:, :], rhs=tot[:, :], start=True, stop=True), r=["BT", "tot"], w=["PT0"])
                    V(lambda e: e.tensor_tensor(out=lat[:, :], in0=PTf[0][:, 0:NH], in1=lfnb[:, :], op=ALU.add), r=["PT0", "lfnb"], w=["lat"])
                    V(lambda e: e.tensor_tensor(out=lat[:, :], in0=lat[:, :], in1=tot[:, :], op=ALU.add), r=["lat", "tot"], w=["lat"])
                    for h in range(NH):
                        V(lambda e, h=h: e.tensor_tensor_scan(out=pref[:, h, :], data0=onesf[:, :], data1=lfg[:, :].rearrange("p (t h) -> p h t", h=NH)[:, h, :],
                                                              initial=0.0, op0=ALU.mult, op1=ALU.add), r=["lfg", "onesf"], w=["pref"])
                    V(lambda e: e.tensor_tensor(out=Fs[:, :, :], in0=lat[:, :].unsqueeze(2).to_broadcast([128, NH, 128]), in1=pref[:, :, :], op=ALU.subtract),
                      r=["lat", "pref"], w=["Fs"])
                    for h in range(NH):
                        T(lambda e, h=h, c=c: e.matmul(PB[h][:, 0:lpg], lhsT=Fs[:, h, :], rhs=EqF[:, :], start=(c == 0), stop=(c == NCH - 1)),
                          r=["Fs", "EqF"], w=["PB%d" % h])
                for h in range(NH):
                    V(lambda e, h=h: e.tensor_copy(out=biasL[:, :, h], in_=PB[h][:, 0:lpg]), r=["PB%d" % h], w=["biasL"])
                for c in range(NCH):
                    V(lambda e, c=c: e.tensor_scalar(out=EqF[:, :], in0=gidx[:, :], scalar1=ptf[:, c:c + 1], scalar2=None, op0=ALU.is_equal),
                      r=["gidx", "ptf", "EqF"], w=["EqF"])
                    T(lambda e, c=c: e.matmul(PB[0][0:NS, 0:lpg], lhsT=IndRow[:, c, :], rhs=EqF[:, :], start=(c == 0), stop=(c == NCH - 1)),
                      r=["IndRow", "EqF"], w=["PB0"])
                V(lambda e: e.tensor_copy(out=OwnT[:, :], in_=PB[0][0:NS, 0:lpg]), r=["PB0"], w=["OwnT"])

                S.barrier()
                SA.off = mark
                PBAT = 4
                Kb = [SA.alloc([128, PBAT, 384]) for _ in range(2)]
                Vb = [SA.alloc([128, PBAT, 385]) for _ in range(2)]
                OQ2 = [SA.alloc([NS, PBAT, 384]) for _ in range(2)]
                sc2 = [SA.alloc([128, PBAT * NH]) for _ in range(2)]
                Pm2 = [SA.alloc([128, PBAT * NH]) for _ in range(2)]
                Pexp2 = [SA.alloc([128, PBAT, NS, NH]) for _ in range(2)]
                psets = [[(PB[0], "PB0"), (PB[1], "PB1"), (PB[2], "PB2")], [(PB[5], "PB5"), (PTf[0], "PT0"), (PTf[1], "PT1")]]
                for s_ in range(2):
                    G(lambda e, s_=s_: e.memset(Vb[s_][:, :, 384:385], 1.0), w=["Vb%d" % s_])
                nb = lpg // PBAT
                for bi in range(nb):
                    s_ = bi % 2
                    i0 = bi * PBAT
                    r0 = (vs * lpg + i0) * 128
                    OQ, sc, Pm, Pexp = OQ2[s_], sc2[s_], Pm2[s_], Pexp2[s_]
                    kOQ, ksc, kPm, kPe = "OQ%d" % s_, "sc%d" % s_, "Pm%d" % s_, "Pexp%d" % s_
                    DMA(Kb[s_][:, :, :], kpool[l, r0:r0 + PBAT * 128, :].rearrange("(i t) c -> t i c", t=128), w=["Kb%d" % s_], dkey="Kb%d" % s_)
                    DMA(Vb[s_][:, :, 0:384], vpool[l, r0:r0 + PBAT * 128, :].rearrange("(i t) c -> t i c", t=128), r=["Vb%d" % s_], w=["Vb%d" % s_], dkey="Vb%d" % s_)
                    V(lambda e, i0=i0, OQ=OQ: e.tensor_tensor(out=OQ[:, :, :], in0=qs32[:, :].unsqueeze(1).to_broadcast([NS, PBAT, 384]),
                                                       in1=OwnT[:, i0:i0 + PBAT].unsqueeze(2).to_broadcast([NS, PBAT, 384]), op=ALU.mult),
                      r=["qs32", "OwnT"], w=[kOQ])
                    OQf = OQ[:, :, :].rearrange("p a b -> p (a b)")
                    Kf = Kb[s_][:, :, :].rearrange("p a b -> p (a b)")
                    for j in range(3):
                        pj, pjk = psets[s_][j]
                        T(lambda e, j=j, OQf=OQf, pj=pj: e.matmul(pj[:, 0:512], lhsT=onesf[0:NS, :], rhs=OQf[:, j * 512:(j + 1) * 512], start=True, stop=True),
                          r=["onesf", kOQ], w=[pjk])
                        V(lambda e, j=j, Kf=Kf, pj=pj: e.tensor_tensor(out=Kf[:, j * 512:(j + 1) * 512], in0=pj[:, 0:512], in1=Kf[:, j * 512:(j + 1) * 512], op=ALU.mult),
                          r=[pjk, "Kb%d" % s_], w=["Kb%d" % s_])
                    V(lambda e, Kf=Kf, sc=sc: e.reduce_sum(out=sc[:, :], in_=Kf.rearrange("p (a d) -> p a d", d=HD), axis=AX.X), r=["Kb%d" % s_], w=[ksc])
                    V(lambda e, i0=i0, sc=sc: e.tensor_tensor(out=sc[:, :], in0=sc[:, :], in1=biasL[:, i0:i0 + PBAT, :].rearrange("p i h -> p (i h)"), op=ALU.add),
                      r=[ksc, "biasL"], w=[ksc])
                    A(lambda e, sc=sc, Pm=Pm: e.activation(out=Pm[:, :], in_=sc[:, :], func=AF.Exp), r=[ksc], w=[kPm])
                    V(lambda e, i0=i0, Pm=Pm, Pexp=Pexp: e.tensor_tensor(out=Pexp[:, :, :, :],
                                                       in0=Pm[:, :].rearrange("p (i h) -> p i h", i=PBAT).unsqueeze(2).to_broadcast([128, PBAT, NS, NH]),
                                                       in1=Ownb[:, :, i0:i0 + PBAT].rearrange("p b i -> p i b").unsqueeze(3).to_broadcast([128, PBAT, NS, NH]),
                                                       op=ALU.mult), r=[kPm, "Ownb"], w=[kPe])
                    for i in range(PBAT):
                        first = (bi == 0 and i == 0)
                        last = (bi == nb - 1 and i == PBAT - 1)
                        for hf in range(2):
                            T(lambda e, i=i, hf=hf, s_=s_, first=first, last=last, Pexp=Pexp: e.matmul(
                                PB[3 + hf][0:96, 0:385], lhsT=Pexp[:, i, hf * 16:(hf + 1) * 16, :].rearrange("p b h -> p (b h)"),
                                rhs=Vb[s_][:, i, :], start=first, stop=last), r=[kPe, "Vb%d" % s_], w=["PB%d" % (3 + hf)])
                for hf in range(2):
                    if vs == 0:
                        V(lambda e, hf=hf: e.tensor_copy(out=acct[:, hf, :], in_=PB[3 + hf][0:96, 0:385]), r=["PB%d" % (3 + hf)], w=["acct"])
                    else:
                        V(lambda e, hf=hf: e.tensor_tensor(out=acct[:, hf, :], in0=PB[3 + hf][0:96, 0:385], in1=acct[:, hf, :], op=ALU.add),
                          r=["PB%d" % (3 + hf), "acct"], w=["acct"])
            if DBG.get("sstop", 99) <= 2:
                return
            for hf in range(2):
                DMA(ARin[l, hf * 96:(hf + 1) * 96, :], acct[:, hf, :], r=["acct"], w=[("ARin", l)], dkey="acct")
            if use_cc:
                S.op("pool", lambda e: e.collective_compute("AllReduce", ALU.add, replica_groups=[list(range(ncores))],
                                                            ins=[ARin[l]], outs=[ARout[l]]),
                     reads=[("ARin", l)], writes=[("ARout", l)], dma=True, dkey="cc")
            else:
                DMA(ARout[l], ARin[l], r=[("ARin", l)], w=[("ARout", l)], dkey="cc")
            S.barrier()
            SA.off = mark
            numS = SA.alloc([NS, NH, HD])
            denS = SA.alloc([NS, NH])
            base_off = l * 192 * 385
            num_src = bass.AP(ARout.tensor, base_off, [[NH * 385, NS], [385 + HD, NH], [1, HD]])
            den_src = bass.AP(ARout.tensor, base_off + 384, [[NH * 385, NS], [385, NH], [1, 1]])
            DMA(numS[:, :, :], num_src, r=[("ARout", l)], w=["numS"], dkey="numS")
            S.op("sp", lambda e: e.dma_start(out=denS[:, :].unsqueeze(2), in_=den_src, allow_slow_non_contiguous=True),
                 reads=[("ARout", l)], writes=["denS"], dma=True, dkey="denS")
            prn = SA.alloc([NS, 384])
            sn = SA.alloc([NS, NH])
            tn3 = SA.alloc([NS, NH, HD])
            V(lambda e: e.tensor_tensor(out=prn[:, :], in0=qs32[:, :], in1=zk[0:NS, :], op=ALU.mult), r=["qs32", "zk%d" % sl], w=["prn"])
            V(lambda e: e.reduce_sum(out=sn[:, :], in_=prn[:, :].rearrange("p (h d) -> p h d", h=NH), axis=AX.X), r=["prn"], w=["sn"])
            A(lambda e: e.activation(out=sn[:, :], in_=sn[:, :], func=AF.Exp), r=["sn"], w=["sn"])
            V(lambda e: e.tensor_tensor(out=tn3[:, :, :], in0=zv[0:NS, :].rearrange("p (h d) -> p h d", h=NH),
                                        in1=sn[:, :].unsqueeze(2).to_broadcast([NS, NH, HD]), op=ALU.mult), r=["zv%d" % sl, "sn"], w=["tn3"])
            V(lambda e: e.tensor_tensor(out=numS[:, :, :], in0=numS[:, :, :], in1=tn3[:, :, :], op=ALU.add), r=["numS", "tn3"], w=["numS"])
            V(lambda e: e.tensor_tensor(out=denS[:, :], in0=denS[:, :], in1=sn[:, :], op=ALU.add), r=["denS", "sn"], w=["denS"])
            V(lambda e: e.reciprocal(out=denS[:, :], in_=denS[:, :]), r=["denS"], w=["denS"])
            V(lambda e: e.tensor_tensor(out=tn3[:, :, :], in0=numS[:, :, :], in1=denS[:, :].unsqueeze(2).to_broadcast([NS, NH, HD]), op=ALU.mult),
              r=["numS", "denS", "tn3"], w=["tn3"])
            A(lambda e: e.copy(out=mix[0:NS, 0:384], in_=tn3[:, :, :].rearrange("p h d -> p (h d)")), r=["tn3", "mix"], w=["mix"])

            if DBG.get("sstop", 99) <= 3:
                return
            S.barrier()
            SA.reset()
            Ch = [SA.alloc([64, NS, 65]) for _ in range(2)]
            qmask = SA.alloc([64, NS, NS])
            kmaskH = SA.alloc([NS, NS, HD])
            E64 = SA.alloc([64, NS, NS])
            abc = SA.alloc([64, NS * NH])
            qmTs = SA.alloc([64, NH, NS])
            m0 = SA.alloc([NS, NH])
            inter = SA.alloc([NS, NH])
            mt = SA.alloc([NS, NH])
            a_ = SA.alloc([NS, NH])
            wg_ = SA.alloc([NS, NH])
            flr_ = SA.alloc([NS, NH])
            qk = SA.alloc([NS, NH])
            den = SA.alloc([NS, NH])
            kw = SA.alloc([NS, NH, HD])
            vaugS = SA.alloc([NS, NH, 65])
            Adiag = SA.alloc([NS, NS, NH])
            qC = SA.alloc([NS, NH, 65])
            t2 = SA.alloc([NS, NH, HD])
            prq = SA.alloc([NS, 384])
            DMA(m0[:, :], stm[l], w=["m0"], dkey="m0")
            V(lambda e: e.tensor_tensor(out=inter[:, :], in0=lc[0:NS, 6:12], in1=m0[:, :], op=ALU.add), r=[lck, "m0"], w=["inter"])
            V(lambda e: e.tensor_tensor(out=mt[:, :], in0=inter[:, :], in1=gpre[0:NS, 6:12], op=ALU.max), r=["inter", "gpre"], w=["mt"])
            DMA(mms[l], mt[:, :], r=["mt"], dkey="mt", o=True)
            V(lambda e: e.tensor_tensor(out=a_[:, :], in0=inter[:, :], in1=mt[:, :], op=ALU.subtract), r=["inter", "mt"], w=["a_"])
            A(lambda e: e.activation(out=a_[:, :], in_=a_[:, :], func=AF.Exp), r=["a_"], w=["a_"])
            V(lambda e: e.tensor_tensor(out=wg_[:, :], in0=gpre[0:NS, 6:12], in1=mt[:, :], op=ALU.subtract), r=["gpre", "mt"], w=["wg_"])
            A(lambda e: e.activation(out=wg_[:, :], in_=wg_[:, :], func=AF.Exp), r=["wg_"], w=["wg_"])
            A(lambda e: e.activation(out=flr_[:, :], in_=mt[:, :], func=AF.Exp, scale=-1.0), r=["mt"], w=["flr_"])
            for h in range(NH):
                T(lambda e, h=h: e.transpose(out=PB[0][0:64, h * NS:(h + 1) * NS], in_=qms32[:, h * 64:(h + 1) * 64], identity=identf[0:NS, 0:NS]),
                  r=["qms32", "identf"], w=["PB0"])
            V(lambda e: e.tensor_copy(out=qmTs[:, :, :], in_=PB[0][0:64, 0:NH * NS].rearrange("p (a b) -> p a b", a=NH)), r=["PB0"], w=["qmTs"])
            V(lambda e: e.scalar_tensor_tensor(out=kw[:, :, :], in0=kmf[0:NS, :].rearrange("p (h d) -> p h d", h=NH), scalar=0.125,
                                               in1=wg_[:, :].unsqueeze(2).to_broadcast([NS, NH, HD]), op0=ALU.mult, op1=ALU.mult), r=["kmf", "wg_"], w=["kw"])
            G(lambda e: e.memset(vaugS[:, :, :], 1.0), w=["vaugS"])
            V(lambda e: e.tensor_copy(out=vaugS[:, :, 0:64], in_=vms32[:, :].rearrange("p (h d) -> p h d", h=NH)), r=["vms32", "vaugS"], w=["vaugS"])
            V(lambda e: e.tensor_tensor(out=Adiag[:, :, :], in0=a_[:, :].unsqueeze(1).to_broadcast([NS, NS, NH]),
                                        in1=identf[0:NS, 0:NS].unsqueeze(2).to_broadcast([NS, NS, NH]), op=ALU.mult), r=["a_", "identf"], w=["Adiag"])
            T(lambda e: e.matmul(PB[1][0:64, 0:NS * NH], lhsT=onesf[0:NS, 0:64], rhs=Adiag[:, :, :].rearrange("p a b -> p (a b)"), start=True, stop=True),
              r=["onesf", "Adiag"], w=["PB1"])
            V(lambda e: e.tensor_copy(out=abc[:, :], in_=PB[1][0:64, 0:NS * NH]), r=["PB1"], w=["abc"])
            G(lambda e: e.memset(E64[:, :, :], 1.0), w=["E64"])
            G(lambda e: e.affine_select(out=E64[:, :, :], in_=E64[:, :, :], pattern=[[1, NS], [-1, NS]], compare_op=ALU.is_equal, fill=0.0, base=0, channel_multiplier=0),
              r=["E64"], w=["E64"])
            abc3 = abc[:, :].rearrange("p (b h) -> p b h", h=NH)
            for h in range(NH):
                s_ = h % 2
                ck = "Ch%d" % s_
                DMA(Ch[s_][:, :, 0:64], stC[l, :, h, :, :].rearrange("b k v -> k b v"), w=[ck], dkey=ck)
                S.op("sp", lambda e, s_=s_, h=h: e.dma_start(out=Ch[s_][:, :, 64:65], in_=stn[l, :, h, :].rearrange("b k -> k b").unsqueeze(2), allow_slow_non_contiguous=True),
                     reads=[ck], writes=[ck], dma=True, dkey=ck)
                V(lambda e, h=h: e.tensor_tensor(out=qmask[:, :, :], in0=qmTs[:, h, :].unsqueeze(1).to_broadcast([64, NS, NS]), in1=E64[:, :, :], op=ALU.mult),
                  r=["qmTs", "E64"], w=["qmask"])
                for b_ in range(NS):
                    T(lambda e, b_=b_, s_=s_: e.matmul(PB[2][0:NS, 0:65], lhsT=qmask[:, b_, :], rhs=Ch[s_][:, b_, :], start=(b_ == 0), stop=(b_ == NS - 1)),
                      r=["qmask", ck], w=["PB2"])
                V(lambda e, h=h: e.tensor_copy(out=qC[:, h, :], in_=PB[2][0:NS, 0:65]), r=["PB2"], w=["qC"])
                V(lambda e, h=h: e.tensor_tensor(out=kmaskH[:, :, :], in0=kw[:, h, :].unsqueeze(1).to_broadcast([NS, NS, HD]),
                                                 in1=identf[0:NS, 0:NS].unsqueeze(2).to_broadcast([NS, NS, HD]), op=ALU.mult), r=["kw", "identf"], w=["kmaskH"])
                for gi7, g7 in enumerate(range(0, NS, 7)):
                    n7 = min(7, NS - g7)
                    pbk = 3 + gi7 % 2
                    for ii in range(n7):
                        T(lambda e, ii=ii, g7=g7, h=h, pbk=pbk: e.matmul(PB[pbk][0:64, ii * 65:(ii + 1) * 65], lhsT=kmaskH[:, g7 + ii, :], rhs=vaugS[:, h, :], start=True, stop=True),
                          r=["kmaskH", "vaugS"], w=["PB%d" % pbk])
                    V(lambda e, g7=g7, n7=n7, s_=s_, h=h: e.tensor_tensor(out=Ch[s_][:, g7:g7 + n7, :], in0=Ch[s_][:, g7:g7 + n7, :],
                                                                         in1=abc3[:, g7:g7 + n7, h].unsqueeze(2).to_broadcast([64, n7, 65]), op=ALU.mult),
                      r=[ck, "abc", "PB2"], w=[ck])
                    V(lambda e, g7=g7, n7=n7, s_=s_, pbk=pbk: e.tensor_tensor(out=Ch[s_][:, g7:g7 + n7, :], in0=Ch[s_][:, g7:g7 + n7, :],
                                                                             in1=PB[pbk][0:64, 0:n7 * 65].rearrange("p (a b) -> p a b", a=n7), op=ALU.add),
                      r=[ck, "PB%d" % pbk], w=[ck])
                DMA(mCs[l, :, h, :, :].rearrange("b k v -> k b v"), Ch[s_][:, :, 0:64], r=[ck], dkey=ck, o=True)
                S.op("sp", lambda e, s_=s_, h=h: e.dma_start(out=mns[l, :, h, :].rearrange("b k -> k b").unsqueeze(2), in_=Ch[s_][:, :, 64:65], allow_slow_non_contiguous=True),
                     reads=[ck], dma=True, dkey=ck, out=True)
            V(lambda e: e.tensor_tensor(out=prq[:, :], in0=qms32[:, :], in1=kmf[0:NS, :], op=ALU.mult), r=["qms32", "kmf"], w=["prq"])
            V(lambda e: e.reduce_sum(out=qk[:, :], in_=prq[:, :].rearrange("p (h d) -> p h d", h=NH), axis=AX.X), r=["prq"], w=["qk"])
            V(lambda e: e.scalar_tensor_tensor(out=qk[:, :], in0=qk[:, :], scalar=0.125, in1=wg_[:, :], op0=ALU.mult, op1=ALU.mult), r=["qk", "wg_"], w=["qk"])
            V(lambda e: e.tensor_tensor(out=hm[0:NS, :, :], in0=qC[:, :, 0:64], in1=a_[:, :].unsqueeze(2).to_broadcast([NS, NH, HD]), op=ALU.mult), r=["qC", "a_"], w=["hm"])
            V(lambda e: e.tensor_tensor(out=t2[:, :, :], in0=vms32[:, :].rearrange("p (h d) -> p h d", h=NH), in1=qk[:, :].unsqueeze(2).to_broadcast([NS, NH, HD]), op=ALU.mult),
              r=["vms32", "qk"], w=["t2"])
            V(lambda e: e.tensor_tensor(out=hm[0:NS, :, :], in0=hm[0:NS, :, :], in1=t2[:, :, :], op=ALU.add), r=["hm", "t2"], w=["hm"])
            V(lambda e: e.tensor_tensor(out=den[:, :], in0=qC[:, :, 64], in1=a_[:, :], op=ALU.mult), r=["qC", "a_"], w=["den"])
            V(lambda e: e.tensor_tensor(out=den[:, :], in0=den[:, :], in1=qk[:, :], op=ALU.add), r=["den", "qk"], w=["den"])
            V(lambda e: e.scalar_tensor_tensor(out=den[:, :], in0=den[:, :], scalar=-1.0, in1=den[:, :], op0=ALU.mult, op1=ALU.max), r=["den"], w=["den"])
            V(lambda e: e.tensor_tensor(out=den[:, :], in0=den[:, :], in1=flr_[:, :], op=ALU.max), r=["den", "flr_"], w=["den"])
            V(lambda e: e.reciprocal(out=den[:, :], in_=den[:, :]), r=["den"], w=["den"])
            V(lambda e: e.tensor_tensor(out=hm[0:NS, :, :], in0=hm[0:NS, :, :], in1=den[:, :].unsqueeze(2).to_broadcast([NS, NH, HD]), op=ALU.mult), r=["hm", "den"], w=["hm"])
            mlstm_finish(L, NS)

            if DBG.get("sstop", 99) <= 4:
                return
            bufS = SA.alloc([NS, 15, 256])
            accp = SA.alloc([NS, 256])
            dS = SA.alloc([NS, 256])
            dTs = SA.alloc([64, 4, NS], BF16)
            DMA(bufS[:, :, :], stp[l], w=["bufS"], dkey="bufS")
            for g, wd_ in enumerate(POOL_W):
                V(lambda e, g=g, wd_=wd_: e.reduce_sum(out=accp[:, g * 64:(g + 1) * 64],
                                                       in_=bufS[:, 15 - (wd_ - 1):15, g * 64:(g + 1) * 64].rearrange("p r c -> p c r"), axis=AX.X),
                  r=["bufS", "accp"], w=["accp"])
                V(lambda e, g=g: e.tensor_tensor(out=accp[:, g * 64:(g + 1) * 64], in0=accp[:, g * 64:(g + 1) * 64], in1=uf[0:NS, g * 64:(g + 1) * 64], op=ALU.add),
                  r=["accp", "uf"], w=["accp"])
                V(lambda e, g=g, wd_=wd_: e.scalar_tensor_tensor(out=dS[:, g * 64:(g + 1) * 64], in0=accp[:, g * 64:(g + 1) * 64], scalar=1.0 / wd_,
                                                                 in1=uf[0:NS, g * 64:(g + 1) * 64], op0=ALU.mult, op1=ALU.subtract), r=["accp", "uf", "dS"], w=["dS"])
            for g in range(4):
                T(lambda e, g=g: e.transpose(out=PB[0][0:64, g * NS:(g + 1) * NS], in_=dS[:, g * 64:(g + 1) * 64], identity=identf[0:NS, 0:NS]), r=["dS", "identf"], w=["PB0"])
            A(lambda e: e.copy(out=dTs[:, :, :], in_=PB[0][0:64, 0:4 * NS].rearrange("p (a b) -> p a b", a=4)), r=["PB0"], w=["dTs"])
            for c in range(2):
                T(lambda e, c=c: e.matmul(PB[1][:, c * NS:(c + 1) * NS], lhsT=wpad[:, 2 * c, :], rhs=dTs[:, 2 * c, :], start=True, stop=False), r=["wpad", "dTs"], w=["PB1"])
                T(lambda e, c=c: e.matmul(PB[1][:, c * NS:(c + 1) * NS], lhsT=wpad[:, 2 * c + 1, :], rhs=dTs[:, 2 * c + 1, :], start=False, stop=True), r=["wpad", "dTs"], w=["PB1"])
                V(lambda e, c=c: e.tensor_scalar(out=mixT[:, 6 + c, 0:NS], in0=PB[1][:, c * NS:(c + 1) * NS], scalar1=psc[:, c:c + 1], scalar2=None, op0=ALU.mult),
                  r=["PB1", "psc", "mixT"], w=["mixT"])
            DMA(pbs[l, :, 0:14, :], bufS[:, 1:15, :], r=["bufS"], dkey="bufS", o=True)
            DMA(pbs[l, :, 14, :], uf[0:NS, :], r=["uf"], dkey="uf", o=True)

        def phase_F(l):
            S.barrier()
            AR.reset()
            tiles_all = list(range(ntp)) + ([ntp] if with_samples else [])
            GS = 8
            groups = [tiles_all[i:i + GS] for i in range(0, ntp, GS)]
            if with_samples:
                groups[-1] = groups[-1] + [ntp] if ntp not in groups[-1] else groups[-1]
            TGM = (GS + 1) * 128
            h2T = AR.alloc([128, 8, TGM], BF16)
            act = AR.alloc([128, 11, TGM], BF16)
            yacc = AR.alloc([128, GS + 1, D])
            mbuf = [AR.alloc([128, D]) for _ in range(2)]
            xt = AR.alloc([128, D])
            tmp = AR.alloc([128, D])
            h2f = AR.alloc([128, D])
            hb = AR.alloc([128, D], BF16)
            wgs = [AR.alloc([128, 8, 128]) for _ in range(2)]
            wus = [AR.alloc([128, 8, 128]) for _ in range(2)]
            wgb = [AR.alloc([128, 8, 128], BF16) for _ in range(2)]
            wub = [AR.alloc([128, 8, 128], BF16) for _ in range(2)]
            wds = [AR.alloc([128, D]) for _ in range(2)]
            wdb = AR.alloc([128, 11, D], BF16)
            sgb = [AR.alloc([128, 512], BF16) for _ in range(2)]
            ss = AR.alloc([128, 1])
            rstd = AR.alloc([128, 1])
            nexp = NE if l == 1 else 1
            if l == 1:
                h2Tf = AR.alloc([128, 8, 128])
                wr = AR.alloc([128, 8, NE])
                gts = AR.alloc([128, GS + 1, NE])
                lg = AR.alloc([128, NE])
                lmx = AR.alloc([128, 1])
                ee = AR.alloc([128, NE])
                mk1 = AR.alloc([128, NE])
                e2 = AR.alloc([128, NE])
                m2 = AR.alloc([128, 1])
                DMA(wr[:, :, :], w_router.rearrange("(k p) e -> p k e", p=128), w=["wr"], dkey="wr")
            else:
                fnb = None
            if l == 1:
                fnb = AR.alloc([128, D])
                DMA(fnb[:, :], fnorm_w.partition_broadcast(128), w=["fnb"], dkey="fnb")
            wcnt = [0]
            dcnt = [0]

            def modload(slot, ti, part, R):
                smp = (ti == ntp)
                key = "mbuf%d" % slot
                if smp:
                    DMA(mbuf[slot][0:R, :], MODS[l, 1, :, part, :], r=[("MODS", l, 1)], w=[key], dkey=key)
                else:
                    DMA(mbuf[slot][:, :], MODP[l, 1, :, part, :], r=[("MODP", l, 1)], w=[key], dkey=key)
                return key

            for grp in groups:
                ng = len(grp)
                TG = ng * 128
                for gi_, ti in enumerate(grp):
                    smp = (ti == ntp)
                    R = NS if smp else 128
                    rows = slice(ti * 128, ti * 128 + R)
                    DMA(xt[0:R, :], XA[rows, :], r=[("XA", ti)], w=["xt"], dkey="xt")
                    k0 = modload(0, ti, 0, R)
                    k1 = modload(1, ti, 1, R)
                    if smp:
                        V(lambda e: e.memset(hb[:, :], 0.0), r=["hb"], w=["hb"])
                        if l == 1:
                            V(lambda e: e.memset(h2f[:, :], 0.0), r=["h2f"], w=["h2f"])
                    A(lambda e, R=R: e.activation(out=tmp[0:R, :], in_=xt[0:R, :], func=AF.Square, accum_out=ss[0:R, :]), r=["xt"], w=["tmp", "ss"])
                    A(lambda e, R=R: e.activation(out=rstd[0:R, :], in_=ss[0:R, :], func=AF.Ln, scale=1.0 / D, bias=EPS), r=["ss"], w=["rstd"])
                    A(lambda e, R=R: e.activation(out=rstd[0:R, :], in_=rstd[0:R, :], func=AF.Exp, scale=-0.5), r=["rstd"], w=["rstd"])
                    V(lambda e, R=R: e.scalar_tensor_tensor(out=tmp[0:R, :], in0=xt[0:R, :], scalar=rstd[0:R, 0:1], in1=mbuf[0][0:R, :], op0=ALU.mult, op1=ALU.mult),
                      r=["xt", "rstd", k0, "tmp"], w=["tmp"])
                    V(lambda e, R=R: e.tensor_tensor(out=h2f[0:R, :], in0=tmp[0:R, :], in1=mbuf[1][0:R, :], op=ALU.add), r=["tmp", k1], w=["h2f"])
                    A(lambda e, R=R: e.copy(out=hb[0:R, :], in_=h2f[0:R, :]), r=["h2f"], w=["hb"])
                    for k in range(8):
                        T(lambda e, k=k: e.transpose(out=PT[0][:, k * 128:(k + 1) * 128], in_=hb[:, k * 128:(k + 1) * 128], identity=identb[:, :]),
                          r=["hb", "identb"], w=["PT0"])
                    A(lambda e, gi_=gi_: e.copy(out=h2T[:, :, gi_ * 128:(gi_ + 1) * 128], in_=PT[0][:, :].rearrange("p (a b) -> p a b", a=8)),
                      r=["PT0"], w=[("h2T", gi_)])
                    if l == 1:
                        for k in range(8):
                            T(lambda e, k=k: e.transpose(out=PB[2 + k // 4][:, (k % 4) * 128:(k % 4 + 1) * 128], in_=h2f[:, k * 128:(k + 1) * 128], identity=identf[:, :]),
                              r=["h2f", "identf"], w=["PB%d" % (2 + k // 4)])
                        for hh in range(2):
                            V(lambda e, hh=hh: e.tensor_copy(out=h2Tf[:, hh * 4:(hh + 1) * 4, :], in_=PB[2 + hh][:, :].rearrange("p (a b) -> p a b", a=4)),
                              r=["PB%d" % (2 + hh)], w=["h2Tf"])
                        for k in range(8):
                            T(lambda e, k=k: e.matmul(PB[4][:, 0:NE], lhsT=h2Tf[:, k, :], rhs=wr[:, k, :], start=(k == 0), stop=(k == 7)),
                              r=["h2Tf", "wr"], w=["PB4"])
                        V(lambda e: e.tensor_copy(out=lg[:, :], in_=PB[4][:, 0:NE]), r=["PB4"], w=["lg"])
                        V(lambda e: e.reduce_max(out=lmx[:, :], in_=lg[:, :], axis=AX.X), r=["lg"], w=["lmx"])
                        V(lambda e: e.tensor_scalar(out=ee[:, :], in0=lg[:, :], scalar1=lmx[:, 0:1], scalar2=None, op0=ALU.subtract), r=["lg", "lmx"], w=["ee"])
                        A(lambda e: e.activation(out=ee[:, :], in_=ee[:, :], func=AF.Exp), r=["ee"], w=["ee"])
                        V(lambda e: e.tensor_single_scalar(out=mk1[:, :], in_=ee[:, :], scalar=1.0, op=ALU.is_ge), r=["ee"], w=["mk1"])
                        V(lambda e: e.scalar_tensor_tensor(out=e2[:, :], in0=mk1[:, :], scalar=-2.0, in1=ee[:, :], op0=ALU.mult, op1=ALU.add),
                          r=["mk1", "ee"], w=["e2"])
                        V(lambda e: e.reduce_max(out=m2[:, :], in_=e2[:, :], axis=AX.X), r=["e2"], w=["m2"])
                        V(lambda e: e.tensor_scalar(out=e2[:, :], in0=e2[:, :], scalar1=m2[:, 0:1], scalar2=None, op0=ALU.is_ge), r=["e2", "m2"], w=["e2"])
                        V(lambda e: e.tensor_tensor(out=e2[:, :], in0=e2[:, :], in1=mk1[:, :], op=ALU.add), r=["e2", "mk1"], w=["e2"])
                        V(lambda e: e.tensor_tensor(out=e2[:, :], in0=e2[:, :], in1=ee[:, :], op=ALU.mult), r=["e2", "ee"], w=["e2"])
                        V(lambda e: e.tensor_scalar(out=m2[:, :], in0=m2[:, :], scalar1=1.0, scalar2=1e-6, op0=ALU.add, op1=ALU.max), r=["m2", "e2"], w=["m2"])
                        V(lambda e: e.reciprocal(out=m2[:, :], in_=m2[:, :]), r=["m2"], w=["m2"])
                        V(lambda e, gi_=gi_: e.tensor_scalar(out=gts[:, gi_, :], in0=e2[:, :], scalar1=m2[:, 0:1], scalar2=None, op0=ALU.mult),
                          r=["e2", "m2"], w=[("gts", gi_)])
                blocks = [(c0, min(512, TG - c0)) for c0 in range(0, TG, 512)]
                for ex in range(nexp):
                    wg_d = w_eg[ex] if l == 1 else w_fg
                    wu_d = w_eu[ex] if l == 1 else w_fu
                    wd_d = w_ed[ex] if l == 1 else w_fd
                    for half in range(2):
                        for fl_ in range(11):
                            fc = half * 11 + fl_
                            s_ = wcnt[0] % 2
                            wcnt[0] += 1
                            DMA(wgs[s_][:, :, :], wg_d[:, fc * 128:(fc + 1) * 128].rearrange("(k p) c -> p k c", p=128), w=["wgs%d" % s_], dkey="wgs%d" % s_)
                            DMA(wus[s_][:, :, :], wu_d[:, fc * 128:(fc + 1) * 128].rearrange("(k p) c -> p k c", p=128), w=["wus%d" % s_], dkey="wus%d" % s_)
                            G(lambda e, s_=s_: e.tensor_copy(out=wgb[s_][:, :, :], in_=wgs[s_][:, :, :]), r=["wgs%d" % s_], w=["wgb%d" % s_])
                            G(lambda e, s_=s_: e.tensor_copy(out=wub[s_][:, :, :], in_=wus[s_][:, :, :]), r=["wus%d" % s_], w=["wub%d" % s_])
                            d_ = dcnt[0] % 2
                            dcnt[0] += 1
                            DMA(wds[d_][:, :], wd_d[fc * 128:(fc + 1) * 128, :], w=["wds%d" % d_], dkey="wds%d" % d_)
                            A(lambda e, d_=d_, fl_=fl_: e.copy(out=wdb[:, fl_, :], in_=wds[d_][:, :]), r=["wds%d" % d_], w=[("wdb", fl_)])
                            for bi, (c0, cw) in enumerate(blocks):
                                pg = PB[(bi % 2) * 2]
                                pu = PB[(bi % 2) * 2 + 1]
                                pgk = "PB%d" % ((bi % 2) * 2)
                                puk = "PB%d" % ((bi % 2) * 2 + 1)
                                rk = [("h2T", gi_) for gi_ in range(c0 // 128, (c0 + cw) // 128)]
                                for k in range(8):
                                    T(lambda e, k=k, s_=s_, pg=pg, c0=c0, cw=cw: e.matmul(pg[:, 0:cw], lhsT=wgb[s_][:, k, :], rhs=h2T[:, k, c0:c0 + cw], start=(k == 0), stop=(k == 7)),
                                      r=["wgb%d" % s_] + rk, w=[pgk])
                                for k in range(8):
                                    T(lambda e, k=k, s_=s_, pu=pu, c0=c0, cw=cw: e.matmul(pu[:, 0:cw], lhsT=wub[s_][:, k, :], rhs=h2T[:, k, c0:c0 + cw], start=(k == 0), stop=(k == 7)),
                                      r=["wub%d" % s_] + rk, w=[puk])
                                sg = sgb[bi % 2]
                                sgk = "sgb%d" % (bi % 2)
                                A(lambda e, pg=pg, sg=sg, cw=cw: e.activation(out=sg[:, 0:cw], in_=pg[:, 0:cw], func=AF.Silu), r=[pgk], w=[sgk])
                                V(lambda e, pu=pu, sg=sg, cw=cw, fl_=fl_, c0=c0: e.tensor_tensor(out=act[:, fl_, c0:c0 + cw], in0=pu[:, 0:cw], in1=sg[:, 0:cw], op=ALU.mult),
                                  r=[puk, sgk], w=[("act", fl_, bi)])
                        for gi_, ti in enumerate(grp):
                            for dh in range(2):
                                pz = PB[4 + (gi_ * 2 + dh) % 2]
                                pk = "PB%d" % (4 + (gi_ * 2 + dh) % 2)
                                bi = (gi_ * 128) // 512
                                for fl_ in range(11):
                                    T(lambda e, fl_=fl_, gi_=gi_, dh=dh, pz=pz: e.matmul(pz[:, :], lhsT=act[:, fl_, gi_ * 128:(gi_ + 1) * 128], rhs=wdb[:, fl_, dh * 512:(dh + 1) * 512],
                                                                                     start=(fl_ == 0), stop=(fl_ == 10)),
                                      r=[("act", fl_, bi), ("wdb", fl_)], w=[pk])
                                yk = ("yacc", gi_, dh)
                                ysl = yacc[:, gi_, dh * 512:(dh + 1) * 512]
                                first = (ex == 0 and half == 0)
                                if l == 1:
                                    if first:
                                        V(lambda e, pz=pz, ysl=ysl, gi_=gi_, ex=ex: e.tensor_scalar(out=ysl, in0=pz[:, :], scalar1=gts[:, gi_, ex:ex + 1], scalar2=None, op0=ALU.mult),
                                          r=[pk, ("gts", gi_)], w=[yk])
                                    else:
                                        V(lambda e, pz=pz, ysl=ysl, gi_=gi_, ex=ex: e.scalar_tensor_tensor(out=ysl, in0=pz[:, :], scalar=gts[:, gi_, ex:ex + 1], in1=ysl, op0=ALU.mult, op1=ALU.add),
                                          r=[pk, ("gts", gi_), yk], w=[yk])
                                else:
                                    if first:
                                        V(lambda e, pz=pz, ysl=ysl: e.tensor_copy(out=ysl, in_=pz[:, :]), r=[pk], w=[yk])
                                    else:
                                        V(lambda e, pz=pz, ysl=ysl: e.tensor_tensor(out=ysl, in0=pz[:, :], in1=ysl, op=ALU.add), r=[pk, yk], w=[yk])
                for gi_, ti in enumerate(grp):
                    smp = (ti == ntp)
                    R = NS if smp else 128
                    rows = slice(ti * 128, ti * 128 + R)
                    DMA(xt[0:R, :], XA[rows, :], r=[("XA", ti)], w=["xt"], dkey="xt")
                    k0 = modload(0, ti, 2, R)
                    yks = [("yacc", gi_, 0), ("yacc", gi_, 1)]
                    V(lambda e, gi_=gi_, R=R: e.tensor_tensor(out=yacc[0:R, gi_, :], in0=yacc[0:R, gi_, :], in1=mbuf[0][0:R, :], op=ALU.mult), r=yks + [k0], w=yks)
                    V(lambda e, gi_=gi_, R=R: e.tensor_tensor(out=yacc[0:R, gi_, :], in0=yacc[0:R, gi_, :], in1=xt[0:R, :], op=ALU.add), r=yks + ["xt"], w=yks)
                    if l == 0:
                        DMA(XB[rows, :], yacc[0:R, gi_, :], r=yks, w=[("XB", ti)], dkey=("yacc", gi_))
                    else:
                        A(lambda e, gi_=gi_, R=R: e.activation(out=tmp[0:R, :], in_=yacc[0:R, gi_, :], func=AF.Square, accum_out=ss[0:R, :]), r=yks, w=["tmp", "ss"])
                        A(lambda e, R=R: e.activation(out=rstd[0:R, :], in_=ss[0:R, :], func=AF.Ln, scale=1.0 / D, bias=EPS), r=["ss"], w=["rstd"])
                        A(lambda e, R=R: e.activation(out=rstd[0:R, :], in_=rstd[0:R, :], func=AF.Exp, scale=-0.5), r=["rstd"], w=["rstd"])
                        V(lambda e, gi_=gi_, R=R: e.scalar_tensor_tensor(out=yacc[0:R, gi_, :], in0=yacc[0:R, gi_, :], scalar=rstd[0:R, 0:1], in1=fnb[0:R, :], op0=ALU.mult, op1=ALU.mult),
                          r=yks + ["rstd", "fnb"], w=yks)
                        if smp:
                            DMA(ys, yacc[0:R, gi_, :], r=yks, dkey=("yacc", gi_), o=True)
                        else:
                            DMA(yp[rows, :], yacc[0:R, gi_, :], r=yks, dkey=("yacc", gi_), o=True)

        for l in range(2):
            if phases is None or ("M%d" % l) in phases:
                phase_M(l)
            if phases is None or ("F%d" % l) in phases:
                phase_F(l)
        S.finish()
        S.emit(st)
        nc._nops = len(S.ops)
        nc._nsem = S.nsem
    return nc


_OUT_ORDER = ("y_prompt", "y_sample", "fox_k_p", "fox_v_p", "fox_lf_p", "mlstm_C_p", "mlstm_n_p", "mlstm_m_p", "pool_buf_p",
              "fox_k_s", "fox_v_s", "fox_lf_s", "mlstm_C_s", "mlstm_n_s", "mlstm_m_s", "pool_buf_s")


def make_in_maps(inp, ntp=32, ncores=NCORES):
    f = lambda a: np.ascontiguousarray(np.asarray(a))
    maps = []
    gb = f(np.concatenate([inp["b_fox_f"], inp["b_mlstm_i"], inp["b_mlstm_f"]], axis=1))
    for c in range(ncores):
        b = c % 4
        m = {
            "xp": f(inp["x_prompt"][b, :ntp * 128]),
            "xs": f(inp["x_sample"][:, 0, :]),
            "call": f(np.concatenate([inp["c_sample"], inp["c_prompt"][b:b + 1]], axis=0)),
            "w_ada": f(inp["w_ada"]), "b_ada": f(inp["b_ada"]),
            "norm1_w": f(inp["norm1_w"]), "norm2_w": f(inp["norm2_w"]),
            "w_in": f(inp["w_in"]), "gbias": gb, "mnorm_w": f(inp["mlstm_norm_w"]),
            "w_pool": f(inp["w_pool"]), "pool_scale": f(inp["pool_scale"]), "w_out": f(inp["w_out"]),
            "w_fg": f(inp["w_ffn_gate"][0]), "w_fu": f(inp["w_ffn_up"][0]), "w_fd": f(inp["w_ffn_down"][0]),
            "w_router": f(inp["w_router"][0]), "w_eg": f(inp["w_exp_gate"][0]), "w_eu": f(inp["w_exp_up"][0]),
            "w_ed": f(inp["w_exp_down"][0]), "fnorm_w": f(inp["final_norm_w"][None, :]),
            "kpool": f(np.asarray(inp["cache_fox_k"]).reshape(2, -1, 384)),
            "vpool": f(np.asarray(inp["cache_fox_v"]).reshape(2, -1, 384)),
            "lfc": f(np.asarray(inp["cache_fox_lf"]).reshape(2 * NPOOL, 768)),
            "ptab": f(np.asarray(inp["page_table"]).reshape(NS * NPG, 1).astype(np.int32)),
            "gbase": np.arange(LPG, dtype=np.float32)[None, :],
            "stC": f(inp["state_mlstm_C"]), "stn": f(inp["state_mlstm_n"]), "stm": f(inp["state_mlstm_m"]),
            "stp": f(inp["state_pool"]),
        }
        maps.append(m)
    return maps


def kernel(**inputs):
    import os
    nc = build(ntp=32, use_cc=False, nvs=NPOOL // LPG)
    maps = make_in_maps(inputs)
    res = run_bass_kernel_spmd(nc, maps, core_ids=list(range(NCORES)))
    r = res.results
    B = 4
    out = {}
    out["y_prompt"] = np.stack([r[b]["yp"] for b in range(B)])
    out["y_sample"] = r[0]["ys"][:, None, :]
    out["fox_k_p"] = np.stack([r[b]["fkp"] for b in range(B)], axis=1).reshape(2, B, 4096, NH, HD)
    out["fox_v_p"] = np.stack([r[b]["fvp"] for b in range(B)], axis=1).reshape(2, B, 4096, NH, HD)
    out["fox_lf_p"] = np.stack([r[b]["flp"] for b in range(B)], axis=1)
    out["mlstm_C_p"] = np.stack([r[b]["mCp"] for b in range(B)], axis=1)
    out["mlstm_n_p"] = np.stack([r[b]["mnp"] for b in range(B)], axis=1)
    out["mlstm_m_p"] = np.stack([r[b]["mmp"] for b in range(B)], axis=1)
    out["pool_buf_p"] = np.stack([r[b]["pbp"] for b in range(B)], axis=1)
    out["fox_k_s"] = r[0]["fks"].reshape(2, NS, 1, NH, HD)
    out["fox_v_s"] = r[0]["fvs"].reshape(2, NS, 1, NH, HD)
    out["fox_lf_s"] = r[0]["fls"].reshape(2, NS, 1, NH)
    out["mlstm_C_s"] = r[0]["mCs"]
    out["mlstm_n_s"] = r[0]["mns"]
    out["mlstm_m_s"] = r[0]["mms"]
    out["pool_buf_s"] = r[0]["pbs"]
    return tuple(np.ascontiguousarray(out[k], dtype=np.float32) for k in _OUT_ORDER)
```

```python
import bisect
from contextlib import ExitStack
import numpy as np
import concourse.bass as bass
import concourse.mybir as mybir
from concourse.bass_utils import run_bass_kernel_spmd

F32 = mybir.dt.float32
BF16 = mybir.dt.bfloat16
I32 = mybir.dt.int32
AF = mybir.ActivationFunctionType
ALU = mybir.AluOpType
AX = mybir.AxisListType

EPOCH = 30000
D = 1024
NH = 6
HD = 64
INW = 2962
DFF = 2816
NE = 8
EPS = 1e-6
NS = 32
NPG = 64
NPOOL = 2560
NCORES = 8
LPG = NPOOL // NCORES
CHUNKS = [("qf", 0, 384), ("kf", 384, 768), ("vf", 768, 1152), ("qm", 1152, 1536),
          ("km", 1536, 1920), ("vm", 1920, 2304), ("om", 2304, 2688), ("ug", 2688, 2962)]
POOL_W = (2, 4, 8, 16)
DBG = {"mstop": None, "tiles": None}


class _Rec:
    def __getattr__(self, name):
        def f(*a, **k):
            self.call = (name, a, k)
            return self
        return f


class Sched:
    ENGS = ("pe", "act", "dve", "pool", "sp")

    def __init__(self, nc):
        self.nc = nc
        self.ops = []
        self.last_w = {}
        self.readers = {}
        self.dkey_ops = {}
        self.out_dmas = []
        self.bar_start = 0

    def op(self, eng, fn, reads=(), writes=(), dma=False, dkey=None, out=False):
        idx = len(self.ops)
        deps = set()
        writes = list(writes) + [r for r in reads if isinstance(r, str) and r[:2] in ("PB", "PT")]
        reads = [r for r in reads if not (isinstance(r, str) and r[:2] in ("PB", "PT"))]
        for r in reads:
            w = self.last_w.get(r)
            if w is not None:
                deps.add(w)
        for w in writes:
            lw = self.last_w.get(w)
            if lw is not None:
                deps.add(lw)
            for rd in self.readers.get(w, ()):
                deps.add(rd)
        rec = _Rec()
        fn(rec)
        name_, a_, k_ = rec.call
        fn = (lambda e, name_=name_, a_=a_, k_=k_: getattr(e, name_)(*a_, **k_))
        self.ops.append(dict(eng=eng, fn=fn, deps=deps, dma=dma, dkey=dkey, sig=False))
        for r in reads:
            self.readers.setdefault(r, []).append(idx)
        for w in writes:
            self.last_w[w] = idx
            self.readers[w] = []
        if dma:
            assert dkey is not None
            self.dkey_ops.setdefault(dkey, []).append(idx)
            if out:
                self.out_dmas.append(idx)
        return idx

    def barrier(self):
        n = len(self.ops)
        deps = set()
        last = {}
        for i in range(self.bar_start, n):
            o = self.ops[i]
            if o["dma"]:
                deps.add(i)
            elif o["fn"] is not None:
                last[o["eng"]] = i
        deps.update(last.values())
        for e in self.ENGS:
            self.ops.append(dict(eng=e, fn=None, deps=set(deps), dma=False, dkey=None, sig=False))
        self.bar_start = len(self.ops)
        self.last_w = {}
        self.readers = {}

    def finish(self):
        self.ops.append(dict(eng="sp", fn=None, deps=set(self.out_dmas), dma=False, dkey=None, sig=False))

    def emit(self, stack):
        nc = self.nc
        ops = self.ops
        for i, o in enumerate(ops):
            for d in o["deps"]:
                od = ops[d]
                if od["dma"] or od["fn"] is None:
                    continue
                if od["eng"] == "pe" and o["eng"] == "pe" and not o["dma"]:
                    continue
                od["sig"] = True
        cnt = {e: 0 for e in self.ENGS}
        sems = {}

        def get_sem(name):
            if name not in sems:
                sems[name] = stack.enter_context(nc.semaphore(name))
            return sems[name]

        for o in ops:
            if o["dma"] or not o["sig"]:
                continue
            e = o["eng"]
            c = cnt[e]
            o["sem"] = get_sem("c_%s_%d" % (e, c // EPOCH))
            o["val"] = c % EPOCH + 1
            cnt[e] = c + 1
        for k, lst in self.dkey_ops.items():
            s = get_sem("d%d" % len(sems))
            for j, i in enumerate(lst):
                ops[i]["sem"] = s
                ops[i]["val"] = 16 * (j + 1)
        self.nsem = len(sems)
        per_eng = {e: [] for e in self.ENGS}
        for i, o in enumerate(ops):
            per_eng[o["eng"]].append(i)

        def collect(i, acc, depth=0):
            o = ops[i]
            for d in o["deps"]:
                od = ops[d]
                if od["fn"] is None:
                    if od["eng"] == o["eng"]:
                        continue
                    collect(d, acc, depth + 1)
                    continue
                if od["dma"]:
                    lst = self.dkey_ops[od["dkey"]]
                    n = bisect.bisect_left(lst, i)
                    s, v = od["sem"], 16 * n
                else:
                    if od["eng"] == "pe" and o["eng"] == "pe" and not o["dma"]:
                        continue
                    s, v = od["sem"], od["val"]
                key = id(s)
                if key not in acc or acc[key][1] < v:
                    acc[key] = (s, v)

        def run_engine(ename, eng):
            known = {}
            for i in per_eng[ename]:
                o = ops[i]
                acc = {}
                collect(i, acc)
                for s, v in acc.values():
                    if known.get(id(s), 0) >= v:
                        continue
                    eng.wait_ge(s, v)
                    known[id(s)] = v
                if o["fn"] is None:
                    continue
                ins = o["fn"](eng)
                if o["dma"]:
                    ins.then_inc(o["sem"], 16)
                elif o["sig"]:
                    ins.then_inc(o["sem"], 1)

        block = stack.enter_context(nc.Block())

        @block.tensor
        def _(e):
            run_engine("pe", e)

        @block.scalar
        def _(e):
            run_engine("act", e)

        @block.vector
        def _(e):
            run_engine("dve", e)

        @block.gpsimd
        def _(e):
            run_engine("pool", e)

        @block.sync
        def _(e):
            run_engine("sp", e)


class Arena:
    def __init__(self, base, width):
        self.base = base
        self.W = width
        self.off = 0

    def reset(self):
        self.off = 0

    def alloc(self, shape, dt=F32):
        p = shape[0]
        n = int(np.prod(shape[1:]))
        words = n if dt in (F32, I32) else (n + 1) // 2
        if words > 32:
            words = (words + 63) // 64 * 64
            self.off = (self.off + 63) // 64 * 64
        else:
            words = (words + 15) // 16 * 16
        v = self.base[0:p, self.off:self.off + words]
        self.off += words
        assert self.off <= self.W, ("arena overflow", self.off, self.W)
        if dt != F32:
            v = v.bitcast(dt)
        v = v[:, 0:n]
        if len(shape) == 3:
            v = v.rearrange("p (a b) -> p a b", a=shape[1])
        elif len(shape) == 4:
            v = v.rearrange("p (a b c) -> p a b c", a=shape[1], b=shape[2])
        return v


def build(ntp=32, with_samples=True, arena_kb=180, phases=None, lpg=LPG, use_cc=False, ncores=NCORES, nvs=1):
    nc = bass.Bass("TRN2", target_bir_lowering=False)
    NTOK = ntp * 128
    NT = ntp + 1

    def din(name, shape, dt=F32):
        return nc.dram_tensor(name, list(shape), dt, kind="ExternalInput").ap()

    def dout(name, shape, dt=F32):
        return nc.dram_tensor(name, list(shape), dt, kind="ExternalOutput").ap()

    def dscr(name, shape, dt=F32):
        return nc.dram_tensor(name, list(shape), dt, kind="Internal").ap()

    xp = din("xp", [NTOK, D])
    xs = din("xs", [NS, D])
    call = din("call", [NS + 1, D])
    w_ada = din("w_ada", [2, D, 6 * D])
    b_ada = din("b_ada", [2, 6 * D])
    norm1_w = din("norm1_w", [2, D])
    norm2_w = din("norm2_w", [2, D])
    w_in = din("w_in", [2, D, INW])
    gbias = din("gbias", [2, 18])
    mnorm_w = din("mnorm_w", [2, 384])
    w_pool = din("w_pool", [2, 4, 64, 64])
    pool_scale = din("pool_scale", [2, 256])
    w_out = din("w_out", [2, D, D])
    w_fg = din("w_fg", [D, DFF])
    w_fu = din("w_fu", [D, DFF])
    w_fd = din("w_fd", [DFF, D])
    w_router = din("w_router", [D, NE])
    w_eg = din("w_eg", [NE, D, DFF])
    w_eu = din("w_eu", [NE, D, DFF])
    w_ed = din("w_ed", [NE, DFF, D])
    fnorm_w = din("fnorm_w", [1, D])
    kpool = din("kpool", [2, nvs * lpg * 128, 384])
    vpool = din("vpool", [2, nvs * lpg * 128, 384])
    lfc = din("lfc", [2 * NPOOL, 768])
    ptab = din("ptab", [NS * NPG, 1], I32)
    gbase = din("gbase", [1, lpg])
    stC = din("stC", [2, NS, NH, HD, HD])
    stn = din("stn", [2, NS, NH, HD])
    stm = din("stm", [2, NS, NH])
    stp = din("stp", [2, NS, 15, 256])

    yp = dout("yp", [NTOK, D])
    ys = dout("ys", [NS, D])
    fkp = dout("fkp", [2, NTOK, 384])
    fvp = dout("fvp", [2, NTOK, 384])
    flp = dout("flp", [2, NTOK, NH])
    mCp = dout("mCp", [2, NH, HD, HD])
    mnp = dout("mnp", [2, NH, HD])
    mmp = dout("mmp", [2, NH])
    pbp = dout("pbp", [2, 15, 256])
    fks = dout("fks", [2, NS, 384])
    fvs = dout("fvs", [2, NS, 384])
    fls = dout("fls", [2, NS, NH])
    mCs = dout("mCs", [2, NS, NH, HD, HD])
    mns = dout("mns", [2, NS, NH, HD])
    mms = dout("mms", [2, NS, NH])
    pbs = dout("pbs", [2, NS, 15, 256])
    ARin = dscr("ARin", [2, 2 * 96, 385])
    ARout = dscr("ARout", [2, 2 * 96, 385])

    XA = dscr("XA", [NT * 128, D])
    XB = dscr("XB", [NT * 128, D])
    WINB = dscr("WINB", [2, D, INW], BF16)
    WOUTB = dscr("WOUTB", [2, D, D], BF16)
    MODP = dscr("MODP", [2, 2, 128, 3, D])
    MODS = dscr("MODS", [2, 2, NS, 3, D])

    st = ExitStack()
    with st:
        S = Sched(nc)
        AW = arena_kb * 256
        arena_t = st.enter_context(nc.sbuf_tensor("arena", [128, AW], F32))
        AR = Arena(arena_t, AW)

        def sb(name, shape, dt=F32):
            return st.enter_context(nc.sbuf_tensor(name, shape, dt))

        PB = [st.enter_context(nc.psum_tensor("pb%d" % i, [128, 512], F32)) for i in range(6)]
        PT = [st.enter_context(nc.psum_tensor("pt%d" % i, [128, 1024], BF16)) for i in range(2)]

        def V(fn, r=(), w=()):
            S.op("dve", fn, reads=r, writes=w)

        def A(fn, r=(), w=()):
            S.op("act", fn, reads=r, writes=w)

        def G(fn, r=(), w=()):
            S.op("pool", fn, reads=r, writes=w)

        def T(fn, r=(), w=()):
            S.op("pe", fn, reads=r, writes=w)

        def DMA(out, in_, r=(), w=(), dkey=None, eng="sp", o=False):
            S.op(eng, lambda e: e.dma_start(out=out, in_=in_), reads=r, writes=w, dma=True, dkey=dkey, out=o)

        identf = sb("identf", [128, 128])
        identb = sb("identb", [128, 128], BF16)
        trif = sb("trif", [128, 128])
        trib = sb("trib", [128, 128], BF16)
        maskb = sb("maskb", [128, 128], BF16)
        onesf = sb("onesf", [128, 128])
        onesb = sb("onesb", [128, 128], BF16)
        sel32 = sb("sel32", [NS + 1, 128])
        bandc = sb("bandc", [128, 4, 128], BF16)
        bandp = sb("bandp", [128, 4, 128], BF16)
        band0 = sb("band0", [128, 4, 128], BF16)
        ctmp = sb("ctmp", [128, 128])
        ctmp2 = sb("ctmp2", [128, 128])
        ioti = sb("ioti", [128, 128], I32)
        iotf = sb("iotf", [128, 128])

        G(lambda e: e.memset(identf[:], 1.0), w=["identf"])
        G(lambda e: e.affine_select(out=identf[:], in_=identf[:], pattern=[[-1, 128]], compare_op=ALU.is_equal,
                                    fill=0.0, base=0, channel_multiplier=1), r=["identf"], w=["identf"])
        V(lambda e: e.tensor_copy(out=identb[:], in_=identf[:]), r=["identf"], w=["identb"])
        G(lambda e: e.memset(trif[:], 1.0), w=["trif"])
        G(lambda e: e.affine_select(out=trif[:], in_=trif[:], pattern=[[1, 128]], compare_op=ALU.is_ge,
                                    fill=0.0, base=0, channel_multiplier=-1), r=["trif"], w=["trif"])
        V(lambda e: e.tensor_copy(out=trib[:], in_=trif[:]), r=["trif"], w=["trib"])
        G(lambda e: e.memset(ctmp[:], 0.0), w=["ctmp"])
        G(lambda e: e.affine_select(out=ctmp[:], in_=ctmp[:], pattern=[[1, 128]], compare_op=ALU.is_ge,
                                    fill=-1e30, base=0, channel_multiplier=-1), r=["ctmp"], w=["ctmp"])
        V(lambda e: e.tensor_copy(out=maskb[:], in_=ctmp[:]), r=["ctmp"], w=["maskb"])
        G(lambda e: e.memset(onesf[:], 1.0), w=["onesf"])
        G(lambda e: e.memset(onesb[:], 1.0), w=["onesb"])
        G(lambda e: e.memset(sel32[:], 0.0), w=["sel32"])
        G(lambda e: e.memset(sel32[32:33, :], 1.0), r=["sel32"], w=["sel32"])
        G(lambda e: e.iota(ioti[:], pattern=[[1, 128]], base=1, channel_multiplier=0), w=["ioti"])
        V(lambda e: e.tensor_copy(out=iotf[:], in_=ioti[:]), r=["ioti"], w=["iotf"])
        for g, wd_ in enumerate(POOL_W):
            G(lambda e: e.memset(ctmp[:], 1.0), r=["maskb"], w=["ctmp"])
            G(lambda e: e.affine_select(out=ctmp[:], in_=ctmp[:], pattern=[[1, 128]], compare_op=ALU.is_ge,
                                        fill=0.0, base=0, channel_multiplier=-1), r=["ctmp"], w=["ctmp"])
            G(lambda e, wd_=wd_: e.affine_select(out=ctmp[:], in_=ctmp[:], pattern=[[-1, 128]], compare_op=ALU.is_ge,
                                                 fill=0.0, base=wd_ - 1, channel_multiplier=1), r=["ctmp"], w=["ctmp"])
            V(lambda e, wd_=wd_: e.scalar_tensor_tensor(out=ctmp2[:], in0=ctmp[:], scalar=1.0 / wd_, in1=identf[:],
                                                       op0=ALU.mult, op1=ALU.subtract), r=["ctmp", "identf"], w=["ctmp2"])
            V(lambda e, g=g: e.tensor_copy(out=bandc[:, g, :], in_=ctmp2[:]), r=["ctmp2"], w=["bandc"])
            V(lambda e, wd_=wd_: e.tensor_scalar(out=ctmp2[:], in0=iotf[:], scalar1=float(wd_), scalar2=None, op0=ALU.min),
              r=["iotf", "bandc"], w=["ctmp2"])
            V(lambda e: e.reciprocal(out=ctmp2[:], in_=ctmp2[:]), r=["ctmp2"], w=["ctmp2"])
            V(lambda e: e.tensor_tensor(out=ctmp2[:], in0=ctmp2[:], in1=ctmp[:], op=ALU.mult), r=["ctmp2", "ctmp"], w=["ctmp2"])
            V(lambda e: e.tensor_tensor(out=ctmp2[:], in0=ctmp2[:], in1=identf[:], op=ALU.subtract), r=["ctmp2"], w=["ctmp2"])
            V(lambda e, g=g: e.tensor_copy(out=band0[:, g, :], in_=ctmp2[:]), r=["ctmp2"], w=["band0"])
            G(lambda e, wd_=wd_: e.memset(ctmp[:], 1.0 / wd_), r=["ctmp2", "band0"], w=["ctmp"])
            G(lambda e, wd_=wd_: e.affine_select(out=ctmp[:], in_=ctmp[:], pattern=[[-1, 128]], compare_op=ALU.is_ge,
                                                 fill=0.0, base=wd_ - 129, channel_multiplier=1), r=["ctmp"], w=["ctmp"])
            V(lambda e, g=g: e.tensor_copy(out=bandp[:, g, :], in_=ctmp[:]), r=["ctmp"], w=["bandp"])

        S.barrier()
        AR.reset()
        stg = [AR.alloc([128, INW]) for _ in range(2)]
        stb = [AR.alloc([128, INW], BF16) for _ in range(2)]
        it = 0
        for l in (range(2) if (phases is None or "W" in phases) else []):
            for k in range(8):
                s_ = it % 2
                DMA(stg[s_][:, :], w_in[l, k * 128:(k + 1) * 128, :], w=["stg%d" % s_], dkey="stg%d" % s_)
                G(lambda e, s_=s_: e.tensor_copy(out=stb[s_][:, :], in_=stg[s_][:, :]), r=["stg%d" % s_], w=["stb%d" % s_])
                DMA(WINB[l, k * 128:(k + 1) * 128, :], stb[s_][:, :], r=["stb%d" % s_], w=[("WINB", l)], dkey="stb%d" % s_)
                it += 1
            for k in range(8):
                s_ = it % 2
                DMA(stg[s_][:, 0:D], w_out[l, k * 128:(k + 1) * 128, :], w=["stg%d" % s_], dkey="stg%d" % s_)
                G(lambda e, s_=s_: e.tensor_copy(out=stb[s_][:, 0:D], in_=stg[s_][:, 0:D]), r=["stg%d" % s_], w=["stb%d" % s_])
                DMA(WOUTB[l, k * 128:(k + 1) * 128, :], stb[s_][:, 0:D], r=["stb%d" % s_], w=[("WOUTB", l)], dkey="stb%d" % s_)
                it += 1

        S.barrier()
        AR.reset()
        NR = NS + 1
        cin = AR.alloc([NR, D])
        ce = AR.alloc([NR, D])
        sB = AR.alloc([NR, D], BF16)
        sT = AR.alloc([128, 8, NR], BF16)
        modall = AR.alloc([NR, 6 * D])
        wst = [AR.alloc([128, 8, 512]) for _ in range(2)]
        wbf = [AR.alloc([128, 8, 512], BF16) for _ in range(2)]
        bst = [AR.alloc([1, 512]) for _ in range(2)]
        nwb = AR.alloc([128, D])
        modP = AR.alloc([128, 3, D])
        modS = AR.alloc([NS, 3, D])
        DMA(cin[:, :], call, w=["cin"], dkey="cin")
        A(lambda e: e.activation(out=ce[:, :], in_=cin[:, :], func=AF.Exp, scale=-1.0), r=["cin"], w=["ce"])
        V(lambda e: e.tensor_scalar_add(out=ce[:, :], in0=ce[:, :], scalar1=1.0), r=["ce"], w=["ce"])
        V(lambda e: e.reciprocal(out=ce[:, :], in_=ce[:, :]), r=["ce"], w=["ce"])
        V(lambda e: e.tensor_tensor(out=sB[:, :], in0=cin[:, :], in1=ce[:, :], op=ALU.mult), r=["ce", "cin"], w=["sB"])
        for k in range(8):
            T(lambda e, k=k: e.transpose(out=PT[0][:, k * 64:k * 64 + NR], in_=sB[:, k * 128:(k + 1) * 128],
                                         identity=identb[0:NR, 0:NR]), r=["sB", "identb"], w=["PT0"])
        A(lambda e: e.copy(out=sT[:, :, :], in_=PT[0][:, 0:512].rearrange("p (a b) -> p a b", a=8)[:, :, 0:NR]), r=["PT0"], w=["sT"])
        it = 0
        for l in (range(2) if (phases is None or "ADA" in phases) else []):
            for j in range(12):
                s_ = it % 2
                DMA(wst[s_][:, :, :], w_ada[l, :, j * 512:(j + 1) * 512].rearrange("(k p) c -> p k c", p=128),
                    w=["wst%d" % s_], dkey="wst%d" % s_)
                DMA(bst[s_][:, :], b_ada[l:l + 1, j * 512:(j + 1) * 512], w=["bst%d" % s_], dkey="bst%d" % s_)
                G(lambda e, s_=s_: e.tensor_copy(out=wbf[s_][:, :, :], in_=wst[s_][:, :, :]), r=["wst%d" % s_], w=["wbf%d" % s_])
                pz = PB[it % 2]
                for k in range(8):
                    T(lambda e, k=k, s_=s_, pz=pz: e.matmul(pz[0:NR, :], lhsT=sT[:, k, :], rhs=wbf[s_][:, k, :],
                                                             start=(k == 0), stop=False),
                      r=["sT", "wbf%d" % s_], w=["PB%d" % (it % 2)])
                T(lambda e, s_=s_, pz=pz: e.matmul(pz[0:NR, :], lhsT=onesf[0:1, 0:NR], rhs=bst[s_][:, :], start=False, stop=True),
                  r=["onesf", "bst%d" % s_], w=["PB%d" % (it % 2)])
                A(lambda e, j=j, pz=pz: e.copy(out=modall[:, j * 512:(j + 1) * 512], in_=pz[0:NR, :]),
                  r=["PB%d" % (it % 2)], w=["modall"])
                it += 1
            for ph in range(2):
                nw = norm1_w if ph == 0 else norm2_w
                DMA(nwb[:, :], nw[l:l + 1, :].partition_broadcast(128), w=["nwb"], dkey="nwb")
                base = ph * 3 * D
                for part, (src, dst) in enumerate([(1, 0), (0, 1), (2, 2)]):
                    for hf in range(2):
                        c0 = base + src * D + hf * 512
                        pz = PB[2 + (part * 2 + hf) % 2]
                        pk = "PB%d" % (2 + (part * 2 + hf) % 2)
                        T(lambda e, c0=c0, pz=pz: e.matmul(pz[:, :], lhsT=sel32[:, :], rhs=modall[:, c0:c0 + 512], start=True, stop=True),
                          r=["sel32", "modall"], w=[pk])
                        if src == 1:
                            V(lambda e, pz=pz, dst=dst, hf=hf: e.scalar_tensor_tensor(
                                out=modP[:, dst, hf * 512:(hf + 1) * 512], in0=pz[:, :], scalar=1.0,
                                in1=nwb[:, hf * 512:(hf + 1) * 512], op0=ALU.add, op1=ALU.mult), r=[pk, "nwb"], w=["modP"])
                        else:
                            A(lambda e, pz=pz, dst=dst, hf=hf: e.copy(out=modP[:, dst, hf * 512:(hf + 1) * 512], in_=pz[:, :]),
                              r=[pk], w=["modP"])
                    if src == 1:
                        V(lambda e, base=base, dst=dst: e.scalar_tensor_tensor(
                            out=modS[:, dst, :], in0=modall[0:NS, base + D:base + 2 * D], scalar=1.0,
                            in1=nwb[0:NS, :], op0=ALU.add, op1=ALU.mult), r=["modall", "nwb"], w=["modS"])
                    else:
                        V(lambda e, base=base, src=src, dst=dst: e.tensor_copy(
                            out=modS[:, dst, :], in_=modall[0:NS, base + src * D:base + (src + 1) * D]), r=["modall"], w=["modS"])
                DMA(MODP[l, ph], modP[:, :, :], r=["modP"], w=[("MODP", l, ph)], dkey="modP")
                DMA(MODS[l, ph], modS[:, :, :], r=["modS"], w=[("MODS", l, ph)], dkey="modS")

        def phase_M(l):
            S.barrier()
            AR.reset()
            wch = [AR.alloc([128, 8, 384], BF16) for _ in range(2)]
            woutb = AR.alloc([128, 8, D], BF16)
            kt_w = (NH * NTOK // 2 + 63) // 64 * 64
            va_w = (ntp * NH * 66 // 2 + 63) // 64 * 64
            SAMP_W = 76 * 256
            blk = AR.alloc([128, max(kt_w + va_w, SAMP_W if with_samples else 0)])
            KT = blk[0:72, 0:NH * NTOK // 2].bitcast(BF16).rearrange("p (a b) -> p a b", a=NH)
            Vaug = blk[:, kt_w:kt_w + ntp * NH * 66 // 2].bitcast(BF16).rearrange("p (a b c) -> p a b c", a=ntp, b=NH)
            SA = Arena(blk, max(kt_w + va_w, SAMP_W))
            qs32 = AR.alloc([NS, 384])
            qms32 = AR.alloc([NS, 384])
            vms32 = AR.alloc([NS, 384])
            modp = AR.alloc([128, 3, D])
            xt = [AR.alloc([128, D]) for _ in range(2)]
            tmp = AR.alloc([128, D])
            hb = AR.alloc([128, D], BF16)
            hT = AR.alloc([128, 8, 128], BF16)
            zk = [AR.alloc([128, 384]) for _ in range(2)]
            zv = [AR.alloc([128, 384]) for _ in range(2)]
            qaug = AR.alloc([128, NH, 72], BF16)
            kaug = AR.alloc([128, NH, 72], BF16)
            QT = AR.alloc([72, NH, 128], BF16)
            qmb = AR.alloc([128, 384], BF16)
            kmf = AR.alloc([128, 384])
            vmaug = AR.alloc([128, NH, 66], BF16)
            gate = AR.alloc([128, 384])
            uf = AR.alloc([128, 256])
            ubf = [AR.alloc([128, 256], BF16) for _ in range(2)]
            gpre = AR.alloc([128, 18])
            lfn = AR.alloc([128, 18])
            lfcat = [AR.alloc([128, 12]) for _ in range(2)]
            gbb = AR.alloc([128, 18])
            carryB = AR.alloc([128, NH])
            Ff = AR.alloc([128, NH])
            r1 = AR.alloc([128, NH])
            Fabc = AR.alloc([128, NH, 3])
            negF = AR.alloc([128, NH, 3])
            tmpb = AR.alloc([128, NH], BF16)
            bm = AR.alloc([128, NH])
            Bb = AR.alloc([128, NH])
            ptb = [AR.alloc([128, 512], BF16) for _ in range(2)]
            mix = AR.alloc([128, D], BF16)
            mixT = AR.alloc([128, 8, 128], BF16)
            rden = AR.alloc([128, NH])
            ss = AR.alloc([128, 1])
            rstd = AR.alloc([128, 1])
            um = AR.alloc([128, NH])
            umax = AR.alloc([NH, 1])
            d6 = AR.alloc([NH, NH])
            mb = AR.alloc([128, NH])
            Mb = AR.alloc([128, NH])
            ab = AR.alloc([128, NH])
            wk = AR.alloc([128, NH])
            flr = AR.alloc([128, NH])
            kpb = AR.alloc([128, NH, HD], BF16)
            kpT = AR.alloc([64, NH, 128], BF16)
            qmT = AR.alloc([64, NH, 128], BF16)
            AT = AR.alloc([128, NH, 128], BF16)
            Cst = AR.alloc([64, NH, 65])
            Cbf = AR.alloc([64, NH, 66], BF16)
            hm = AR.alloc([128, NH, HD])
            sq = AR.alloc([128, NH, HD])
            hss = AR.alloc([128, NH])
            mnw = AR.alloc([128, 384])
            dT = AR.alloc([64, 4, 128], BF16)
            wpf = AR.alloc([64, 4, 64])
            wpad = AR.alloc([64, 4, 128], BF16)
            psc = AR.alloc([128, 2])
            x2o = xt

            DMA(woutb[:, :, :], WOUTB[l].rearrange("(k p) c -> p k c", p=128), r=[("WOUTB", l)], w=["woutb"], dkey="woutb")
            DMA(modp[:, :, :], MODP[l, 0], r=[("MODP", l, 0)], w=["modp"], dkey="modp")
            DMA(gbb[:, :], gbias[l:l + 1, :].partition_broadcast(128), w=["gbb"], dkey="gbb")
            DMA(mnw[:, :], mnorm_w[l:l + 1, :].partition_broadcast(128), w=["mnw"], dkey="mnw")
            DMA(wpf[:, :, :], w_pool[l].rearrange("g c e -> c g e"), w=["wpf"], dkey="wpf")
            S.op("sp", lambda e: e.dma_start(out=psc[:, :], in_=pool_scale[l].rearrange("(j p) -> p j", p=128),
                                             allow_slow_non_contiguous=True), writes=["psc"], dma=True, dkey="psc")
            G(lambda e: e.memset(wpad[:, :, :], 0.0), w=["wpad"])
            for g in range(4):
                o_ = (g % 2) * 64
                V(lambda e, g=g, o_=o_: e.tensor_copy(out=wpad[:, g, o_:o_ + 64], in_=wpf[:, g, :]), r=["wpf", "wpad"], w=["wpad"])
            G(lambda e: e.memset(qaug[:, :, :], 0.0), w=["qaug"])
            G(lambda e: e.memset(kaug[:, :, :], 0.0), w=["kaug"])
            G(lambda e: e.memset(qaug[:, :, 68:71], 1.0), r=["qaug"], w=["qaug"])
            G(lambda e: e.memset(kaug[:, :, 64:67], 1.0), r=["kaug"], w=["kaug"])
            G(lambda e: e.memset(Vaug[:, :, :, :], 1.0), w=["VaugAll"])
            G(lambda e: e.memset(vmaug[:, :, :], 1.0), w=["vmaug"])
            G(lambda e: e.memset(carryB[:, :], 0.0), w=["carryB"])
            G(lambda e: e.memset(mb[:, :], 0.0), w=["mb"])
            G(lambda e: e.memset(Cst[:, :, :], 0.0), w=["Cst"])
            G(lambda e: e.memset(ubf[1][:, :], 0.0), w=["ubf1"])
            G(lambda e: e.memset(mix[:, :], 0.0), w=["mix"])
            G(lambda e: e.memset(mixT[:, :, :], 0.0), w=["mixT"])

            for t in range(NT):
                if DBG["tiles"] is not None and t >= DBG["tiles"]:
                    continue
                smp = (t == ntp)
                if smp and not with_samples:
                    continue
                R = NS if smp else 128
                sl = t % 2
                xk = "x%d" % sl
                if smp:
                    S.barrier()
                x = xt[sl]
                rows = slice(t * 128, t * 128 + R)
                if smp:
                    DMA(modp[0:NS, :, :], MODS[l, 0], r=[("MODS", l, 0)], w=["modp"], dkey="modp")
                if l == 0:
                    src = xs if smp else xp[rows, :]
                else:
                    src = XB[rows, :]
                DMA(x[0:R, :], src, r=[("XB", t)] if l else [], w=[xk], dkey=xk)
                A(lambda e, x=x, R=R: e.activation(out=tmp[0:R, :], in_=x[0:R, :], func=AF.Square, accum_out=ss[0:R, :]),
                  r=[xk], w=["tmp", "ss"])
                if DBG.get("astop") == 2:
                    continue
                A(lambda e, R=R: e.activation(out=rstd[0:R, :], in_=ss[0:R, :], func=AF.Ln, scale=1.0 / D, bias=EPS), r=["ss"], w=["rstd"])
                if DBG.get("astop") == 3:
                    continue
                A(lambda e, R=R: e.activation(out=rstd[0:R, :], in_=rstd[0:R, :], func=AF.Exp, scale=-0.5), r=["rstd"], w=["rstd"])
                if DBG.get("astop") == 4:
                    continue
                V(lambda e, x=x, R=R: e.scalar_tensor_tensor(out=tmp[0:R, :], in0=x[0:R, :], scalar=rstd[0:R, 0:1],
                                                             in1=modp[0:R, 0, :], op0=ALU.mult, op1=ALU.mult),
                  r=[xk, "rstd", "modp", "tmp"], w=["tmp"])
                if DBG.get("astop") == 5:
                    continue
                if DBG.get("v6") == 1:
                    V(lambda e, R=R: e.tensor_copy(out=hb[0:R, :], in_=tmp[0:R, :]), r=["tmp", "modp"], w=["hb"])
                elif DBG.get("v6") == 2:
                    V(lambda e, R=R: e.tensor_tensor(out=tmp[0:R, :], in0=tmp[0:R, :], in1=modp[0:R, 1, :], op=ALU.add), r=["tmp", "modp"], w=["tmp"])
                elif DBG.get("v6") == 3:
                    G(lambda e, R=R: e.tensor_tensor(out=hb[0:R, :], in0=tmp[0:R, :], in1=modp[0:R, 1, :], op=ALU.add), r=["tmp", "modp"], w=["hb"])
                else:
                    V(lambda e, R=R: e.tensor_tensor(out=hb[0:R, :], in0=tmp[0:R, :], in1=modp[0:R, 1, :], op=ALU.add),
                      r=["tmp", "modp"], w=["hb"])
                if DBG["mstop"] == "A":
                    continue
                for k in range(8):
                    T(lambda e, k=k, R=R: e.transpose(out=PT[0][:, k * 128:k * 128 + R], in_=hb[0:R, k * 128:(k + 1) * 128],
                                                      identity=identb[0:R, 0:R]), r=["hb", "identb"], w=["PT0"])
                A(lambda e, R=R: e.copy(out=hT[:, :, 0:R], in_=PT[0][:, :].rearrange("p (a b) -> p a b", a=8)[:, :, 0:R]),
                  r=["PT0"], w=["hT"])
                if DBG["mstop"] == "B":
                    continue
                for ci, (cn, c0, c1) in enumerate(CHUNKS):
                    if DBG.get("cmax") is not None and ci >= DBG["cmax"]:
                        continue
                    wd_ = c1 - c0
                    ws = ci % 2
                    wkey = "wch%d" % ws
                    DMA(wch[ws][:, :, 0:wd_], WINB[l, :, c0:c1].rearrange("(k p) c -> p k c", p=128),
                        r=[("WINB", l)], w=[wkey], dkey=wkey)
                    pz = PB[ci % 2]
                    pk = "PB%d" % (ci % 2)
                    for k in range(8):
                        T(lambda e, k=k, ws=ws, wd_=wd_, pz=pz, R=R: e.matmul(pz[0:R, 0:wd_], lhsT=hT[:, k, 0:R], rhs=wch[ws][:, k, 0:wd_],
                                                                            start=(k == 0), stop=(k == 7)),
                          r=["hT", wkey], w=[pk])
                    pz3 = pz[0:R, 0:384].rearrange("p (h d) -> p h d", h=NH)
                    if DBG.get("cevac") == 0:
                        continue
                    if cn == "qf":
                        A(lambda e, pz3=pz3, R=R: e.mul(out=qaug[0:R, :, 0:64], in_=pz3, mul=0.125), r=[pk], w=["qaug"])
                        if smp:
                            A(lambda e, pz=pz: e.mul(out=qs32[:, :], in_=pz[0:NS, 0:384], mul=0.125), r=[pk], w=["qs32"])
                    elif cn == "kf":
                        km_ = DBG.get("kf", 7)
                        if km_ & 1:
                            V(lambda e, pz=pz, R=R, sl=sl: e.tensor_copy(out=zk[sl][0:R, :], in_=pz[0:R, 0:384]), r=[pk], w=["zk%d" % sl])
                        if km_ & 2:
                            A(lambda e, pz3=pz3, R=R: e.copy(out=kaug[0:R, :, 0:64], in_=pz3), r=[pk], w=["kaug"])
                        if km_ & 4:
                            if smp:
                                DMA(fks[l], zk[sl][0:R, :], r=["zk%d" % sl], dkey="zk%d" % sl, o=True)
                            else:
                                DMA(fkp[l, rows, :], zk[sl][0:R, :], r=["zk%d" % sl], dkey="zk%d" % sl, o=True)
                    elif cn == "vf":
                        V(lambda e, pz=pz, R=R, sl=sl: e.tensor_copy(out=zv[sl][0:R, :], in_=pz[0:R, 0:384]), r=[pk], w=["zv%d" % sl])
                        if smp:
                            DMA(fvs[l], zv[sl][0:R, :], r=["zv%d" % sl], dkey="zv%d" % sl, o=True)
                        else:
                            A(lambda e, pz3=pz3, t=t: e.copy(out=Vaug[:, t, :, 0:64], in_=pz3), r=[pk, "VaugAll"], w=[("Vaug", t)])
                            DMA(fvp[l, rows, :], zv[sl][0:R, :], r=["zv%d" % sl], dkey="zv%d" % sl, o=True)
                    elif cn == "qm":
                        A(lambda e, pz=pz, R=R: e.copy(out=qmb[0:R, :], in_=pz[0:R, 0:384]), r=[pk], w=["qmb"])
                        if smp:
                            V(lambda e, pz=pz: e.tensor_copy(out=qms32[:, :], in_=pz[0:NS, 0:384]), r=[pk], w=["qms32"])
                    elif cn == "km":
                        V(lambda e, pz=pz, R=R: e.tensor_copy(out=kmf[0:R, :], in_=pz[0:R, 0:384]), r=[pk], w=["kmf"])
                    elif cn == "vm":
                        A(lambda e, pz3=pz3, R=R: e.copy(out=vmaug[0:R, :, 0:64], in_=pz3), r=[pk], w=["vmaug"])
                        if smp:
                            V(lambda e, pz=pz: e.tensor_copy(out=vms32[:, :], in_=pz[0:NS, 0:384]), r=[pk], w=["vms32"])
                    elif cn == "om":
                        A(lambda e, pz=pz, R=R: e.activation(out=gate[0:R, :], in_=pz[0:R, 0:384], func=AF.Exp, scale=-1.0), r=[pk], w=["gate"])
                        V(lambda e, R=R: e.tensor_scalar_add(out=gate[0:R, :], in0=gate[0:R, :], scalar1=1.0), r=["gate"], w=["gate"])
                        V(lambda e, R=R: e.reciprocal(out=gate[0:R, :], in_=gate[0:R, :]), r=["gate"], w=["gate"])
                    else:
                        V(lambda e, pz=pz, R=R: e.tensor_copy(out=uf[0:R, :], in_=pz[0:R, 0:256]), r=[pk], w=["uf"])
                        A(lambda e, pz=pz, R=R, sl=sl: e.copy(out=ubf[sl][0:R, :], in_=pz[0:R, 0:256]), r=[pk], w=["ubf%d" % sl])
                        V(lambda e, pz=pz, R=R: e.tensor_tensor(out=gpre[0:R, :], in0=pz[0:R, 256:274], in1=gbb[0:R, :], op=ALU.add),
                          r=[pk, "gbb"], w=["gpre"])
                if DBG["mstop"] == "C":
                    continue
                lc = lfcat[sl]
                lck = "lfcat%d" % sl
                A(lambda e, R=R: e.activation(out=lfn[0:R, :], in_=gpre[0:R, :], func=AF.Exp, scale=-1.0), r=["gpre"], w=["lfn"])
                A(lambda e, R=R: e.activation(out=lfn[0:R, :], in_=lfn[0:R, :], func=AF.Ln, bias=1.0), r=["lfn"], w=["lfn"])
                V(lambda e, R=R, lc=lc: e.tensor_scalar(out=lc[0:R, 0:6], in0=lfn[0:R, 0:6], scalar1=-1.0, scalar2=None, op0=ALU.mult),
                  r=["lfn"], w=[lck])
                V(lambda e, R=R, lc=lc: e.tensor_scalar(out=lc[0:R, 6:12], in0=lfn[0:R, 12:18], scalar1=-1.0, scalar2=None, op0=ALU.mult),
                  r=["lfn", lck], w=[lck])
                if smp:
                    S.op("sp", lambda e, lc=lc: e.dma_start(out=fls[l], in_=lc[0:NS, 0:6]), reads=[lck], writes=[("fls", l)], dma=True, dkey=lck, out=True)
                    sample_mixers(l, locals())
                else:
                    S.op("sp", lambda e, lc=lc, rows=rows: e.dma_start(out=flp[l, rows, :], in_=lc[:, 0:6]), reads=[lck], dma=True, dkey=lck, out=True)
                    T(lambda e, lc=lc: e.matmul(PB[5][:, 0:12], lhsT=trif[:, :], rhs=lc[:, :], start=True, stop=True), r=["trif", lck], w=["PB5"])
                    T(lambda e, lc=lc: e.matmul(PB[5][:, 16:28], lhsT=onesf[:, :], rhs=lc[:, :], start=True, stop=True), r=["onesf", lck], w=["PB5"])
                    V(lambda e: e.tensor_tensor(out=Ff[:, :], in0=PB[5][:, 0:6], in1=carryB[:, :], op=ALU.add), r=["PB5", "carryB"], w=["Ff"])
                    V(lambda e: e.tensor_tensor(out=carryB[:, :], in0=PB[5][:, 16:22], in1=carryB[:, :], op=ALU.add), r=["PB5", "carryB"], w=["carryB"])
                    V(lambda e: e.tensor_copy(out=bm[:, :], in_=PB[5][:, 6:12]), r=["PB5"], w=["bm"])
                    V(lambda e: e.tensor_copy(out=Bb[:, :], in_=PB[5][:, 22:28]), r=["PB5"], w=["Bb"])
                    for i3 in range(3):
                        src3 = Ff if i3 == 0 else r1
                        V(lambda e, src3=src3: e.tensor_copy(out=tmpb[:, :], in_=src3[:, :]), r=["Ff", "r1"], w=["tmpb"])
                        V(lambda e, i3=i3: e.tensor_copy(out=Fabc[:, :, i3], in_=tmpb[:, :]), r=["tmpb"], w=["Fabc"])
                        if i3 < 2:
                            V(lambda e, i3=i3, src3=src3: e.tensor_tensor(out=r1[:, :], in0=src3[:, :], in1=Fabc[:, :, i3], op=ALU.subtract), r=["Ff", "r1", "Fabc"], w=["r1"])
                    V(lambda e: e.tensor_scalar(out=negF[:, :, :], in0=Fabc[:, :, :], scalar1=-1.0, scalar2=None, op0=ALU.mult), r=["Fabc"], w=["negF"])
                    V(lambda e: e.tensor_copy(out=qaug[:, :, 64:67], in_=Fabc[:, :, :]), r=["Fabc", "qaug"], w=["qaug"])
                    V(lambda e: e.tensor_copy(out=kaug[:, :, 68:71], in_=negF[:, :, :]), r=["negF", "kaug"], w=["kaug"])
                    if DBG["mstop"] == "D":
                        continue
                    for h in range(NH):
                        T(lambda e, h=h: e.transpose(out=PT[1][0:72, h * 128:(h + 1) * 128], in_=kaug[:, h, :], identity=identb[:, :]),
                          r=["kaug", "identb"], w=["PT1"])
                    A(lambda e, t=t: e.copy(out=KT[:, :, t * 128:(t + 1) * 128], in_=PT[1][0:72, 0:768].rearrange("p (a b) -> p a b", a=NH)),
                      r=["PT1"], w=[("KT", t)])
                    for h in range(NH):
                        T(lambda e, h=h: e.transpose(out=PT[1][0:72, h * 128:(h + 1) * 128], in_=qaug[:, h, :], identity=identb[:, :]),
                          r=["qaug", "identb"], w=["PT1"])
                    V(lambda e: e.tensor_copy(out=QT[:, :, :], in_=PT[1][0:72, 0:768].rearrange("p (a b) -> p a b", a=NH)), r=["PT1"], w=["QT"])
                    if DBG["mstop"] == "E":
                        continue
                    gi = 0
                    for h in range(NH):
                        for j0 in range(0, t + 1, 4):
                            js = list(range(j0, min(j0 + 4, t + 1)))
                            pb_ = PB[2 + gi % 2]
                            pk = "PB%d" % (2 + gi % 2)
                            pt_ = ptb[gi % 2]
                            ptk = "ptb%d" % (gi % 2)
                            for i, j in enumerate(js):
                                T(lambda e, i=i, j=j, h=h, pb_=pb_: e.matmul(pb_[:, i * 128:(i + 1) * 128], lhsT=KT[:, h, j * 128:(j + 1) * 128],
                                                                            rhs=QT[:, h, :], start=True, stop=(j != t)),
                                  r=[("KT", j), "QT"], w=[pk])
                                if j == t:
                                    T(lambda e, i=i, pb_=pb_: e.matmul(pb_[:, i * 128:(i + 1) * 128], lhsT=identb[:, :], rhs=maskb[:, :],
                                                                      start=False, stop=True), r=["identb", "maskb"], w=[pk])
                            n = len(js) * 128
                            A(lambda e, pb_=pb_, pt_=pt_, n=n: e.activation(out=pt_[:, 0:n], in_=pb_[:, 0:n], func=AF.Exp), r=[pk], w=[ptk])
                            for i, j in enumerate(js):
                                T(lambda e, i=i, j=j, h=h, pt_=pt_: e.matmul(PB[4][:, h * 65:(h + 1) * 65], lhsT=pt_[:, i * 128:(i + 1) * 128],
                                                                            rhs=Vaug[:, j, h, 0:65], start=(j == 0), stop=(j == t)),
                                  r=[ptk, ("Vaug", j)], w=["PB4"])
                            gi += 1
                    po3 = PB[4][:, 0:390].rearrange("p (h d) -> p h d", h=NH)
                    V(lambda e, po3=po3: e.reciprocal(out=rden[:, :], in_=po3[:, :, 64]), r=["PB4"], w=["rden"])
                    V(lambda e, po3=po3: e.tensor_tensor(out=mix[:, 0:384].rearrange("p (h d) -> p h d", h=NH), in0=po3[:, :, 0:64],
                                                         in1=rden[:, :].unsqueeze(2).to_broadcast([128, NH, HD]), op=ALU.mult),
                      r=["PB4", "rden"], w=["mix"])
                    if DBG["mstop"] == "F":
                        continue
                    V(lambda e: e.tensor_tensor(out=um[:, :], in0=gpre[:, 6:12], in1=bm[:, :], op=ALU.subtract), r=["gpre", "bm"], w=["um"])
                    T(lambda e: e.transpose(out=PB[5][0:NH, 0:128], in_=um[:, :], identity=identf[:, :]), r=["um", "identf"], w=["PB5"])
                    V(lambda e: e.reduce_max(out=umax[:, :], in_=PB[5][0:NH, 0:128], axis=AX.X), r=["PB5"], w=["umax"])
                    V(lambda e: e.tensor_scalar(out=d6[:, :], in0=identf[0:NH, 0:NH], scalar1=umax[:, 0:1], scalar2=None, op0=ALU.mult),
                      r=["umax", "identf"], w=["d6"])
                    T(lambda e: e.matmul(PB[5][:, 128:134], lhsT=onesf[0:NH, :], rhs=d6[:, :], start=True, stop=True), r=["d6", "onesf"], w=["PB5"])
                    V(lambda e: e.tensor_tensor(out=Mb[:, :], in0=PB[5][:, 128:134], in1=mb[:, :], op=ALU.max), r=["PB5", "mb"], w=["Mb"])
                    V(lambda e: e.tensor_tensor(out=ab[:, :], in0=mb[:, :], in1=Mb[:, :], op=ALU.subtract), r=["mb", "Mb"], w=["ab"])
                    A(lambda e: e.activation(out=ab[:, :], in_=ab[:, :], func=AF.Exp), r=["ab"], w=["ab"])
                    V(lambda e: e.tensor_tensor(out=wk[:, :], in0=um[:, :], in1=Mb[:, :], op=ALU.subtract), r=["um", "Mb"], w=["wk"])
                    A(lambda e: e.activation(out=wk[:, :], in_=wk[:, :], func=AF.Exp), r=["wk"], w=["wk"])
                    V(lambda e: e.tensor_tensor(out=mb[:, :], in0=Bb[:, :], in1=Mb[:, :], op=ALU.add), r=["Bb", "Mb", "mb", "ab"], w=["mb"])
                    V(lambda e: e.tensor_tensor(out=flr[:, :], in0=bm[:, :], in1=Mb[:, :], op=ALU.add), r=["bm", "Mb"], w=["flr"])
                    A(lambda e: e.activation(out=flr[:, :], in_=flr[:, :], func=AF.Exp, scale=-1.0), r=["flr"], w=["flr"])
                    V(lambda e: e.scalar_tensor_tensor(out=kpb[:, :, :], in0=kmf[:, :].rearrange("p (h d) -> p h d", h=NH), scalar=0.125,
                                                       in1=wk[:, :].unsqueeze(2).to_broadcast([128, NH, HD]), op0=ALU.mult, op1=ALU.mult),
                      r=["kmf", "wk"], w=["kpb"])
                    for h in range(NH):
                        T(lambda e, h=h: e.transpose(out=PT[1][0:64, h * 128:(h + 1) * 128], in_=kpb[:, h, :], identity=identb[:, :]),
                          r=["kpb", "identb"], w=["PT1"])
                    A(lambda e: e.copy(out=kpT[:, :, :], in_=PT[1][0:64, 0:768].rearrange("p (a b) -> p a b", a=NH)), r=["PT1"], w=["kpT"])
                    for h in range(NH):
                        T(lambda e, h=h: e.transpose(out=PT[0][0:64, h * 128:(h + 1) * 128], in_=qmb[:, h * 64:(h + 1) * 64], identity=identb[:, :]),
                          r=["qmb", "identb"], w=["PT0"])
                    V(lambda e: e.tensor_copy(out=qmT[:, :, :], in_=PT[0][0:64, 0:768].rearrange("p (a b) -> p a b", a=NH)), r=["PT0"], w=["qmT"])
                    for h in range(NH):
                        pb_ = PB[2 + h // 3]
                        T(lambda e, h=h, pb_=pb_: e.matmul(pb_[:, (h % 3) * 128:(h % 3 + 1) * 128], lhsT=kpT[:, h, :], rhs=qmT[:, h, :], start=True, stop=True),
                          r=["kpT", "qmT"], w=["PB%d" % (2 + h // 3)])
                    for half in range(2):
                        V(lambda e, half=half: e.tensor_tensor(out=AT[:, half * 3:(half + 1) * 3, :],
                                                               in0=PB[2 + half][:, 0:384].rearrange("p (a b) -> p a b", a=3),
                                                               in1=trif[:, :].unsqueeze(1).to_broadcast([128, 3, 128]), op=ALU.mult),
                          r=["PB%d" % (2 + half), "trif"], w=["AT"])
                    V(lambda e: e.tensor_tensor(out=Cst[:, :, :], in0=Cst[:, :, :], in1=ab[0:64, :].unsqueeze(2).to_broadcast([64, NH, 65]), op=ALU.mult),
                      r=["Cst", "ab"], w=["Cst"])
                    V(lambda e: e.tensor_copy(out=Cbf[:, :, 0:65], in_=Cst[:, :, :]), r=["Cst"], w=["Cbf"])
                    for h in range(NH):
                        T(lambda e, h=h: e.matmul(PB[5][:, h * 65:(h + 1) * 65], lhsT=AT[:, h, :], rhs=vmaug[:, h, 0:65], start=True, stop=False),
                          r=["AT", "vmaug"], w=["PB5"])
                        T(lambda e, h=h: e.matmul(PB[5][:, h * 65:(h + 1) * 65], lhsT=qmT[:, h, :], rhs=Cbf[:, h, 0:65], start=False, stop=True),
                          r=["qmT", "Cbf"], w=["PB5"])
                    pn3 = PB[5][:, 0:390].rearrange("p (h d) -> p h d", h=NH)
                    V(lambda e, pn3=pn3: e.tensor_copy(out=hss[:, :], in_=pn3[:, :, 64]), r=["PB5", "mix"], w=["hss"])
                    V(lambda e: e.scalar_tensor_tensor(out=rden[:, :], in0=hss[:, :], scalar=-1.0, in1=hss[:, :], op0=ALU.mult, op1=ALU.max), r=["hss"], w=["rden"])
                    V(lambda e: e.tensor_tensor(out=rden[:, :], in0=rden[:, :], in1=flr[:, :], op=ALU.max), r=["rden", "flr"], w=["rden"])
                    V(lambda e: e.reciprocal(out=rden[:, :], in_=rden[:, :]), r=["rden"], w=["rden"])
                    V(lambda e, pn3=pn3: e.tensor_tensor(out=hm[:, :, :], in0=pn3[:, :, 0:64], in1=rden[:, :].unsqueeze(2).to_broadcast([128, NH, HD]), op=ALU.mult),
                      r=["PB5", "rden"], w=["hm"])
                    for h in range(NH):
                        T(lambda e, h=h: e.matmul(PB[4][0:64, h * 65:(h + 1) * 65], lhsT=kpb[:, h, :], rhs=vmaug[:, h, 0:65], start=True, stop=True),
                          r=["kpb", "vmaug", "mix"], w=["PB4"])
                    V(lambda e: e.tensor_tensor(out=Cst[:, :, :], in0=Cst[:, :, :], in1=PB[4][0:64, 0:390].rearrange("p (h d) -> p h d", h=NH), op=ALU.add),
                      r=["Cst", "PB4", "Cbf"], w=["Cst"])
                    mlstm_finish(locals(), 128)
                    if DBG["mstop"] == "G":
                        continue
                    bcur = band0 if t == 0 else bandc
                    for g in range(4):
                        T(lambda e, g=g, sl=sl, bcur=bcur: e.matmul(PB[4][0:64, g * 128:(g + 1) * 128], lhsT=ubf[sl][:, g * 64:(g + 1) * 64], rhs=bcur[:, g, :],
                                                                  start=True, stop=False), r=["ubf%d" % sl, "bandc", "band0"], w=["PB4"])
                        T(lambda e, g=g, sl=sl: e.matmul(PB[4][0:64, g * 128:(g + 1) * 128], lhsT=ubf[1 - sl][:, g * 64:(g + 1) * 64], rhs=bandp[:, g, :],
                                                         start=False, stop=True), r=["ubf%d" % (1 - sl), "bandp"], w=["PB4"])
                    A(lambda e: e.copy(out=dT[:, :, :], in_=PB[4][0:64, :].rearrange("p (a b) -> p a b", a=4)), r=["PB4"], w=["dT"])
                    for c in range(2):
                        T(lambda e, c=c: e.matmul(PB[5][:, c * 128:(c + 1) * 128], lhsT=wpad[:, 2 * c, :], rhs=dT[:, 2 * c, :], start=True, stop=False),
                          r=["wpad", "dT", "hm"], w=["PB5"])
                        T(lambda e, c=c: e.matmul(PB[5][:, c * 128:(c + 1) * 128], lhsT=wpad[:, 2 * c + 1, :], rhs=dT[:, 2 * c + 1, :], start=False, stop=True),
                          r=["wpad", "dT"], w=["PB5"])
                        V(lambda e, c=c: e.tensor_scalar(out=mixT[:, 6 + c, :], in0=PB[5][:, c * 128:(c + 1) * 128], scalar1=psc[:, c:c + 1], scalar2=None, op0=ALU.mult),
                          r=["PB5", "psc"], w=["mixT"])
                    if t == ntp - 1:
                        DMA(pbp[l], uf[113:128, :], r=["uf"], dkey="uf", o=True)
                if DBG["mstop"] == "H":
                    continue
                for k in range(6):
                    T(lambda e, k=k, R=R: e.transpose(out=PT[0][:, k * 128:k * 128 + R], in_=mix[0:R, k * 128:(k + 1) * 128], identity=identb[0:R, 0:R]),
                      r=["mix", "identb"], w=["PT0"])
                A(lambda e, R=R: e.copy(out=mixT[:, 0:6, 0:R], in_=PT[0][:, 0:768].rearrange("p (a b) -> p a b", a=6)[:, :, 0:R]), r=["PT0"], w=["mixT"])
                if DBG["mstop"] == "I":
                    continue
                for hf in range(2):
                    pz = PB[hf]
                    pk = "PB%d" % hf
                    for k in range(8):
                        T(lambda e, k=k, hf=hf, pz=pz, R=R: e.matmul(pz[0:R, :], lhsT=mixT[:, k, 0:R], rhs=woutb[:, k, hf * 512:(hf + 1) * 512],
                                                                     start=(k == 0), stop=(k == 7)), r=["mixT", "woutb"], w=[pk])
                    V(lambda e, hf=hf, pz=pz, R=R: e.tensor_tensor(out=tmp[0:R, hf * 512:(hf + 1) * 512], in0=pz[0:R, :], in1=modp[0:R, 2, hf * 512:(hf + 1) * 512], op=ALU.mult),
                      r=[pk, "modp", "tmp"], w=["tmp"])
                    V(lambda e, hf=hf, x=x, R=R: e.tensor_tensor(out=x[0:R, hf * 512:(hf + 1) * 512], in0=tmp[0:R, hf * 512:(hf + 1) * 512], in1=x[0:R, hf * 512:(hf + 1) * 512], op=ALU.add),
                      r=["tmp", xk], w=[xk])
                DMA(XA[rows, :], x[0:R, :], r=[xk], w=[("XA", t)], dkey=xk)
            DMA(mCp[l].rearrange("h k v -> k h v"), Cst[:, :, 0:64], r=["Cst"], dkey="Cst", o=True)
            S.op("sp", lambda e: e.dma_start(out=mnp[l].rearrange("h k -> k h"), in_=Cst[:, :, 64], allow_slow_non_contiguous=True),
                 reads=["Cst"], dma=True, dkey="Cst", out=True)
            DMA(mmp[l:l + 1, :], mb[0:1, :], r=["mb"], dkey="mb", o=True)

        def mlstm_finish(L, R):
            hm, sq, hss, gate, mnw, mix = L["hm"], L["sq"], L["hss"], L["gate"], L["mnw"], L["mix"]
            V(lambda e: e.tensor_tensor(out=sq[0:R, :, :], in0=hm[0:R, :, :], in1=hm[0:R, :, :], op=ALU.mult), r=["hm"], w=["sq"])
            V(lambda e: e.reduce_sum(out=hss[0:R, :], in_=sq[0:R, :, :], axis=AX.X), r=["sq"], w=["hss"])
            A(lambda e: e.activation(out=hss[0:R, :], in_=hss[0:R, :], func=AF.Ln, scale=1.0 / HD, bias=EPS), r=["hss"], w=["hss"])
            A(lambda e: e.activation(out=hss[0:R, :], in_=hss[0:R, :], func=AF.Exp, scale=-0.5), r=["hss"], w=["hss"])
            V(lambda e: e.tensor_tensor(out=sq[0:R, :, :], in0=hm[0:R, :, :], in1=hss[0:R, :].unsqueeze(2).to_broadcast([R, NH, HD]), op=ALU.mult),
              r=["hm", "hss", "sq"], w=["sq"])
            V(lambda e: e.tensor_tensor(out=sq[0:R, :, :], in0=sq[0:R, :, :], in1=mnw[0:R, :].rearrange("p (h d) -> p h d", h=NH), op=ALU.mult),
              r=["sq", "mnw"], w=["sq"])
            V(lambda e: e.tensor_tensor(out=mix[0:R, 384:768].rearrange("p (h d) -> p h d", h=NH), in0=sq[0:R, :, :],
                                        in1=gate[0:R, :].rearrange("p (h d) -> p h d", h=NH), op=ALU.mult), r=["sq", "gate", "mix"], w=["mix"])

        def sample_mixers(l, L):
            SA, lc, gpre, sl = L["SA"], L["lc"], L["gpre"], L["sl"]
            zk, zv, kmf, uf, mix, mixT, hm = L["zk"][sl], L["zv"][sl], L["kmf"], L["uf"], L["mix"], L["mixT"], L["hm"]
            qs32, qms32, vms32, wpad, psc = L["qs32"], L["qms32"], L["vms32"], L["wpad"], L["psc"]
            lck = "lfcat%d" % sl
            NCH = NS * NPG // 128
            PTf = [PT[0][:, :].bitcast(F32), PT[1][:, :].bitcast(F32)]
            SA.reset()
            acct = SA.alloc([96, 2, 385])
            base0 = SA.off
            for vs in range(nvs):
                SA.off = base0
                if vs > 0:
                    S.barrier()
                Ownb = SA.alloc([128, NS, lpg], BF16)
                biasL = SA.alloc([128, lpg, NH])
                OwnT = SA.alloc([NS, lpg])
                mark = SA.off
                pti = SA.alloc([128, NCH], I32)
                ptf = SA.alloc([128, NCH])
                gidx = SA.alloc([128, lpg])
                EqF = SA.alloc([128, lpg])
                EqB = SA.alloc([128, lpg], BF16)
                lfg = SA.alloc([128, 768])
                pref = SA.alloc([128, NH, 128])
                Fs = SA.alloc([128, NH, 128])
                tot = SA.alloc([128, NH])
                lat = SA.alloc([128, NH])
                lfnb = SA.alloc([128, NH])
                BT = SA.alloc([128, 128])
                halfsel = SA.alloc([128, 2, 128], BF16)
                IndRow = SA.alloc([128, NCH, NS])
                S.op("sp", lambda e: e.dma_start(out=pti[:, :], in_=ptab.rearrange("(c p) o -> p (c o)", p=128), allow_slow_non_contiguous=True),
                     writes=["pti"], dma=True, dkey="pti")
                V(lambda e: e.tensor_copy(out=ptf[:, :], in_=pti[:, :]), r=["pti"], w=["ptf"])
                DMA(gidx[:, :], gbase.partition_broadcast(128), w=["gidx"], dkey="gidx")
                if vs > 0:
                    V(lambda e: e.tensor_scalar_add(out=gidx[:, :], in0=gidx[:, :], scalar1=float(vs * lpg)), r=["gidx"], w=["gidx"])
                G(lambda e: e.memset(BT[:, :], 1.0), w=["BT"])
                G(lambda e: e.affine_select(out=BT[:, :], in_=BT[:, :], pattern=[[-1, 128]], compare_op=ALU.is_ge, fill=0.0, base=-1, channel_multiplier=1),
                  r=["BT"], w=["BT"])
                G(lambda e: e.memset(BT[64:128, 0:64], 0.0), r=["BT"], w=["BT"])
                G(lambda e: e.memset(halfsel[:, :, :], 0.0), w=["halfsel"])
                G(lambda e: e.memset(halfsel[0:64, 0, :], 1.0), r=["halfsel"], w=["halfsel"])
                G(lambda e: e.memset(halfsel[64:128, 1, :], 1.0), r=["halfsel"], w=["halfsel"])
                G(lambda e: e.memset(IndRow[:, :, :], 1.0), w=["IndRow"])
                for s_ in range(2):
                    G(lambda e, s_=s_: e.affine_select(out=IndRow[s_ * 64:(s_ + 1) * 64, :, :], in_=IndRow[s_ * 64:(s_ + 1) * 64, :, :],
                                                       pattern=[[-2, NCH], [1, NS]], compare_op=ALU.is_equal, fill=0.0, base=-s_, channel_multiplier=0),
                      r=["IndRow"], w=["IndRow"])
                ptl = SA.alloc([128, NCH], I32)
                ptlf = SA.alloc([128, NCH])
                V(lambda e: e.tensor_scalar_add(out=ptlf[:, :], in0=ptf[:, :], scalar1=float(l * NPOOL)), r=["ptf"], w=["ptlf"])
                V(lambda e: e.tensor_copy(out=ptl[:, :], in_=ptlf[:, :]), r=["ptlf"], w=["ptl"])
                for c in range(NCH):
                    V(lambda e, c=c: e.tensor_scalar(out=EqF[:, :], in0=gidx[:, :], scalar1=ptf[:, c:c + 1], scalar2=None, op0=ALU.is_equal),
                      r=["gidx", "ptf"], w=["EqF"])
                    A(lambda e: e.copy(out=EqB[:, :], in_=EqF[:, :]), r=["EqF"], w=["EqB"])
                    for s_ in range(2):
                        T(lambda e, s_=s_: e.matmul(PTf[1][:, 0:lpg], lhsT=halfsel[:, s_, :], rhs=EqB[:, :], start=True, stop=True), r=["halfsel", "EqB"], w=["PT1"])
                        A(lambda e, c=c, s_=s_: e.copy(out=Ownb[:, 2 * c + s_, :], in_=PTf[1][:, 0:lpg]), r=["PT1"], w=["Ownb"])
                    S.op("pool", lambda e, c=c: e.indirect_dma_start(out=lfg[:, :], out_offset=None, in_=lfc,
                                                                     in_offset=bass.IndirectOffsetOnAxis(ap=ptl[:, c:c + 1], axis=0)),
                         reads=["ptl"], writes=["lfg"], dma=True, dkey="lfg")
                    V(lambda e: e.reduce_sum(out=tot[:, :], in_=lfg[:, :].rearrange("p (t h) -> p h t", h=NH), axis=AX.X), r=["lfg"], w=["tot"])
                    for s_ in range(2):
                        b_ = 2 * c + s_
                        DMA(lfnb[s_ * 64:(s_ + 1) * 64, :], fls[l, b_:b_ + 1, :].partition_broadcast(64), r=[("fls", l)], w=["lfnb"], dkey="lfnb")
                    T(lambda e: e.matmul(PTf[0][:, 0:NH], lhsT=BT[:, :], rhs=tot[:, :], start=True, stop=True), r=["BT", "tot"], w=["PT0"])
                    V(lambda e: e.tensor_tensor(out=lat[:, :], in0=PTf[0][:, 0:NH], in1=lfnb[:, :], op=ALU.add), r=["PT0", "lfnb"], w=["lat"])
                    V(lambda e: e.tensor_tensor(out=lat[:, :], in0=lat[:, :], in1=tot[:, :], op=ALU.add), r=["lat", "tot"], w=["lat"])
                    for h in range(NH):
                        V(lambda e, h=h: e.tensor_tensor_scan(out=pref[:, h, :], data0=onesf[:, :], data1=lfg[:, :].rearrange("p (t h) -> p h t", h=NH)[:, h, :],
                                                              initial=0.0, op0=ALU.mult, op1=ALU.add), r=["lfg", "onesf"], w=["pref"])
                    V(lambda e: e.tensor_tensor(out=Fs[:, :, :], in0=lat[:, :].unsqueeze(2).to_broadcast([128, NH, 128]), in1=pref[:, :, :], op=ALU.subtract),
                      r=["lat", "pref"], w=["Fs"])
                    for h in range(NH):
                        T(lambda e, h=h, c=c: e.matmul(PB[h][:, 0:lpg], lhsT=Fs[:, h, :], rhs=EqF[:, :], start=(c == 0), stop=(c == NCH - 1)),
                          r=["Fs", "EqF"], w=["PB%d" % h])
                for h in range(NH):
                    V(lambda e, h=h: e.tensor_copy(out=biasL[:, :, h], in_=PB[h][:, 0:lpg]), r=["PB%d" % h], w=["biasL"])
                for c in range(NCH):
                    V(lambda e, c=c: e.tensor_scalar(out=EqF[:, :], in0=gidx[:, :], scalar1=ptf[:, c:c + 1], scalar2=None, op0=ALU.is_equal),
                      r=["gidx", "ptf", "EqF"], w=["EqF"])
                    T(lambda e, c=c: e.matmul(PB[0][0:NS, 0:lpg], lhsT=IndRow[:, c, :], rhs=EqF[:, :], start=(c == 0), stop=(c == NCH - 1)),
                      r=["IndRow", "EqF"], w=["PB0"])
                V(lambda e: e.tensor_copy(out=OwnT[:, :], in_=PB[0][0:NS, 0:lpg]), r=["PB0"], w=["OwnT"])

                S.barrier()
                SA.off = mark
                PBAT = 4
                Kb = [SA.alloc([128, PBAT, 384]) for _ in range(2)]
                Vb = [SA.alloc([128, PBAT, 384]) for _ in range(2)]
                Vbb = [SA.alloc([128, PBAT, 386], BF16) for _ in range(2)]
                OQ2 = [SA.alloc([NS, PBAT, 384], BF16) for _ in range(2)]
                sc2 = [SA.alloc([128, PBAT * NH]) for _ in range(2)]
                Pm2 = [SA.alloc([128, PBAT * NH]) for _ in range(2)]
                Pexp2 = [SA.alloc([128, PBAT, NS, NH], BF16) for _ in range(2)]
                psets = [[(PB[0], "PB0"), (PB[1], "PB1"), (PB[2], "PB2")], [(PB[5], "PB5"), (PTf[0], "PT0"), (PTf[1], "PT1")]]
                for s_ in range(2):
                    G(lambda e, s_=s_: e.memset(Vbb[s_][:, :, :], 1.0), w=["Vbb%d" % s_])
                nb = DBG.get("nb", lpg // PBAT)
                for bi in range(nb):
                    s_ = bi % 2
                    i0 = bi * PBAT
                    r0 = (vs * lpg + i0) * 128
                    OQ, sc, Pm, Pexp = OQ2[s_], sc2[s_], Pm2[s_], Pexp2[s_]
                    kOQ, ksc, kPm, kPe = "OQ%d" % s_, "sc%d" % s_, "Pm%d" % s_, "Pexp%d" % s_
                    DMA(Kb[s_][:, :, :], kpool[l, r0:r0 + PBAT * 128, :].rearrange("(i t) c -> t i c", t=128), w=["Kb%d" % s_], dkey="Kb%d" % s_)
                    DMA(Vb[s_][:, :, :], vpool[l, r0:r0 + PBAT * 128, :].rearrange("(i t) c -> t i c", t=128), w=["Vb%d" % s_], dkey="Vb%d" % s_)
                    G(lambda e, s_=s_: e.tensor_copy(out=Vbb[s_][:, :, 0:384], in_=Vb[s_][:, :, :]), r=["Vb%d" % s_, "Vbb%d" % s_], w=["Vbb%d" % s_])
                    V(lambda e, i0=i0, OQ=OQ: e.tensor_tensor(out=OQ[:, :, :], in0=qs32[:, :].unsqueeze(1).to_broadcast([NS, PBAT, 384]),
                                                       in1=OwnT[:, i0:i0 + PBAT].unsqueeze(2).to_broadcast([NS, PBAT, 384]), op=ALU.mult),
                      r=["qs32", "OwnT"], w=[kOQ])
                    OQf = OQ[:, :, :].rearrange("p a b -> p (a b)")
                    Kf = Kb[s_][:, :, :].rearrange("p a b -> p (a b)")
                    for j in range(3):
                        pj, pjk = psets[s_][j]
                        T(lambda e, j=j, OQf=OQf, pj=pj: e.matmul(pj[:, 0:512], lhsT=onesb[0:NS, :], rhs=OQf[:, j * 512:(j + 1) * 512], start=True, stop=True),
                          r=["onesb", kOQ], w=[pjk])
                        V(lambda e, j=j, Kf=Kf, pj=pj: e.tensor_tensor(out=Kf[:, j * 512:(j + 1) * 512], in0=pj[:, 0:512], in1=Kf[:, j * 512:(j + 1) * 512], op=ALU.mult),
                          r=[pjk, "Kb%d" % s_], w=["Kb%d" % s_])
                    V(lambda e, Kf=Kf, sc=sc: e.reduce_sum(out=sc[:, :], in_=Kf.rearrange("p (a d) -> p a d", d=HD), axis=AX.X), r=["Kb%d" % s_], w=[ksc])
                    V(lambda e, i0=i0, sc=sc: e.tensor_tensor(out=sc[:, :], in0=sc[:, :], in1=biasL[:, i0:i0 + PBAT, :].rearrange("p i h -> p (i h)"), op=ALU.add),
                      r=[ksc, "biasL"], w=[ksc])
                    A(lambda e, sc=sc, Pm=Pm: e.activation(out=Pm[:, :], in_=sc[:, :], func=AF.Exp), r=[ksc], w=[kPm])
                    V(lambda e, i0=i0, Pm=Pm, Pexp=Pexp: e.tensor_tensor(out=Pexp[:, :, :, :],
                                                       in0=Pm[:, :].rearrange("p (i h) -> p i h", i=PBAT).unsqueeze(2).to_broadcast([128, PBAT, NS, NH]),
                                                       in1=Ownb[:, :, i0:i0 + PBAT].rearrange("p b i -> p i b").unsqueeze(3).to_broadcast([128, PBAT, NS, NH]),
                                                       op=ALU.mult), r=[kPm, "Ownb"], w=[kPe])
                    for i in range(PBAT):
                        first = (bi == 0 and i == 0)
                        last = (bi == nb - 1 and i == PBAT - 1)
                        for hf in range(2):
                            T(lambda e, i=i, hf=hf, s_=s_, first=first, last=last, Pexp=Pexp: e.matmul(
                                PB[3 + hf][0:96, 0:385], lhsT=Pexp[:, i, hf * 16:(hf + 1) * 16, :].rearrange("p b h -> p (b h)"),
                                rhs=Vbb[s_][:, i, 0:385], start=first, stop=last), r=[kPe, "Vbb%d" % s_], w=["PB%d" % (3 + hf)])
                for hf in range(2):
                    if vs == 0:
                        V(lambda e, hf=hf: e.tensor_copy(out=acct[:, hf, :], in_=PB[3 + hf][0:96, 0:385]), r=["PB%d" % (3 + hf)], w=["acct"])
                    else:
                        V(lambda e, hf=hf: e.tensor_tensor(out=acct[:, hf, :], in0=PB[3 + hf][0:96, 0:385], in1=acct[:, hf, :], op=ALU.add),
                          r=["PB%d" % (3 + hf), "acct"], w=["acct"])
            if DBG.get("sstop", 99) <= 2:
                return
            for hf in range(2):
                DMA(ARin[l, hf * 96:(hf + 1) * 96, :], acct[:, hf, :], r=["acct"], w=[("ARin", l)], dkey="acct")
            if use_cc:
                S.op("pool", lambda e: e.collective_compute("AllReduce", ALU.add, replica_groups=[list(range(ncores))],
                                                            ins=[ARin[l]], outs=[ARout[l]]),
                     reads=[("ARin", l)], writes=[("ARout", l)], dma=True, dkey="cc")
            else:
                DMA(ARout[l], ARin[l], r=[("ARin", l)], w=[("ARout", l)], dkey="cc")
            S.barrier()
            SA.off = mark
            numS = SA.alloc([NS, NH, HD])
            denS = SA.alloc([NS, NH])
            base_off = l * 192 * 385
            num_src = bass.AP(ARout.tensor, base_off, [[NH * 385, NS], [385 + HD, NH], [1, HD]])
            den_src = bass.AP(ARout.tensor, base_off + 384, [[NH * 385, NS], [385, NH], [1, 1]])
            DMA(numS[:, :, :], num_src, r=[("ARout", l)], w=["numS"], dkey="numS")
            S.op("sp", lambda e: e.dma_start(out=denS[:, :].unsqueeze(2), in_=den_src, allow_slow_non_contiguous=True),
                 reads=[("ARout", l)], writes=["denS"], dma=True, dkey="denS")
            prn = SA.alloc([NS, 384])
            sn = SA.alloc([NS, NH])
            tn3 = SA.alloc([NS, NH, HD])
            V(lambda e: e.tensor_tensor(out=prn[:, :], in0=qs32[:, :], in1=zk[0:NS, :], op=ALU.mult), r=["qs32", "zk%d" % sl], w=["prn"])
            V(lambda e: e.reduce_sum(out=sn[:, :], in_=prn[:, :].rearrange("p (h d) -> p h d", h=NH), axis=AX.X), r=["prn"], w=["sn"])
            A(lambda e: e.activation(out=sn[:, :], in_=sn[:, :], func=AF.Exp), r=["sn"], w=["sn"])
            V(lambda e: e.tensor_tensor(out=tn3[:, :, :], in0=zv[0:NS, :].rearrange("p (h d) -> p h d", h=NH),
                                        in1=sn[:, :].unsqueeze(2).to_broadcast([NS, NH, HD]), op=ALU.mult), r=["zv%d" % sl, "sn"], w=["tn3"])
            V(lambda e: e.tensor_tensor(out=numS[:, :, :], in0=numS[:, :, :], in1=tn3[:, :, :], op=ALU.add), r=["numS", "tn3"], w=["numS"])
            V(lambda e: e.tensor_tensor(out=denS[:, :], in0=denS[:, :], in1=sn[:, :], op=ALU.add), r=["denS", "sn"], w=["denS"])
            V(lambda e: e.reciprocal(out=denS[:, :], in_=denS[:, :]), r=["denS"], w=["denS"])
            V(lambda e: e.tensor_tensor(out=tn3[:, :, :], in0=numS[:, :, :], in1=denS[:, :].unsqueeze(2).to_broadcast([NS, NH, HD]), op=ALU.mult),
              r=["numS", "denS", "tn3"], w=["tn3"])
            A(lambda e: e.copy(out=mix[0:NS, 0:384], in_=tn3[:, :, :].rearrange("p h d -> p (h d)")), r=["tn3", "mix"], w=["mix"])

            if DBG.get("sstop", 99) <= 3:
                return
            S.barrier()
            SA.reset()
            Ch = [SA.alloc([64, NS, 65]) for _ in range(2)]
            qmask = SA.alloc([64, NS, NS])
            kmaskH = SA.alloc([NS, NS, HD])
            E64 = SA.alloc([64, NS, NS])
            abc = SA.alloc([64, NS * NH])
            qmTs = SA.alloc([64, NH, NS])
            m0 = SA.alloc([NS, NH])
            inter = SA.alloc([NS, NH])
            mt = SA.alloc([NS, NH])
            a_ = SA.alloc([NS, NH])
            wg_ = SA.alloc([NS, NH])
            flr_ = SA.alloc([NS, NH])
            qk = SA.alloc([NS, NH])
            den = SA.alloc([NS, NH])
            kw = SA.alloc([NS, NH, HD])
            vaugS = SA.alloc([NS, NH, 65])
            Adiag = SA.alloc([NS, NS, NH])
            qC = SA.alloc([NS, NH, 65])
            t2 = SA.alloc([NS, NH, HD])
            prq = SA.alloc([NS, 384])
            DMA(m0[:, :], stm[l], w=["m0"], dkey="m0")
            V(lambda e: e.tensor_tensor(out=inter[:, :], in0=lc[0:NS, 6:12], in1=m0[:, :], op=ALU.add), r=[lck, "m0"], w=["inter"])
            V(lambda e: e.tensor_tensor(out=mt[:, :], in0=inter[:, :], in1=gpre[0:NS, 6:12], op=ALU.max), r=["inter", "gpre"], w=["mt"])
            DMA(mms[l], mt[:, :], r=["mt"], dkey="mt", o=True)
            V(lambda e: e.tensor_tensor(out=a_[:, :], in0=inter[:, :], in1=mt[:, :], op=ALU.subtract), r=["inter", "mt"], w=["a_"])
            A(lambda e: e.activation(out=a_[:, :], in_=a_[:, :], func=AF.Exp), r=["a_"], w=["a_"])
            V(lambda e: e.tensor_tensor(out=wg_[:, :], in0=gpre[0:NS, 6:12], in1=mt[:, :], op=ALU.subtract), r=["gpre", "mt"], w=["wg_"])
            A(lambda e: e.activation(out=wg_[:, :], in_=wg_[:, :], func=AF.Exp), r=["wg_"], w=["wg_"])
            A(lambda e: e.activation(out=flr_[:, :], in_=mt[:, :], func=AF.Exp, scale=-1.0), r=["mt"], w=["flr_"])
            for h in range(NH):
                T(lambda e, h=h: e.transpose(out=PB[0][0:64, h * NS:(h + 1) * NS], in_=qms32[:, h * 64:(h + 1) * 64], identity=identf[0:NS, 0:NS]),
                  r=["qms32", "identf"], w=["PB0"])
            V(lambda e: e.tensor_copy(out=qmTs[:, :, :], in_=PB[0][0:64, 0:NH * NS].rearrange("p (a b) -> p a b", a=NH)), r=["PB0"], w=["qmTs"])
            V(lambda e: e.scalar_tensor_tensor(out=kw[:, :, :], in0=kmf[0:NS, :].rearrange("p (h d) -> p h d", h=NH), scalar=0.125,
                                               in1=wg_[:, :].unsqueeze(2).to_broadcast([NS, NH, HD]), op0=ALU.mult, op1=ALU.mult), r=["kmf", "wg_"], w=["kw"])
            G(lambda e: e.memset(vaugS[:, :, :], 1.0), w=["vaugS"])
            V(lambda e: e.tensor_copy(out=vaugS[:, :, 0:64], in_=vms32[:, :].rearrange("p (h d) -> p h d", h=NH)), r=["vms32", "vaugS"], w=["vaugS"])
            V(lambda e: e.tensor_tensor(out=Adiag[:, :, :], in0=a_[:, :].unsqueeze(1).to_broadcast([NS, NS, NH]),
                                        in1=identf[0:NS, 0:NS].unsqueeze(2).to_broadcast([NS, NS, NH]), op=ALU.mult), r=["a_", "identf"], w=["Adiag"])
            T(lambda e: e.matmul(PB[1][0:64, 0:NS * NH], lhsT=onesf[0:NS, 0:64], rhs=Adiag[:, :, :].rearrange("p a b -> p (a b)"), start=True, stop=True),
              r=["onesf", "Adiag"], w=["PB1"])
            V(lambda e: e.tensor_copy(out=abc[:, :], in_=PB[1][0:64, 0:NS * NH]), r=["PB1"], w=["abc"])
            G(lambda e: e.memset(E64[:, :, :], 1.0), w=["E64"])
            G(lambda e: e.affine_select(out=E64[:, :, :], in_=E64[:, :, :], pattern=[[1, NS], [-1, NS]], compare_op=ALU.is_equal, fill=0.0, base=0, channel_multiplier=0),
              r=["E64"], w=["E64"])
            abc3 = abc[:, :].rearrange("p (b h) -> p b h", h=NH)
            for h in range(NH):
                s_ = h % 2
                ck = "Ch%d" % s_
                DMA(Ch[s_][:, :, 0:64], stC[l, :, h, :, :].rearrange("b k v -> k b v"), w=[ck], dkey=ck)
                S.op("sp", lambda e, s_=s_, h=h: e.dma_start(out=Ch[s_][:, :, 64:65], in_=stn[l, :, h, :].rearrange("b k -> k b").unsqueeze(2), allow_slow_non_contiguous=True),
                     reads=[ck], writes=[ck], dma=True, dkey=ck)
                V(lambda e, h=h: e.tensor_tensor(out=qmask[:, :, :], in0=qmTs[:, h, :].unsqueeze(1).to_broadcast([64, NS, NS]), in1=E64[:, :, :], op=ALU.mult),
                  r=["qmTs", "E64"], w=["qmask"])
                for b_ in range(NS):
                    T(lambda e, b_=b_, s_=s_: e.matmul(PB[2][0:NS, 0:65], lhsT=qmask[:, b_, :], rhs=Ch[s_][:, b_, :], start=(b_ == 0), stop=(b_ == NS - 1)),
                      r=["qmask", ck], w=["PB2"])
                V(lambda e, h=h: e.tensor_copy(out=qC[:, h, :], in_=PB[2][0:NS, 0:65]), r=["PB2"], w=["qC"])
                V(lambda e, h=h: e.tensor_tensor(out=kmaskH[:, :, :], in0=kw[:, h, :].unsqueeze(1).to_broadcast([NS, NS, HD]),
                                                 in1=identf[0:NS, 0:NS].unsqueeze(2).to_broadcast([NS, NS, HD]), op=ALU.mult), r=["kw", "identf"], w=["kmaskH"])
                for gi7, g7 in enumerate(range(0, NS, 7)):
                    n7 = min(7, NS - g7)
                    pbk = 3 + gi7 % 2
                    for ii in range(n7):
                        T(lambda e, ii=ii, g7=g7, h=h, pbk=pbk: e.matmul(PB[pbk][0:64, ii * 65:(ii + 1) * 65], lhsT=kmaskH[:, g7 + ii, :], rhs=vaugS[:, h, :], start=True, stop=True),
                          r=["kmaskH", "vaugS"], w=["PB%d" % pbk])
                    V(lambda e, g7=g7, n7=n7, s_=s_, h=h: e.tensor_tensor(out=Ch[s_][:, g7:g7 + n7, :], in0=Ch[s_][:, g7:g7 + n7, :],
                                                                         in1=abc3[:, g7:g7 + n7, h].unsqueeze(2).to_broadcast([64, n7, 65]), op=ALU.mult),
                      r=[ck, "abc", "PB2"], w=[ck])
                    V(lambda e, g7=g7, n7=n7, s_=s_, pbk=pbk: e.tensor_tensor(out=Ch[s_][:, g7:g7 + n7, :], in0=Ch[s_][:, g7:g7 + n7, :],
                                                                             in1=PB[pbk][0:64, 0:n7 * 65].rearrange("p (a b) -> p a b", a=n7), op=ALU.add),
                      r=[ck, "PB%d" % pbk], w=[ck])
                DMA(mCs[l, :, h, :, :].rearrange("b k v -> k b v"), Ch[s_][:, :, 0:64], r=[ck], dkey=ck, o=True)
                S.op("sp", lambda e, s_=s_, h=h: e.dma_start(out=mns[l, :, h, :].rearrange("b k -> k b").unsqueeze(2), in_=Ch[s_][:, :, 64:65], allow_slow_non_contiguous=True),
                     reads=[ck], dma=True, dkey=ck, out=True)
            V(lambda e: e.tensor_tensor(out=prq[:, :], in0=qms32[:, :], in1=kmf[0:NS, :], op=ALU.mult), r=["qms32", "kmf"], w=["prq"])
            V(lambda e: e.reduce_sum(out=qk[:, :], in_=prq[:, :].rearrange("p (h d) -> p h d", h=NH), axis=AX.X), r=["prq"], w=["qk"])
            V(lambda e: e.scalar_tensor_tensor(out=qk[:, :], in0=qk[:, :], scalar=0.125, in1=wg_[:, :], op0=ALU.mult, op1=ALU.mult), r=["qk", "wg_"], w=["qk"])
            V(lambda e: e.tensor_tensor(out=hm[0:NS, :, :], in0=qC[:, :, 0:64], in1=a_[:, :].unsqueeze(2).to_broadcast([NS, NH, HD]), op=ALU.mult), r=["qC", "a_"], w=["hm"])
            V(lambda e: e.tensor_tensor(out=t2[:, :, :], in0=vms32[:, :].rearrange("p (h d) -> p h d", h=NH), in1=qk[:, :].unsqueeze(2).to_broadcast([NS, NH, HD]), op=ALU.mult),
              r=["vms32", "qk"], w=["t2"])
            V(lambda e: e.tensor_tensor(out=hm[0:NS, :, :], in0=hm[0:NS, :, :], in1=t2[:, :, :], op=ALU.add), r=["hm", "t2"], w=["hm"])
            V(lambda e: e.tensor_tensor(out=den[:, :], in0=qC[:, :, 64], in1=a_[:, :], op=ALU.mult), r=["qC", "a_"], w=["den"])
            V(lambda e: e.tensor_tensor(out=den[:, :], in0=den[:, :], in1=qk[:, :], op=ALU.add), r=["den", "qk"], w=["den"])
            V(lambda e: e.scalar_tensor_tensor(out=den[:, :], in0=den[:, :], scalar=-1.0, in1=den[:, :], op0=ALU.mult, op1=ALU.max), r=["den"], w=["den"])
            V(lambda e: e.tensor_tensor(out=den[:, :], in0=den[:, :], in1=flr_[:, :], op=ALU.max), r=["den", "flr_"], w=["den"])
            V(lambda e: e.reciprocal(out=den[:, :], in_=den[:, :]), r=["den"], w=["den"])
            V(lambda e: e.tensor_tensor(out=hm[0:NS, :, :], in0=hm[0:NS, :, :], in1=den[:, :].unsqueeze(2).to_broadcast([NS, NH, HD]), op=ALU.mult), r=["hm", "den"], w=["hm"])
            mlstm_finish(L, NS)

            if DBG.get("sstop", 99) <= 4:
                return
            bufS = SA.alloc([NS, 15, 256])
            accp = SA.alloc([NS, 256])
            dS = SA.alloc([NS, 256])
            dTs = SA.alloc([64, 4, NS], BF16)
            DMA(bufS[:, :, :], stp[l], w=["bufS"], dkey="bufS")
            for g, wd_ in enumerate(POOL_W):
                V(lambda e, g=g, wd_=wd_: e.reduce_sum(out=accp[:, g * 64:(g + 1) * 64],
                                                       in_=bufS[:, 15 - (wd_ - 1):15, g * 64:(g + 1) * 64].rearrange("p r c -> p c r"), axis=AX.X),
                  r=["bufS", "accp"], w=["accp"])
                V(lambda e, g=g: e.tensor_tensor(out=accp[:, g * 64:(g + 1) * 64], in0=accp[:, g * 64:(g + 1) * 64], in1=uf[0:NS, g * 64:(g + 1) * 64], op=ALU.add),
                  r=["accp", "uf"], w=["accp"])
                V(lambda e, g=g, wd_=wd_: e.scalar_tensor_tensor(out=dS[:, g * 64:(g + 1) * 64], in0=accp[:, g * 64:(g + 1) * 64], scalar=1.0 / wd_,
                                                                 in1=uf[0:NS, g * 64:(g + 1) * 64], op0=ALU.mult, op1=ALU.subtract), r=["accp", "uf", "dS"], w=["dS"])
            for g in range(4):
                T(lambda e, g=g: e.transpose(out=PB[0][0:64, g * NS:(g + 1) * NS], in_=dS[:, g * 64:(g + 1) * 64], identity=identf[0:NS, 0:NS]), r=["dS", "identf"], w=["PB0"])
            A(lambda e: e.copy(out=dTs[:, :, :], in_=PB[0][0:64, 0:4 * NS].rearrange("p (a b) -> p a b", a=4)), r=["PB0"], w=["dTs"])
            for c in range(2):
                T(lambda e, c=c: e.matmul(PB[1][:, c * NS:(c + 1) * NS], lhsT=wpad[:, 2 * c, :], rhs=dTs[:, 2 * c, :], start=True, stop=False), r=["wpad", "dTs"], w=["PB1"])
                T(lambda e, c=c: e.matmul(PB[1][:, c * NS:(c + 1) * NS], lhsT=wpad[:, 2 * c + 1, :], rhs=dTs[:, 2 * c + 1, :], start=False, stop=True), r=["wpad", "dTs"], w=["PB1"])
                V(lambda e, c=c: e.tensor_scalar(out=mixT[:, 6 + c, 0:NS], in0=PB[1][:, c * NS:(c + 1) * NS], scalar1=psc[:, c:c + 1], scalar2=None, op0=ALU.mult),
                  r=["PB1", "psc", "mixT"], w=["mixT"])
            DMA(pbs[l, :, 0:14, :], bufS[:, 1:15, :], r=["bufS"], dkey="bufS", o=True)
            DMA(pbs[l, :, 14, :], uf[0:NS, :], r=["uf"], dkey="uf", o=True)

        def phase_F(l):
            S.barrier()
            AR.reset()
            tiles_all = list(range(ntp)) + ([ntp] if with_samples else [])
            GS = 8
            groups = [tiles_all[i:i + GS] for i in range(0, ntp, GS)]
            if with_samples:
                groups[-1] = groups[-1] + [ntp] if ntp not in groups[-1] else groups[-1]
            TGM = (GS + 1) * 128
            h2T = AR.alloc([128, 8, TGM], BF16)
            act = AR.alloc([128, 11, TGM], BF16)
            yacc = AR.alloc([128, GS + 1, D])
            mbuf = [AR.alloc([128, D]) for _ in range(2)]
            xt = AR.alloc([128, D])
            tmp = AR.alloc([128, D])
            h2f = AR.alloc([128, D])
            hb = AR.alloc([128, D], BF16)
            wgs = [AR.alloc([128, 8, 128]) for _ in range(2)]
            wus = [AR.alloc([128, 8, 128]) for _ in range(2)]
            wgb = [AR.alloc([128, 8, 128], BF16) for _ in range(2)]
            wub = [AR.alloc([128, 8, 128], BF16) for _ in range(2)]
            wds = [AR.alloc([128, D]) for _ in range(2)]
            wdb = AR.alloc([128, 11, D], BF16)
            sgb = [AR.alloc([128, 512], BF16) for _ in range(2)]
            ss = AR.alloc([128, 1])
            rstd = AR.alloc([128, 1])
            nexp = NE if l == 1 else 1
            if l == 1:
                h2Tf = AR.alloc([128, 8, 128])
                wr = AR.alloc([128, 8, NE])
                gts = AR.alloc([128, GS + 1, NE])
                lg = AR.alloc([128, NE])
                lmx = AR.alloc([128, 1])
                ee = AR.alloc([128, NE])
                mk1 = AR.alloc([128, NE])
                e2 = AR.alloc([128, NE])
                m2 = AR.alloc([128, 1])
                DMA(wr[:, :, :], w_router.rearrange("(k p) e -> p k e", p=128), w=["wr"], dkey="wr")
            else:
                fnb = None
            if l == 1:
                fnb = AR.alloc([128, D])
                DMA(fnb[:, :], fnorm_w.partition_broadcast(128), w=["fnb"], dkey="fnb")
            wcnt = [0]
            dcnt = [0]

            def modload(slot, ti, part, R):
                smp = (ti == ntp)
                key = "mbuf%d" % slot
                if smp:
                    DMA(mbuf[slot][0:R, :], MODS[l, 1, :, part, :], r=[("MODS", l, 1)], w=[key], dkey=key)
                else:
                    DMA(mbuf[slot][:, :], MODP[l, 1, :, part, :], r=[("MODP", l, 1)], w=[key], dkey=key)
                return key

            for grp in groups:
                ng = len(grp)
                TG = ng * 128
                for gi_, ti in enumerate(grp):
                    smp = (ti == ntp)
                    R = NS if smp else 128
                    rows = slice(ti * 128, ti * 128 + R)
                    DMA(xt[0:R, :], XA[rows, :], r=[("XA", ti)], w=["xt"], dkey="xt")
                    k0 = modload(0, ti, 0, R)
                    k1 = modload(1, ti, 1, R)
                    if smp:
                        V(lambda e: e.memset(hb[:, :], 0.0), r=["hb"], w=["hb"])
                        if l == 1:
                            V(lambda e: e.memset(h2f[:, :], 0.0), r=["h2f"], w=["h2f"])
                    A(lambda e, R=R: e.activation(out=tmp[0:R, :], in_=xt[0:R, :], func=AF.Square, accum_out=ss[0:R, :]), r=["xt"], w=["tmp", "ss"])
                    A(lambda e, R=R: e.activation(out=rstd[0:R, :], in_=ss[0:R, :], func=AF.Ln, scale=1.0 / D, bias=EPS), r=["ss"], w=["rstd"])
                    A(lambda e, R=R: e.activation(out=rstd[0:R, :], in_=rstd[0:R, :], func=AF.Exp, scale=-0.5), r=["rstd"], w=["rstd"])
                    V(lambda e, R=R: e.scalar_tensor_tensor(out=tmp[0:R, :], in0=xt[0:R, :], scalar=rstd[0:R, 0:1], in1=mbuf[0][0:R, :], op0=ALU.mult, op1=ALU.mult),
                      r=["xt", "rstd", k0, "tmp"], w=["tmp"])
                    V(lambda e, R=R: e.tensor_tensor(out=h2f[0:R, :], in0=tmp[0:R, :], in1=mbuf[1][0:R, :], op=ALU.add), r=["tmp", k1], w=["h2f"])
                    A(lambda e, R=R: e.copy(out=hb[0:R, :], in_=h2f[0:R, :]), r=["h2f"], w=["hb"])
                    for k in range(8):
                        T(lambda e, k=k: e.transpose(out=PT[0][:, k * 128:(k + 1) * 128], in_=hb[:, k * 128:(k + 1) * 128], identity=identb[:, :]),
                          r=["hb", "identb"], w=["PT0"])
                    A(lambda e, gi_=gi_: e.copy(out=h2T[:, :, gi_ * 128:(gi_ + 1) * 128], in_=PT[0][:, :].rearrange("p (a b) -> p a b", a=8)),
                      r=["PT0"], w=[("h2T", gi_)])
                    if l == 1:
                        for k in range(8):
                            T(lambda e, k=k: e.transpose(out=PB[2 + k // 4][:, (k % 4) * 128:(k % 4 + 1) * 128], in_=h2f[:, k * 128:(k + 1) * 128], identity=identf[:, :]),
                              r=["h2f", "identf"], w=["PB%d" % (2 + k // 4)])
                        for hh in range(2):
                            V(lambda e, hh=hh: e.tensor_copy(out=h2Tf[:, hh * 4:(hh + 1) * 4, :], in_=PB[2 + hh][:, :].rearrange("p (a b) -> p a b", a=4)),
                              r=["PB%d" % (2 + hh)], w=["h2Tf"])
                        for k in range(8):
                            T(lambda e, k=k: e.matmul(PB[4][:, 0:NE], lhsT=h2Tf[:, k, :], rhs=wr[:, k, :], start=(k == 0), stop=(k == 7)),
                              r=["h2Tf", "wr"], w=["PB4"])
                        V(lambda e: e.tensor_copy(out=lg[:, :], in_=PB[4][:, 0:NE]), r=["PB4"], w=["lg"])
                        V(lambda e: e.reduce_max(out=lmx[:, :], in_=lg[:, :], axis=AX.X), r=["lg"], w=["lmx"])
                        V(lambda e: e.tensor_scalar(out=ee[:, :], in0=lg[:, :], scalar1=lmx[:, 0:1], scalar2=None, op0=ALU.subtract), r=["lg", "lmx"], w=["ee"])
                        A(lambda e: e.activation(out=ee[:, :], in_=ee[:, :], func=AF.Exp), r=["ee"], w=["ee"])
                        V(lambda e: e.tensor_single_scalar(out=mk1[:, :], in_=ee[:, :], scalar=1.0, op=ALU.is_ge), r=["ee"], w=["mk1"])
                        V(lambda e: e.scalar_tensor_tensor(out=e2[:, :], in0=mk1[:, :], scalar=-2.0, in1=ee[:, :], op0=ALU.mult, op1=ALU.add),
                          r=["mk1", "ee"], w=["e2"])
                        V(lambda e: e.reduce_max(out=m2[:, :], in_=e2[:, :], axis=AX.X), r=["e2"], w=["m2"])
                        V(lambda e: e.tensor_scalar(out=e2[:, :], in0=e2[:, :], scalar1=m2[:, 0:1], scalar2=None, op0=ALU.is_ge), r=["e2", "m2"], w=["e2"])
                        V(lambda e: e.tensor_tensor(out=e2[:, :], in0=e2[:, :], in1=mk1[:, :], op=ALU.add), r=["e2", "mk1"], w=["e2"])
                        V(lambda e: e.tensor_tensor(out=e2[:, :], in0=e2[:, :], in1=ee[:, :], op=ALU.mult), r=["e2", "ee"], w=["e2"])
                        V(lambda e: e.tensor_scalar(out=m2[:, :], in0=m2[:, :], scalar1=1.0, scalar2=1e-6, op0=ALU.add, op1=ALU.max), r=["m2", "e2"], w=["m2"])
                        V(lambda e: e.reciprocal(out=m2[:, :], in_=m2[:, :]), r=["m2"], w=["m2"])
                        V(lambda e, gi_=gi_: e.tensor_scalar(out=gts[:, gi_, :], in0=e2[:, :], scalar1=m2[:, 0:1], scalar2=None, op0=ALU.mult),
                          r=["e2", "m2"], w=[("gts", gi_)])
                blocks = [(c0, min(512, TG - c0)) for c0 in range(0, TG, 512)]
                for ex in range(nexp):
                    wg_d = w_eg[ex] if l == 1 else w_fg
                    wu_d = w_eu[ex] if l == 1 else w_fu
                    wd_d = w_ed[ex] if l == 1 else w_fd
                    for half in range(2):
                        for fl_ in range(11):
                            fc = half * 11 + fl_
                            s_ = wcnt[0] % 2
                            wcnt[0] += 1
                            DMA(wgs[s_][:, :, :], wg_d[:, fc * 128:(fc + 1) * 128].rearrange("(k p) c -> p k c", p=128), w=["wgs%d" % s_], dkey="wgs%d" % s_)
                            DMA(wus[s_][:, :, :], wu_d[:, fc * 128:(fc + 1) * 128].rearrange("(k p) c -> p k c", p=128), w=["wus%d" % s_], dkey="wus%d" % s_)
                            G(lambda e, s_=s_: e.tensor_copy(out=wgb[s_][:, :, :], in_=wgs[s_][:, :, :]), r=["wgs%d" % s_], w=["wgb%d" % s_])
                            G(lambda e, s_=s_: e.tensor_copy(out=wub[s_][:, :, :], in_=wus[s_][:, :, :]), r=["wus%d" % s_], w=["wub%d" % s_])
                            d_ = dcnt[0] % 2
                            dcnt[0] += 1
                            DMA(wds[d_][:, :], wd_d[fc * 128:(fc + 1) * 128, :], w=["wds%d" % d_], dkey="wds%d" % d_)
                            A(lambda e, d_=d_, fl_=fl_: e.copy(out=wdb[:, fl_, :], in_=wds[d_][:, :]), r=["wds%d" % d_], w=[("wdb", fl_)])
                            for bi, (c0, cw) in enumerate(blocks):
                                pg = PB[(bi % 2) * 2]
                                pu = PB[(bi % 2) * 2 + 1]
                                pgk = "PB%d" % ((bi % 2) * 2)
                                puk = "PB%d" % ((bi % 2) * 2 + 1)
                                rk = [("h2T", gi_) for gi_ in range(c0 // 128, (c0 + cw) // 128)]
                                for k in range(8):
                                    T(lambda e, k=k, s_=s_, pg=pg, c0=c0, cw=cw: e.matmul(pg[:, 0:cw], lhsT=wgb[s_][:, k, :], rhs=h2T[:, k, c0:c0 + cw], start=(k == 0), stop=(k == 7)),
                                      r=["wgb%d" % s_] + rk, w=[pgk])
                                for k in range(8):
                                    T(lambda e, k=k, s_=s_, pu=pu, c0=c0, cw=cw: e.matmul(pu[:, 0:cw], lhsT=wub[s_][:, k, :], rhs=h2T[:, k, c0:c0 + cw], start=(k == 0), stop=(k == 7)),
                                      r=["wub%d" % s_] + rk, w=[puk])
                                sg = sgb[bi % 2]
                                sgk = "sgb%d" % (bi % 2)
                                A(lambda e, pg=pg, sg=sg, cw=cw: e.activation(out=sg[:, 0:cw], in_=pg[:, 0:cw], func=AF.Silu), r=[pgk], w=[sgk])
                                V(lambda e, pu=pu, sg=sg, cw=cw, fl_=fl_, c0=c0: e.tensor_tensor(out=act[:, fl_, c0:c0 + cw], in0=pu[:, 0:cw], in1=sg[:, 0:cw], op=ALU.mult),
                                  r=[puk, sgk], w=[("act", fl_, bi)])
                        for gi_, ti in enumerate(grp):
                            for dh in range(2):
                                pz = PB[4 + (gi_ * 2 + dh) % 2]
                                pk = "PB%d" % (4 + (gi_ * 2 + dh) % 2)
                                bi = (gi_ * 128) // 512
                                for fl_ in range(11):
                                    T(lambda e, fl_=fl_, gi_=gi_, dh=dh, pz=pz: e.matmul(pz[:, :], lhsT=act[:, fl_, gi_ * 128:(gi_ + 1) * 128], rhs=wdb[:, fl_, dh * 512:(dh + 1) * 512],
                                                                                     start=(fl_ == 0), stop=(fl_ == 10)),
                                      r=[("act", fl_, bi), ("wdb", fl_)], w=[pk])
                                yk = ("yacc", gi_, dh)
                                ysl = yacc[:, gi_, dh * 512:(dh + 1) * 512]
                                first = (ex == 0 and half == 0)
                                if l == 1:
                                    if first:
                                        V(lambda e, pz=pz, ysl=ysl, gi_=gi_, ex=ex: e.tensor_scalar(out=ysl, in0=pz[:, :], scalar1=gts[:, gi_, ex:ex + 1], scalar2=None, op0=ALU.mult),
                                          r=[pk, ("gts", gi_)], w=[yk])
                                    else:
                                        V(lambda e, pz=pz, ysl=ysl, gi_=gi_, ex=ex: e.scalar_tensor_tensor(out=ysl, in0=pz[:, :], scalar=gts[:, gi_, ex:ex + 1], in1=ysl, op0=ALU.mult, op1=ALU.add),
                                          r=[pk, ("gts", gi_), yk], w=[yk])
                                else:
                                    if first:
                                        V(lambda e, pz=pz, ysl=ysl: e.tensor_copy(out=ysl, in_=pz[:, :]), r=[pk], w=[yk])
                                    else:
                                        V(lambda e, pz=pz, ysl=ysl: e.tensor_tensor(out=ysl, in0=pz[:, :], in1=ysl, op=ALU.add), r=[pk, yk], w=[yk])
                for gi_, ti in enumerate(grp):
                    smp = (ti == ntp)
                    R = NS if smp else 128
                    rows = slice(ti * 128, ti * 128 + R)
                    DMA(xt[0:R, :], XA[rows, :], r=[("XA", ti)], w=["xt"], dkey="xt")
                    k0 = modload(0, ti, 2, R)
                    yks = [("yacc", gi_, 0), ("yacc", gi_, 1)]
                    V(lambda e, gi_=gi_, R=R: e.tensor_tensor(out=yacc[0:R, gi_, :], in0=yacc[0:R, gi_, :], in1=mbuf[0][0:R, :], op=ALU.mult), r=yks + [k0], w=yks)
                    V(lambda e, gi_=gi_, R=R: e.tensor_tensor(out=yacc[0:R, gi_, :], in0=yacc[0:R, gi_, :], in1=xt[0:R, :], op=ALU.add), r=yks + ["xt"], w=yks)
                    if l == 0:
                        DMA(XB[rows, :], yacc[0:R, gi_, :], r=yks, w=[("XB", ti)], dkey=("yacc", gi_))
                    else:
                        A(lambda e, gi_=gi_, R=R: e.activation(out=tmp[0:R, :], in_=yacc[0:R, gi_, :], func=AF.Square, accum_out=ss[0:R, :]), r=yks, w=["tmp", "ss"])
                        A(lambda e, R=R: e.activation(out=rstd[0:R, :], in_=ss[0:R, :], func=AF.Ln, scale=1.0 / D, bias=EPS), r=["ss"], w=["rstd"])
                        A(lambda e, R=R: e.activation(out=rstd[0:R, :], in_=rstd[0:R, :], func=AF.Exp, scale=-0.5), r=["rstd"], w=["rstd"])
                        V(lambda e, gi_=gi_, R=R: e.scalar_tensor_tensor(out=yacc[0:R, gi_, :], in0=yacc[0:R, gi_, :], scalar=rstd[0:R, 0:1], in1=fnb[0:R, :], op0=ALU.mult, op1=ALU.mult),
                          r=yks + ["rstd", "fnb"], w=yks)
                        if smp:
                            DMA(ys, yacc[0:R, gi_, :], r=yks, dkey=("yacc", gi_), o=True)
                        else:
                            DMA(yp[rows, :], yacc[0:R, gi_, :], r=yks, dkey=("yacc", gi_), o=True)

        for l in range(2):
            if phases is None or ("M%d" % l) in phases:
                phase_M(l)
            if phases is None or ("F%d" % l) in phases:
                phase_F(l)
        S.finish()
        S.emit(st)
        nc._nops = len(S.ops)
        nc._nsem = S.nsem
    return nc


_OUT_ORDER = ("y_prompt", "y_sample", "fox_k_p", "fox_v_p", "fox_lf_p", "mlstm_C_p", "mlstm_n_p", "mlstm_m_p", "pool_buf_p",
              "fox_k_s", "fox_v_s", "fox_lf_s", "mlstm_C_s", "mlstm_n_s", "mlstm_m_s", "pool_buf_s")


def make_in_maps(inp, ntp=32, ncores=NCORES):
    f = lambda a: np.ascontiguousarray(np.asarray(a))
    maps = []
    gb = f(np.concatenate([inp["b_fox_f"], inp["b_mlstm_i"], inp["b_mlstm_f"]], axis=1))
    for c in range(ncores):
        b = c % 4
        m = {
            "xp": f(inp["x_prompt"][b, :ntp * 128]),
            "xs": f(inp["x_sample"][:, 0, :]),
            "call": f(np.concatenate([inp["c_sample"], inp["c_prompt"][b:b + 1]], axis=0)),
            "w_ada": f(inp["w_ada"]), "b_ada": f(inp["b_ada"]),
            "norm1_w": f(inp["norm1_w"]), "norm2_w": f(inp["norm2_w"]),
            "w_in": f(inp["w_in"]), "gbias": gb, "mnorm_w": f(inp["mlstm_norm_w"]),
            "w_pool": f(inp["w_pool"]), "pool_scale": f(inp["pool_scale"]), "w_out": f(inp["w_out"]),
            "w_fg": f(inp["w_ffn_gate"][0]), "w_fu": f(inp["w_ffn_up"][0]), "w_fd": f(inp["w_ffn_down"][0]),
            "w_router": f(inp["w_router"][0]), "w_eg": f(inp["w_exp_gate"][0]), "w_eu": f(inp["w_exp_up"][0]),
            "w_ed": f(inp["w_exp_down"][0]), "fnorm_w": f(inp["final_norm_w"][None, :]),
            "kpool": f(np.asarray(inp["cache_fox_k"]).reshape(2, -1, 384)),
            "vpool": f(np.asarray(inp["cache_fox_v"]).reshape(2, -1, 384)),
            "lfc": f(np.asarray(inp["cache_fox_lf"]).reshape(2 * NPOOL, 768)),
            "ptab": f(np.asarray(inp["page_table"]).reshape(NS * NPG, 1).astype(np.int32)),
            "gbase": np.arange(LPG, dtype=np.float32)[None, :],
            "stC": f(inp["state_mlstm_C"]), "stn": f(inp["state_mlstm_n"]), "stm": f(inp["state_mlstm_m"]),
            "stp": f(inp["state_pool"]),
        }
        maps.append(m)
    return maps


def kernel(**inputs):
    import os
    nc = build(ntp=32, use_cc=False, nvs=NPOOL // LPG)
    maps = make_in_maps(inputs)
    res = run_bass_kernel_spmd(nc, maps, core_ids=list(range(NCORES)))
    r = res.results
    B = 4
    out = {}
    out["y_prompt"] = np.stack([r[b]["yp"] for b in range(B)])
    out["y_sample"] = r[0]["ys"][:, None, :]
    out["fox_k_p"] = np.stack([r[b]["fkp"] for b in range(B)], axis=1).reshape(2, B, 4096, NH, HD)
    out["fox_v_p"] = np.stack([r[b]["fvp"] for b in range(B)], axis=1).reshape(2, B, 4096, NH, HD)
    out["fox_lf_p"] = np.stack([r[b]["flp"] for b in range(B)], axis=1)
    out["mlstm_C_p"] = np.stack([r[b]["mCp"] for b in range(B)], axis=1)
    out["mlstm_n_p"] = np.stack([r[b]["mnp"] for b in range(B)], axis=1)
    out["mlstm_m_p"] = np.stack([r[b]["mmp"] for b in range(B)], axis=1)
    out["pool_buf_p"] = np.stack([r[b]["pbp"] for b in range(B)], axis=1)
    out["fox_k_s"] = r[0]["fks"].reshape(2, NS, 1, NH, HD)
    out["fox_v_s"] = r[0]["fvs"].reshape(2, NS, 1, NH, HD)
    out["fox_lf_s"] = r[0]["fls"].reshape(2, NS, 1, NH)
    out["mlstm_C_s"] = r[0]["mCs"]
    out["mlstm_n_s"] = r[0]["mns"]
    out["mlstm_m_s"] = r[0]["mms"]
    out["pool_buf_s"] = r[0]["pbs"]
    return tuple(np.ascontiguousarray(out[k], dtype=np.float32) for k in _OUT_ORDER)
```
